# Optimizing a Trainium2 kernel written in Bass

```python
import math
import jax, jax.numpy as jnp
from jax import lax
import numpy as np

D_MODEL = 1024
BATCH = 32
SEQ = 2048
DEPTH = 2

CHUNK = 64
N_META = 16
ROPE_THETA = 10000.0
EPS = 1e-6
A_HEADS = 4
A_DK = 128
A_DV = 128
A_CONV = 4
B_HEADS = 4
B_DH = 128
IDX_HEADS = 8
IDX_DIM = 64
TOPK_MAX = 256
TOPK_DIV = 4
Q_BLOCK = 64
D_FF = 2816
FFN_CONV = 3

A_WIDTH = A_HEADS * A_DV
B_WIDTH = B_HEADS * B_DH
IN_SPLITS = (A_HEADS * A_DK, A_HEADS * A_DK, A_WIDTH, A_WIDTH, A_HEADS, A_HEADS,
             B_WIDTH, B_DH, B_DH, IDX_HEADS * IDX_DIM, IDX_DIM, IDX_HEADS)
IN_COLS = sum(IN_SPLITS)
SPLIT_POINTS = tuple(int(c) for c in np.cumsum(IN_SPLITS)[:-1])

kernel_name = "hybrid_gdn_dsa_convffn_meta"


def rms_norm(x, g):
    xf = x.astype(jnp.float32)
    y = xf * lax.rsqrt(jnp.mean(xf * xf, axis=-1, keepdims=True) + EPS)
    return (y * g.astype(jnp.float32)).astype(x.dtype)


def l2_norm(x):
    xf = x.astype(jnp.float32)
    return xf * lax.rsqrt(jnp.sum(xf * xf, axis=-1, keepdims=True) + EPS)


def rope_tables(T, dim):
    inv_freq = 1.0 / (ROPE_THETA ** (jnp.arange(0, dim, 2, dtype=jnp.float32) / dim))
    ang = jnp.arange(T, dtype=jnp.float32)[:, None] * inv_freq[None, :]
    return jnp.cos(ang), jnp.sin(ang)


def apply_rope(x, cos, sin):
    xf = x.astype(jnp.float32)
    x1, x2 = jnp.split(xf, 2, axis=-1)
    c, s = cos[:, None, :], sin[:, None, :]
    return jnp.concatenate([x1 * c - x2 * s, x2 * c + x1 * s], axis=-1).astype(x.dtype)


def causal_dwconv(x, w):
    width = w.shape[0]
    T = x.shape[1]
    xp = jnp.pad(x, ((0, 0), (width - 1, 0), (0, 0)))
    out = xp[:, 0:T] * w[0]
    for i in range(1, width):
        out = out + xp[:, i:i + T] * w[i]
    return out


def gated_deltanet(q, k, v, z, a, b, conv_w, a_log, dt_bias, norm_g):
    f32 = jnp.float32
    Bsz, T, _ = q.shape
    qkv = jax.nn.silu(causal_dwconv(jnp.concatenate([q, k, v], axis=-1), conv_w))
    q, k, v = jnp.split(qkv, [A_HEADS * A_DK, 2 * A_HEADS * A_DK], axis=-1)
    q = l2_norm(q.reshape(Bsz, T, A_HEADS, A_DK)) * (A_DK ** -0.5)
    k = l2_norm(k.reshape(Bsz, T, A_HEADS, A_DK))
    v = v.reshape(Bsz, T, A_HEADS, A_DV).astype(f32)
    g = -jnp.exp(a_log.astype(f32)) * jax.nn.softplus(a.astype(f32) + dt_bias.astype(f32))
    beta = jax.nn.sigmoid(b.astype(f32))
    pad = (-T) % CHUNK
    Tp = T + pad
    N = Tp // CHUNK

    def chunks(t):
        t = jnp.pad(t, ((0, 0), (pad, 0)) + ((0, 0),) * (t.ndim - 2))
        t = t.reshape((Bsz, N, CHUNK) + t.shape[2:])
        return jnp.moveaxis(t, 3, 1)

    qc, kc, vc, gch, bc = chunks(q), chunks(k), chunks(v), chunks(g), chunks(beta)
    gcum = jnp.cumsum(gch, axis=-1)
    pos = jnp.arange(CHUNK)
    incl = pos[:, None] >= pos[None, :]
    strict = pos[:, None] > pos[None, :]
    decay = jnp.exp(jnp.where(incl, gcum[..., :, None] - gcum[..., None, :], -jnp.inf))
    kk = jnp.einsum('bhncd,bhnsd->bhncs', kc, kc)
    m = jnp.where(strict, bc[..., None] * kk * decay, 0.0)
    lhs = m + jnp.eye(CHUNK, dtype=f32)
    rhs = jnp.concatenate([vc * bc[..., None], kc * (bc * jnp.exp(gcum))[..., None]], axis=-1)
    sol = lax.linalg.triangular_solve(lhs, rhs, left_side=True, lower=True, unit_diagonal=True)
    u, w = sol[..., :A_DV], sol[..., A_DV:]
    qk = jnp.einsum('bhncd,bhnsd->bhncs', qc, kc) * decay

    def step(S, inp):
        q_c, k_c, u_c, w_c, g_c, qk_c = inp
        v_new = u_c - jnp.einsum('bhcd,bhde->bhce', w_c, S)
        o = (jnp.einsum('bhcd,bhde->bhce', q_c * jnp.exp(g_c)[..., None], S)
             + jnp.einsum('bhcs,bhse->bhce', qk_c, v_new))
        g_last = g_c[..., -1]
        S = (S * jnp.exp(g_last)[..., None, None]
             + jnp.einsum('bhcd,bhce->bhde', k_c * jnp.exp(g_last[..., None] - g_c)[..., None], v_new))
        return S, o

    xs = tuple(jnp.moveaxis(t, 2, 0) for t in (qc, kc, u, w, gcum, qk))
    S0 = jnp.zeros((Bsz, A_HEADS, A_DK, A_DV), f32)
    _, o = lax.scan(step, S0, xs)
    o = jnp.transpose(o, (1, 0, 3, 2, 4)).reshape(Bsz, Tp, A_HEADS, A_DV)[:, pad:]
    o = rms_norm(o, norm_g) * jax.nn.silu(z.reshape(Bsz, T, A_HEADS, A_DV).astype(f32))
    return o.reshape(Bsz, T, A_WIDTH).astype(z.dtype)


def dsa_attention(q, k, v, iq, ik, iw, q_norm, k_norm, kidx_norm, cos_a, sin_a, cos_i, sin_i):
    f32 = jnp.float32
    Bsz, T, _ = q.shape
    S = T - N_META
    topk = min(TOPK_MAX, S // TOPK_DIV)
    q = apply_rope(rms_norm(q.reshape(Bsz, T, B_HEADS, B_DH), q_norm), cos_a, sin_a)
    k = apply_rope(rms_norm(k.reshape(Bsz, T, 1, B_DH), k_norm), cos_a, sin_a)[:, :, 0]
    iq = apply_rope(iq.reshape(Bsz, T, IDX_HEADS, IDX_DIM), cos_i, sin_i)
    ik = apply_rope(rms_norm(ik.reshape(Bsz, T, 1, IDX_DIM), kidx_norm), cos_i, sin_i)[:, :, 0]
    iw = iw * (IDX_HEADS ** -0.5 * IDX_DIM ** -0.5)
    scale = B_DH ** -0.5

    q_m, k_m, v_m = q[:, :N_META], k[:, :N_META], v[:, :N_META]
    s_mm = jnp.einsum('bqhd,bkd->bhqk', q_m, k_m).astype(f32) * scale
    o_meta = jnp.einsum('bhqk,bkd->bqhd', jax.nn.softmax(s_mm, axis=-1).astype(v.dtype), v_m)

    nb = S // Q_BLOCK
    q_r = q[:, N_META:].reshape(Bsz, nb, Q_BLOCK, B_HEADS, B_DH).swapaxes(0, 1)
    iq_r = iq[:, N_META:].reshape(Bsz, nb, Q_BLOCK, IDX_HEADS, IDX_DIM).swapaxes(0, 1)
    iw_r = iw[:, N_META:].reshape(Bsz, nb, Q_BLOCK, IDX_HEADS).swapaxes(0, 1)
    ik_r = ik[:, N_META:]
    kv_r = jnp.concatenate([k[:, N_META:], v[:, N_META:]], axis=-1)
    key_pos = jnp.arange(S)

    def one_block(args):
        j, qb, iqb, iwb = args
        q_pos = j * Q_BLOCK + jnp.arange(Q_BLOCK)
        limit = (q_pos // CHUNK + 1) * CHUNK
        valid = key_pos[None, :] < limit[:, None]
        logit_i = jnp.einsum('bqhd,bsd->bhqs', iqb, ik_r)
        score = jnp.einsum('bhqs,bqh->bqs', jax.nn.relu(logit_i), iwb).astype(f32)
        score = jnp.where(valid[None], score, -jnp.inf)
        _, sel = lax.top_k(score, topk)
        sel_valid = sel < limit[None, :, None]
        kv_sel = jax.vmap(lambda arr, ix: arr[ix])(kv_r, sel)
        k_sel, v_sel = kv_sel[..., :B_DH], kv_sel[..., B_DH:]
        s_meta = jnp.einsum('bqhd,bmd->bhqm', qb, k_m).astype(f32)
        s_sel = jnp.einsum('bqhd,bqkd->bhqk', qb, k_sel).astype(f32)
        s_sel = jnp.where(sel_valid[:, None], s_sel, -jnp.inf)
        p = jax.nn.softmax(jnp.concatenate([s_meta, s_sel], axis=-1) * scale, axis=-1).astype(v.dtype)
        return (jnp.einsum('bhqm,bmd->bqhd', p[..., :N_META], v_m)
                + jnp.einsum('bhqk,bqkd->bqhd', p[..., N_META:], v_sel))

    o_r = lax.map(one_block, (jnp.arange(nb), q_r, iq_r, iw_r))
    o_r = o_r.swapaxes(0, 1).reshape(Bsz, S, B_HEADS, B_DH)
    return jnp.concatenate([o_meta, o_r], axis=1).reshape(Bsz, T, B_WIDTH)


def hybrid_mixer(n, w_in, conv_a, a_log, dt_bias, a_out_norm, q_norm, k_norm, kidx_norm,
                 w_branch_a, w_branch_b, w_gate, b_gate, w_out, cos_a, sin_a, cos_i, sin_i):
    Bsz, T, D = n.shape
    proj = n @ w_in
    aq, ak, av, az, aa, ab, bq, bk, bv, iq, ik, iw = jnp.split(proj, SPLIT_POINTS, axis=-1)
    o_a = gated_deltanet(aq, ak, av, az, aa, ab, conv_a, a_log, dt_bias, a_out_norm)
    o_b = dsa_attention(bq, bk, bv, iq, ik, iw, q_norm, k_norm, kidx_norm, cos_a, sin_a, cos_i, sin_i)
    gates = jax.nn.sigmoid(n @ w_gate + b_gate).reshape(Bsz, T, 2, D)
    y = gates[:, :, 0] * (o_a @ w_branch_a) + gates[:, :, 1] * (o_b @ w_branch_b)
    return y @ w_out


def conv_gated_mlp(n, w_up, conv_w, w_down):
    gate, up = jnp.split(n @ w_up, 2, axis=-1)
    gate = causal_dwconv(gate, conv_w)
    return (jax.nn.silu(gate) * up) @ w_down


def setup_inputs(seed: int = 0) -> dict:
    key = jax.random.key(seed)
    ks = jax.random.split(key, 24)
    L, D = DEPTH, D_MODEL
    f32 = jnp.float32

    def nrm(k, shape, scale):
        return jax.random.normal(k, shape, f32) * scale

    def gain(k, shape):
        return 1.0 + nrm(k, shape, 0.02)

    a_init = jax.random.uniform(ks[5], (L, A_HEADS), f32, 1.0, 16.0)
    dt = jnp.exp(jax.random.uniform(ks[6], (L, A_HEADS), f32, math.log(1e-3), math.log(1e-1)))
    return {
        "x": nrm(ks[0], (BATCH, SEQ, D), 1.0),
        "meta_tokens": nrm(ks[1], (N_META, D), 0.5),
        "norm_mix": gain(ks[2], (L, D)),
        "w_in": nrm(ks[3], (L, D, IN_COLS), D ** -0.5),
        "conv_a": nrm(ks[4], (L, A_CONV, 2 * A_HEADS * A_DK + A_WIDTH), 0.5),
        "a_log": jnp.log(a_init),
        "dt_bias": dt + jnp.log(-jnp.expm1(-dt)),
        "a_out_norm": gain(ks[7], (L, A_DV)),
        "q_norm": gain(ks[8], (L, B_DH)),
        "k_norm": gain(ks[9], (L, B_DH)),
        "kidx_norm": gain(ks[10], (L, IDX_DIM)),
        "w_branch_a": nrm(ks[11], (L, A_WIDTH, D), A_WIDTH ** -0.5),
        "w_branch_b": nrm(ks[12], (L, B_WIDTH, D), B_WIDTH ** -0.5),
        "w_gate": nrm(ks[13], (L, D, 2 * D), D ** -0.5),
        "b_gate": nrm(ks[14], (L, 2 * D), 0.1),
        "w_out": nrm(ks[15], (L, D, D), D ** -0.5),
        "norm_ffn": gain(ks[16], (L, D)),
        "w_up": nrm(ks[17], (L, D, 2 * D_FF), D ** -0.5),
        "conv_ffn": nrm(ks[18], (L, FFN_CONV, D_FF), 0.5),
        "w_down": nrm(ks[19], (L, D_FF, D), D_FF ** -0.5),
    }


def reference(x, meta_tokens, norm_mix, w_in, conv_a, a_log, dt_bias, a_out_norm, q_norm, k_norm,
              kidx_norm, w_branch_a, w_branch_b, w_gate, b_gate, w_out, norm_ffn, w_up, conv_ffn,
              w_down) -> jnp.ndarray:
    Bsz = x.shape[0]
    T = N_META + x.shape[1]
    cos_a, sin_a = rope_tables(T, B_DH)
    cos_i, sin_i = rope_tables(T, IDX_DIM)
    meta = jnp.broadcast_to(meta_tokens.astype(x.dtype)[None], (Bsz, N_META, x.shape[2]))
    h = jnp.concatenate([meta, x], axis=1)
    for l in range(DEPTH):
        n = rms_norm(h, norm_mix[l])
        h = h + hybrid_mixer(n, w_in[l], conv_a[l], a_log[l], dt_bias[l], a_out_norm[l], q_norm[l],
                             k_norm[l], kidx_norm[l], w_branch_a[l], w_branch_b[l], w_gate[l], b_gate[l],
                             w_out[l], cos_a, sin_a, cos_i, sin_i).astype(h.dtype)
        n = rms_norm(h, norm_ffn[l])
        h = h + conv_gated_mlp(n, w_up[l], conv_ffn[l], w_down[l]).astype(h.dtype)
    return h[:, N_META:]
```

```python
import contextlib
import math
import numpy as np
import concourse.bass as bass
import concourse.mybir as mybir
from concourse.bass_utils import run_bass_kernel_spmd

F32 = mybir.dt.float32
BF16 = mybir.dt.bfloat16
ALU = mybir.AluOpType
AF = mybir.ActivationFunctionType

ENGS = ("pe", "act", "dve", "pool", "sp")
DMA_POOL = 16
EPOCH = 12000
EPS = 1e-6
NEGB = -30000.0


class Op:
    __slots__ = ("eng", "idx", "fn", "dma", "deps", "signal", "sem", "val")

    def __init__(self, eng, idx, fn, dma):
        self.eng, self.idx, self.fn, self.dma = eng, idx, fn, dma
        self.deps = ()
        self.signal = False
        self.sem = None
        self.val = 0


class Prog:
    def __init__(self, nc):
        self.nc = nc
        self.ops = {e: [] for e in ENGS}
        self.lastw = {}
        self.readers = {}
        self.fence = {}
        self.dma_since = []

    def barrier(self):
        f = list(self.dma_since)
        for e in ENGS:
            for o in reversed(self.ops[e]):
                if not o.dma:
                    f.append(o)
                    break
        self.dma_since = []
        for e in ENGS:
            self.fence[e] = list(self.fence.get(e, [])) + f

    def op(self, eng, fn, reads=(), writes=(), dma=False):
        o = Op(eng, len(self.ops[eng]), fn, dma)
        deps = {}
        if dma:
            self.dma_since.append(o)
        if self.fence.get(eng):
            for d in self.fence[eng]:
                if d.eng == eng and not d.dma and eng == "pe":
                    continue
                deps[(d.eng, d.idx)] = d
            self.fence[eng] = []

        def add(d, raw):
            if d is None:
                return
            if d.eng == eng and not d.dma:
                if eng == "pe":
                    return
            deps[(d.eng, d.idx)] = d

        for k in reads:
            for wr in self.lastw.get(k, ()):
                add(wr, True)
        for k in writes:
            rd = self.readers.get(k, {})
            prev = self.lastw.get(k, ())
            group = dma and prev and all(p.dma for p in prev) and not rd
            if not group:
                for wr in prev:
                    add(wr, False)
            for r in rd.values():
                if isinstance(r, list):
                    for r_ in r:
                        add(r_, None)
                else:
                    add(r, None)
        o.deps = tuple(deps.values())
        for d in o.deps:
            d.signal = True
        for k in reads:
            rd = self.readers.setdefault(k, {})
            if dma:
                rd.setdefault("dma", []).append(o)
            else:
                rd[eng] = o
        for k in writes:
            prev = self.lastw.get(k, ())
            if dma and prev and all(p.dma for p in prev) and not self.readers.get(k):
                self.lastw[k] = tuple(prev) + (o,)
            else:
                self.lastw[k] = (o,)
            self.readers[k] = {}
        self.ops[eng].append(o)
        return o

    def emit(self, final_ops=()):
        nc = self.nc
        for o in final_ops:
            o.signal = True
        n_sems = {}
        for e in ENGS:
            cnt = 0
            for o in self.ops[e]:
                if o.signal and not o.dma:
                    o.sem = (e, cnt // EPOCH)
                    o.val = cnt % EPOCH + 1
                    cnt += 1
            n_sems[e] = (cnt + EPOCH - 1) // EPOCH
        dma_sems = []
        for e in ENGS:
            dcnt = 0
            dvals = [0] * DMA_POOL
            for o in self.ops[e]:
                if o.signal and o.dma:
                    j = dcnt % DMA_POOL
                    dcnt += 1
                    dvals[j] += 16
                    o.sem = ("dma" + e, j)
                    o.val = dvals[j]
            dma_sems += [("dma" + e, j) for j in range(min(DMA_POOL, dcnt))]
        with contextlib.ExitStack() as st:
            sems = {}
            for e in ENGS:
                for ep in range(n_sems[e]):
                    sems[(e, ep)] = st.enter_context(nc.semaphore(f"s_{e}_{ep}"))
            for k in dma_sems:
                sems[k] = st.enter_context(nc.semaphore(f"s_{k[0]}_{k[1]}"))
            block = st.enter_context(nc.Block())

            def run(e, eng):
                waited = {}
                for o in self.ops[e]:
                    need = {}
                    for d in o.deps:
                        if d.val > need.get(d.sem, 0):
                            need[d.sem] = d.val
                    for sm, v in need.items():
                        if waited.get(sm, 0) >= v:
                            continue
                        eng.wait_ge(sems[sm], v)
                        waited[sm] = v
                    if o.signal and o.dma and o.val > 16 and waited.get(o.sem, 0) < o.val - 16:
                        eng.wait_ge(sems[o.sem], o.val - 16)
                        waited[o.sem] = o.val - 16
                    ins = o.fn(eng)
                    if o.signal:
                        ins.then_inc(sems[o.sem], 16 if o.dma else 1)
                if e == "sp":
                    for d in final_ops:
                        if waited.get(d.sem, 0) >= d.val:
                            continue
                        eng.wait_ge(sems[d.sem], d.val)
                        waited[d.sem] = d.val

            @block.sync
            def _(eng):
                run("sp", eng)

            @block.tensor
            def _(eng):
                run("pe", eng)

            @block.scalar
            def _(eng):
                run("act", eng)

            @block.vector
            def _(eng):
                run("dve", eng)

            @block.gpsimd
            def _(eng):
                run("pool", eng)


class Cfg:
    def __init__(self, D=1024, NCH=32, DFF=2816, NSEQ=4, DEPTH=2, TOPK=256, NIT=16, NSEG=0, NBT=8, GF=4, NWB=3, ARENA_MIN=0, PACK=0, POOLCONV=0, RATIO=5):
        self.D, self.NCH, self.DFF, self.NSEQ, self.DEPTH, self.TOPK, self.NIT = D, NCH, DFF, NSEQ, DEPTH, TOPK, NIT
        self.NSEG, self.NBT, self.GF, self.NWB, self.ARENA_MIN, self.PACK = NSEG, NBT, GF, NWB, ARENA_MIN, PACK
        self.POOLCONV = POOLCONV
        self.RATIO = RATIO
        self.KC = D // 128
        self.S = 64 * NCH
        self.Tp = 64 * (NCH + 1)
        self.NF = DFF // 128
        self.INC = 3408
        o = 0
        self.c = {}
        for name, w in (("ident", 128), ("r128", 128), ("r64", 128), ("blk64", 128), ("i4", 512),
                        ("mls", 64), ("mus", 64), ("mui", 64), ("pq", 4), ("frow", 32), ("i2", 64)):
            self.c[name] = (o, w)
            o += w
        self.NSMALL = o
        for name, w in (("cosA", self.Tp), ("sinA", self.Tp), ("cosI", self.Tp), ("sinI", self.Tp), ("rst", self.Tp)):
            self.c[name] = (o, w)
            o += w
        self.NCONST = o
        o = 0
        self.p = {}
        for name, w in (("gmix", self.KC), ("gffn", self.KC), ("conva", 48), ("convf", 3 * self.NF),
                        ("bgate", 2 * self.KC), ("aon", 1), ("qn", 1), ("kn", 1), ("kin", 1), ("alog", 1), ("dtb", 1)):
            self.p[name] = (o, w)
            o += w
        self.NP = o


def host_consts(cfg):
    C = np.zeros((128, cfg.NCONST), np.float32)

    def put(name, arr):
        o, w = cfg.c[name]
        C[: arr.shape[0], o:o + arr.shape[1]] = arr

    I = np.eye(128, dtype=np.float32)
    put("ident", I)
    R = np.zeros((128, 128), np.float32)
    for i in range(128):
        R[i, (i + 64) % 128] = 1
    put("r128", R)
    R2 = np.zeros((128, 128), np.float32)
    for i in range(128):
        b, j = divmod(i, 64)
        R2[i, b * 64 + (j + 32) % 64] = 1
    put("r64", R2)
    B = np.zeros((128, 128), np.float32)
    B[:64, :64] = 1
    B[64:, 64:] = 1
    put("blk64", B)
    put("i4", np.concatenate([I] * 4, axis=1))
    p = np.arange(64)[:, None]
    j = np.arange(64)[None, :]
    NEG = -10000.0
    put("mls", np.where(p > j, 0.0, NEG).astype(np.float32))
    put("mus", np.where(j > p, 0.0, NEG).astype(np.float32))
    put("mui", np.where(j >= p, 0.0, NEG).astype(np.float32))
    pq = np.zeros((128, 4), np.float32)
    pq[0] = [1, 0, 0, 1]
    pq[1] = [0, 1, -1, 0]
    put("pq", pq)
    put("i2", np.concatenate([np.eye(64, dtype=np.float32)] * 2, axis=0))
    put("frow", np.tile((2.0 ** -(np.arange(32, dtype=np.float64) + 1)).astype(np.float32)[None, :], (128, 1)))
    Tp = cfg.Tp
    pos = (np.arange(Tp) - 48).astype(np.float32)
    pos[:48] = 0

    def tables(dim, reps):
        inv = (1.0 / (10000.0 ** (np.arange(0, dim, 2, dtype=np.float32) / np.float32(dim)))).astype(np.float32)
        ang = pos[:, None] * inv[None, :]
        c, s = np.cos(ang).astype(np.float32), np.sin(ang).astype(np.float32)
        cf = np.concatenate([c, c], axis=1).T
        sf = np.concatenate([-s, s], axis=1).T
        return np.tile(cf, (reps, 1)), np.tile(sf, (reps, 1))

    ca, sa = tables(128, 1)
    ci, si = tables(64, 2)
    put("cosA", ca); put("sinA", sa); put("cosI", ci); put("sinI", si)
    rst = np.ones((128, Tp), np.float32)
    rst[:, ::64] = 0
    put("rst", rst)
    return C


def host_lparams(cfg, l, norm_mix, norm_ffn, conv_a, conv_ffn, b_gate, a_out_norm, q_norm, k_norm, kidx_norm, a_log, dt_bias):
    P = np.zeros((128, cfg.NP), np.float32)

    def put(name, arr):
        o, w = cfg.p[name]
        P[: arr.shape[0], o:o + arr.shape[1]] = arr

    put("gmix", norm_mix[l].reshape(cfg.KC, 128).T)
    put("gffn", norm_ffn[l].reshape(cfg.KC, 128).T)
    put("conva", conv_a[l].reshape(4, 12, 128).transpose(2, 1, 0).reshape(128, 48))
    put("convf", conv_ffn[l].reshape(3, cfg.NF, 128).transpose(2, 1, 0).reshape(128, 3 * cfg.NF))
    put("bgate", b_gate[l].reshape(2 * cfg.KC, 128).T)
    put("aon", a_out_norm[l].reshape(128, 1))
    put("qn", q_norm[l].reshape(128, 1))
    put("kn", k_norm[l].reshape(128, 1))
    put("kin", np.concatenate([kidx_norm[l], kidx_norm[l]]).reshape(128, 1))
    put("alog", a_log[l].reshape(4, 1))
    put("dtb", dt_bias[l].reshape(4, 1))
    return P


def build(cfg):
    nc = bass.Bass("TRN2", target_bir_lowering=False)
    D, KC, NCH, S, Tp, NF, NSEQ, DEPTH, TOPK = cfg.D, cfg.KC, cfg.NCH, cfg.S, cfg.Tp, cfg.NF, cfg.NSEQ, cfg.DEPTH, cfg.TOPK
    NCK = NCH + 1
    NBT, GF, NWB = cfg.NBT, cfg.GF, cfg.NWB
    x_d = nc.dram_tensor("x", [NSEQ, S, D], F32, kind="ExternalInput").ap()
    meta_d = nc.dram_tensor("meta", [16, D], F32, kind="ExternalInput").ap()
    const_d = nc.dram_tensor("consts", [128, cfg.NCONST], F32, kind="ExternalInput").ap()
    lp_d = nc.dram_tensor("lparams", [DEPTH, 128, cfg.NP], F32, kind="ExternalInput").ap()
    w_in_d = nc.dram_tensor("w_in", [DEPTH, D, cfg.INC], F32, kind="ExternalInput").ap()
    w_ba_d = nc.dram_tensor("w_branch_a", [DEPTH, 512, D], F32, kind="ExternalInput").ap()
    w_bb_d = nc.dram_tensor("w_branch_b", [DEPTH, 512, D], F32, kind="ExternalInput").ap()
    w_gate_d = nc.dram_tensor("w_gate", [DEPTH, D, 2 * D], F32, kind="ExternalInput").ap()
    w_out_d = nc.dram_tensor("w_out", [DEPTH, D, D], F32, kind="ExternalInput").ap()
    w_up_d = nc.dram_tensor("w_up", [DEPTH, D, 2 * cfg.DFF], F32, kind="ExternalInput").ap()
    w_down_d = nc.dram_tensor("w_down", [DEPTH, cfg.DFF, D], F32, kind="ExternalInput").ap()
    y_d = nc.dram_tensor("y", [NSEQ, S, D], F32, kind="ExternalOutput").ap()
    gq_d = nc.dram_tensor("gq_scr", [16, 128, Tp], BF16).ap()
    grow_d = nc.dram_tensor("grow_scr", [8, Tp], F32).ap()
    hsp_d = nc.dram_tensor("hsp_scr", [128, KC * Tp], F32).ap()

    def tab_d(name):
        o, w = cfg.c[name]
        return const_d[:, o:o + w]

    segs = []
    if cfg.NSEG == 0:
        segs.append((0, 1))
        a = 1
        while a < NCK:
            segs.append((a, min(NCK, a + cfg.NBT)))
            a += cfg.NBT
    else:
        base, rem = divmod(NCK, cfg.NSEG)
        a = 0
        for i in range(cfg.NSEG):
            n_ = base + (1 if i < rem else 0)
            if n_:
                segs.append((a, a + n_))
            a += n_
    NCKm = max(b - a for a, b in segs)
    Tsm = 64 * NCKm

    P = Prog(nc)
    st = contextlib.ExitStack()
    with st:
        def sb(name, shape, dt):
            return st.enter_context(nc.sbuf_tensor(name, shape, dt))

        cst = sb("cst", [128, cfg.NSMALL], F32)
        lpt = sb("lpt", [128, cfg.NP], F32)
        ident_b = sb("ident_b", [128, 128], BF16)
        ones_b = sb("ones_b", [128, 128], BF16)
        r128_b = sb("r128_b", [128, 128], BF16)
        r64_b = sb("r64_b", [128, 128], BF16)
        blk64_b = sb("blk64_b", [128, 128], BF16)
        i4_b = sb("i4_b", [128, 512], BF16)
        i4n_b = sb("i4n_b", [128, 512], BF16)
        HWD = KC * Tp
        ARH = max(HWD, cfg.ARENA_MIN)
        ARO = max(4 * Tp + 64, cfg.ARENA_MIN)
        arenaH = sb("arenaH", [128, ARH], F32)
        arenaN = sb("arenaN", [128, HWD // 2], F32)
        arenaO = sb("arenaO", [128, ARO], F32)
        hT = arenaH[:, 0:HWD].rearrange("p (k t) -> p k t", k=KC)
        nT = arenaN[:, :].bitcast(BF16).rearrange("p (k t) -> p k t", k=KC)
        oab = arenaO[:, 0:4 * Tp].bitcast(BF16).rearrange("p (k t) -> p k t", k=8)
        wbuf = [sb(f"wbuf{i}", [128, 4096], BF16) for i in range(NWB)]
        wab = sb("wab", [128, KC, 64], BF16)
        sq_b = sb("sq_b", [128, KC, 512], BF16)
        rs_f = [sb(f"rs_f{i}", [128, 512], F32) for i in range(2)]
        t_f = [sb(f"t_f{i}", [128, 512], F32) for i in range(2)]
        t_b = [sb(f"t_b{i}", [128, 512], BF16) for i in range(2)]
        rtab_all = sb("rtab_all", [128, 4, 512], F32)
        rtab = [rtab_all[:, 0:2, :], rtab_all[:, 2:4, :]]
        sml = sb("sml", [128, 16], F32)
        bis = sb("bis", [128, 8], F32)
        dfs = sb("dfs", [128, 32], F32)
        bisA = sb("bisA", [128, 8], F32)
        dfsA = sb("dfsA", [128, 32], F32)
        ps = [st.enter_context(nc.psum_tensor(f"ps{i}", [128, 512], F32)) for i in range(7)]
        psb = st.enter_context(nc.psum_tensor("psb", [128, 1024], BF16))
        NQT = S // 128

        class Carver:
            def __init__(self, arena, nwords, o0=0):
                self.a, self.n, self.o = arena, nwords, o0

            def get(self, rows, free, dt):
                n = 1
                for f_ in free:
                    n *= f_
                words = n if dt == F32 else (n + 1) // 2
                words = (words + 7) // 8 * 8
                assert self.o + words <= self.n, (self.o, words, self.n)
                v = self.a[0:rows, self.o:self.o + words]
                self.o += words
                if dt == BF16:
                    v = v.bitcast(BF16)
                v = v[:, 0:n]
                if len(free) == 2:
                    v = v.rearrange("p (a b) -> p a b", a=free[0])
                return v

        def cs(name, rows=128):
            o, w = cfg.c[name]
            assert o + w <= cfg.NSMALL
            return cst[0:rows, o:o + w]

        def lp(name, j=0, rows=128):
            o, w = cfg.p[name]
            return lpt[0:rows, o + j:o + j + 1]

        def MM(out, lhsT, rhs, start, stop, r, w):
            P.op("pe", lambda e: e.matmul(out, lhsT=lhsT, rhs=rhs, start=start, stop=stop), r, w)

        def TR(out, in_, idn, r, w):
            P.op("pe", lambda e: e.transpose(out, in_, idn), r, w)

        def ACT(out, in_, func, r, w, bias=None, scale=None, accum=None):
            kw = {}
            if bias is not None:
                kw["bias"] = bias
            if scale is not None:
                kw["scale"] = scale
            if accum is not None:
                kw["accum_out"] = accum
            P.op("act", lambda e: e.activation(out=out, in_=in_, func=func, **kw), r, w)

        def TT(out, a, b, op, r, w, eng="dve"):
            P.op(eng, lambda e: e.tensor_tensor(out=out, in0=a, in1=b, op=op), r, w)

        def TS(out, a, s1, s2, op0, op1, r, w, accum=None, eng="dve"):
            kw = {}
            if op1 is not None:
                kw["op1"] = op1
            if accum is not None:
                kw["accum_out"] = accum
            P.op(eng, lambda e: e.tensor_scalar(out=out, in0=a, scalar1=s1, scalar2=s2, op0=op0, **kw), r, w)

        def STT(out, a, s, b, op0, op1, r, w, eng="dve"):
            P.op(eng, lambda e: e.scalar_tensor_tensor(out=out, in0=a, scalar=s, in1=b, op0=op0, op1=op1), r, w)

        def CP(out, in_, r, w, eng="dve"):
            if eng == "act":
                P.op("act", lambda e: e.copy(out=out, in_=in_), r, w)
            else:
                P.op(eng, lambda e: e.tensor_copy(out=out, in_=in_), r, w)

        def MSET(ap, v, w, eng="dve"):
            P.op(eng, lambda e: e.memset(ap, v), (), w)

        def DMA(out, in_, r, w, eng="sp"):
            return P.op(eng, lambda e: e.dma_start(out=out, in_=in_), r, w, dma=True)

        blocks = [(t0, min(512, Tp - t0)) for t0 in range(0, Tp, 512)]
        psrr = [0]

        psmod = [5]

        def nps():
            psrr[0] = (psrr[0] + 1) % psmod[0]
            return psrr[0]

        wrr = [0]

        def wnext(nfree, a=None):
            i = wrr[0] % NWB
            wrr[0] += 1
            v = wbuf[i][:, 0:nfree]
            if a is not None:
                v = v.rearrange("p (a b) -> p a b", a=a)
            return v, f"wbuf{i}"

        def wload(src_ap, shape_view):
            n = 1
            for s_ in shape_view[1:]:
                n *= s_
            v, key = wnext(n, shape_view[1] if len(shape_view) == 3 else None)
            DMA(v, src_ap, (), [key], eng="pool")
            return v, key

        def wk(wd, l, c0, ncols):
            return wd[l].rearrange("(kc p) n -> p kc n", p=128)[:, :, c0:c0 + ncols]

        def rstd_from_ps(psi, w, rsb, rskey, inv_n, rows=128):
            ACT(rsb[0:rows, 0:w], ps[psi][0:rows, 0:w], AF.Ln, [f"ps{psi}", "sml"], [rskey], bias=EPS_AP[0:rows, 0:1], scale=inv_n)
            ACT(rsb[0:rows, 0:w], rsb[0:rows, 0:w], AF.Exp, [rskey], [rskey], scale=-0.5)

        DMA(cst[:], const_d[:, 0:cfg.NSMALL], (), ["cst"])
        EPS_AP = sml[:, 15:16]
        MSET(sml[:], 0.0, ["sml"])
        MSET(sml[:, 15:16], EPS, ["sml"])
        MSET(ones_b[:], 1.0, ["ones_b"])
        CP(ident_b[:], cs("ident"), ["cst"], ["ident_b"])
        CP(r128_b[:], cs("r128"), ["cst"], ["r128_b"])
        CP(r64_b[:], cs("r64"), ["cst"], ["r64_b"])
        CP(blk64_b[:], cs("blk64"), ["cst"], ["blk64_b"])
        CP(i4_b[:], cs("i4"), ["cst"], ["i4_b"])
        TS(i4n_b[:], cs("i4"), -1.0, None, ALU.mult, None, ["cst"], ["i4n_b"])
        MSET(wab[:], 0.0, ["wab"])
        identF = cs("ident")
        MSET(t_b[0][:, :], 0.0, ["t_b0"])
        for ft in range(16):
            DMA(gq_d[ft][:, 0:48], t_b[0][:, 0:48], ["t_b0"], [f"gq{ft}"])

        def rmsnorm_to_nT(gname):
            for bi, (t0, w) in enumerate(blocks):
                for kc in range(KC):
                    ACT(sq_b[:, kc, 0:w], hT[:, kc, t0:t0 + w], AF.Square, ["hT"], ["sq_b"])
                pi = nps()
                for kc in range(KC):
                    MM(ps[pi][:, 0:w], ones_b[:], sq_b[:, kc, 0:w], kc == 0, kc == KC - 1, ["ones_b", "sq_b"], [f"ps{pi}"])
                rsb = rs_f[bi % 2]
                rstd_from_ps(pi, w, rsb, f"rs_f{bi % 2}", 1.0 / D)
                for kc in range(KC):
                    STT(nT[:, kc, t0:t0 + w], hT[:, kc, t0:t0 + w], lp(gname, kc), rsb[:, 0:w], ALU.mult, ALU.mult,
                        ["hT", f"rs_f{bi % 2}", "lpt"], ["nT"])

        def proj_block(wv, wkey, col0, M, t0, w, pi, kcs=None, src=None, srckey="nT", srcoff=0):
            src = nT if src is None else src
            kcs = KC if kcs is None else kcs
            for kc in range(kcs):
                MM(ps[pi][0:M, 0:w], wv[:, kc, col0:col0 + M], src[:, srcoff + kc, t0:t0 + w], kc == 0, kc == kcs - 1,
                   [wkey, srckey], [f"ps{pi}"])

        final = []
        for sq in range(NSEQ):
            P.barrier()
            xin = Carver(arenaO, ARO).get(128, [D], F32)
            MSET(hT[:, :, 0:48], 0.0, ["hT"])
            for r in range(-1, NQT):
                if r < 0:
                    DMA(xin[0:16, :], meta_d, (), ["xin"])
                    rows, c0 = 16, 48
                else:
                    DMA(xin[:, :], x_d[sq, 128 * r:128 * (r + 1), :], (), ["xin"])
                    rows, c0 = 128, 64 + 128 * r
                for kc in range(KC):
                    pi = nps()
                    TR(ps[pi][:, 0:rows], xin[0:rows, kc * 128:(kc + 1) * 128], identF[0:rows, 0:rows], ["xin", "cst"], [f"ps{pi}"])
                    CP(hT[:, kc, c0:c0 + rows], ps[pi][:, 0:rows], [f"ps{pi}"], ["hT"], eng=("act" if kc % 2 else "dve"))

            for l in range(DEPTH):
                P.barrier()
                DMA(lpt[:], lp_d[l], (), ["lpt"])
                ACT(sml[0:4, 0:1], lp("alog", rows=4), AF.Exp, ["lpt"], ["sml"])
                TS(sml[0:4, 0:1], sml[0:4, 0:1], -1.0, None, ALU.mult, None, ["sml"], ["sml"])
                psmod[0] = 7
                DMA(hsp_d[:, :], arenaH[:, 0:HWD], ["hT"], ["hsp"])
                rmsnorm_to_nT("gmix")
                P.barrier()
                MSET(oab[:, :, 0:48], 0.0, ["oab"])
                cv = Carver(arenaH, ARH)
                f_aL = [cv.get(128, [Tp + 8], F32) for _ in range(2)]
                f_bL = [cv.get(128, [Tp], F32) for _ in range(2)]
                b_aL = [cv.get(128, [Tp], BF16) for _ in range(2)]
                b_bL = [cv.get(128, [Tp], BF16) for _ in range(2)]
                for i_ in range(2):
                    MSET(f_aL[i_][:, 0:4], 0.0, [f"f_a{i_}"])
                for grp in range(4):
                    wv, wkey = wload(wk(w_in_d, l, grp * 512, 512), [128, KC, 512])
                    for j in range(4):
                        ft = grp * 4 + j
                        pp = ft % 2
                        f_a, f_b, b_a, b_b = f_aL[pp], f_bL[pp], b_aL[pp], b_bL[pp]
                        ka, kb_, kba, kbb = f"f_a{pp}", f"f_b{pp}", f"b_a{pp}", f"b_b{pp}"
                        if grp < 3:
                            for (t0, w) in blocks:
                                pi = nps()
                                proj_block(wv, wkey, j * 128, 128, t0, w, pi)
                                CP(f_a[:, 3 + t0:3 + t0 + w], ps[pi][:, 0:w], [f"ps{pi}"], [ka], eng="act")
                            cw = lambda i, grp=grp, j=j: lp("conva", (grp * 4 + j) * 4 + i)
                            ceng = "pool" if (pp == 1 and cfg.POOLCONV) else "dve"
                            TS(f_b[:, :], f_a[:, 3:3 + Tp], cw(3), None, ALU.mult, None, [ka, "lpt"], [kb_], eng=ceng)
                            for i in (2, 1, 0):
                                STT(f_b[:, :], f_a[:, i:i + Tp], cw(i), f_b[:, :], ALU.mult, ALU.add, [ka, kb_, "lpt"], [kb_], eng=ceng)
                            if grp == 2:
                                ACT(b_a[:, :], f_b[:, :], AF.Silu, [kb_], [kba])
                            else:
                                ACT(f_b[:, :], f_b[:, :], AF.Silu, [kb_], [kb_])
                                ACT(b_b[:, :], f_b[:, :], AF.Square, [kb_], [kbb])
                                for bi, (t0, w) in enumerate(blocks):
                                    pi = nps()
                                    MM(ps[pi][:, 0:w], ones_b[:], b_b[:, t0:t0 + w], True, True, ["ones_b", kbb], [f"ps{pi}"])
                                    rsb = rs_f[bi % 2]
                                    rstd_from_ps(pi, w, rsb, f"rs_f{bi % 2}", 1.0)
                                    STT(b_a[:, t0:t0 + w], f_b[:, t0:t0 + w], (128.0 ** -0.5) if grp == 0 else 1.0, rsb[:, 0:w],
                                        ALU.mult, ALU.mult, [kb_, f"rs_f{bi % 2}"], [kba])
                        else:
                            for (t0, w) in blocks:
                                pi = nps()
                                proj_block(wv, wkey, j * 128, 128, t0, w, pi)
                                ACT(b_a[:, t0:t0 + w], ps[pi][:, 0:w], AF.Silu, [f"ps{pi}"], [kba])
                        DMA(gq_d[ft][:, 48:Tp], b_a[:, 48:Tp], [kba], [f"gq{ft}"])
                P.barrier()
                cv = Carver(arenaH, ARH)
                grs = cv.get(64, [Tp], F32)
                grs2 = cv.get(64, [Tp], F32)
                rstt = cv.get(4, [Tp], F32)
                DMA(rstt[:, :], tab_d("rst")[0:4, :], (), ["rstt"])
                wi = w_in_d[l].rearrange("(kc p) n -> p kc n", p=128)
                DMA(wab[:, :, 0:4], wi[:, :, 2048:2052], (), ["wab"], eng="pool")
                DMA(wab[:, :, 32:36], wi[:, :, 2052:2056], (), ["wab"], eng="pool")
                for (t0, w) in blocks:
                    pi = nps()
                    proj_block(wab, "wab", 0, 64, t0, w, pi)
                    ACT(grs[0:4, t0:t0 + w], ps[pi][0:4, 0:w], AF.Exp, [f"ps{pi}", "lpt"], ["grs"], bias=lp("dtb", rows=4))
                    ACT(grs[32:36, t0:t0 + w], ps[pi][32:36, 0:w], AF.Sigmoid, [f"ps{pi}"], ["grs"])
                ACT(grs[0:4, :], grs[0:4, :], AF.Ln, ["grs"], ["grs"], bias=1.0)
                TS(grs[0:4, :], grs[0:4, :], sml[0:4, 0:1], None, ALU.mult, None, ["grs", "sml"], ["grs"])
                MSET(grs[0:4, 0:48], 0.0, ["grs"])
                MSET(grs[32:36, 0:48], 0.0, ["grs"])
                P.op("dve", lambda e, grs=grs, grs2=grs2, rstt=rstt: e.tensor_tensor_scan(
                    out=grs2[0:4, :], data0=rstt[0:4, :], data1=grs[0:4, :], initial=0.0, op0=ALU.mult, op1=ALU.add),
                    ["grs", "rstt"], ["grs2"])
                DMA(grow_d[0:4, :], grs2[0:4, :], ["grs2"], ["grow"])
                DMA(grow_d[4:8, :], grs[32:36, :], ["grs"], ["grow"])
                P.barrier()

                psmod[0] = 4
                cv = Carver(arenaH, ARH)
                Gc = cv.get(128, [Tsm], F32)
                Bt = cv.get(128, [Tsm], F32)
                qT_, kT_, vT_, kbe, kbT, kdT = [cv.get(128, [Tsm], BF16) for _ in range(6)]
                BW = NBT * 64
                pqm = cv.get(2, [2, BW], F32)
                BWh = BW // (2 if cfg.PACK else 1)
                Lb = [cv.get(128, [BWh], F32) for _ in range(2)]
                Nb = [cv.get(128, [BWh], F32) for _ in range(2)]
                Dm = [cv.get(128, [BWh], F32) for _ in range(2)]
                Yb = [cv.get(128, [BWh], F32) for _ in range(2)]
                XT = cv.get(128, [BWh], BF16)
                kb_sb = cv.get(128, [NBT // (2 if cfg.PACK else 1), 128], BF16)
                vb_sb = cv.get(128, [NBT // (2 if cfg.PACK else 1), 128], BF16)
                S_f = cv.get(128, [128], F32)
                S_b = cv.get(128, [128], BF16)
                vn = [cv.get(128, [128], BF16) for _ in range(2)]
                E2 = [cv.get(128, [Tsm], F32) for _ in range(2)]
                oT2 = [cv.get(128, [Tsm], F32) for _ in range(2)]
                qeT2 = [cv.get(128, [Tsm], BF16) for _ in range(2)]
                zT2 = [cv.get(128, [Tsm], BF16) for _ in range(2)]
                NH = (NCKm + 1) // 2 if cfg.PACK else NCKm
                qkm2 = [cv.get(128, [NH, 64], BF16) for _ in range(2)]
                u2 = [cv.get(128, [NH, 128], F32) for _ in range(2)]
                wT2 = [cv.get(128, [NCKm * 64], BF16) for _ in range(2)]
                kd2 = [cv.get(128, [NH, 128], BF16) for _ in range(2)]
                pq = cs("pq", 2)
                items = [(h, si) for h in range(4) for si in range(len(segs))]

                HB = NBT // (2 if cfg.PACK else 1)
                BWc = HB * 64

                def place(j):
                    half, jj = divmod(j, HB)
                    return slice(64 * half, 64 * half + 64), slice(jj * 64, (jj + 1) * 64), half, jj

                def setup_gen(k):
                    h, si = items[k]
                    par = k % 2
                    cA, cB = segs[si]
                    nck = cB - cA
                    Ts = nck * 64
                    g0 = cA * 64
                    E, qeT, zT_, qkm, u_sb, wT_sb, kd_sb = E2[par], qeT2[par], zT2[par], qkm2[par], u2[par], wT2[par], kd2[par]
                    kE, kqe, kz, kqk, ku, kw, kkd = f"E{par}", f"qeT{par}", f"zT{par}", f"qkm{par}", f"u{par}", f"wT{par}", f"kd{par}"
                    DMA(qT_[:, 0:Ts], gq_d[h][:, g0:g0 + Ts], [f"gq{h}"], ["qT_"])
                    DMA(kT_[:, 0:Ts], gq_d[4 + h][:, g0:g0 + Ts], [f"gq{4 + h}"], ["kT_"])
                    DMA(vT_[:, 0:Ts], gq_d[8 + h][:, g0:g0 + Ts], [f"gq{8 + h}"], ["vT_"])
                    DMA(zT_[:, 0:Ts], gq_d[12 + h][:, g0:g0 + Ts], [f"gq{12 + h}"], [kz])
                    DMA(Gc[:, 0:Ts], grow_d[h:h + 1, g0:g0 + Ts].partition_broadcast(128), ["grow"], ["Gc"])
                    DMA(Bt[:, 0:Ts], grow_d[4 + h:5 + h, g0:g0 + Ts].partition_broadcast(128), ["grow"], ["Bt"])
                    yield
                    ACT(E[:, 0:Ts], Gc[:, 0:Ts], AF.Exp, ["Gc"], [kE])
                    TT(kbe[:, 0:Ts], kT_[:, 0:Ts], Bt[:, 0:Ts], ALU.mult, ["kT_", "Bt"], ["kbe"])
                    TT(kbT[:, 0:Ts], kbe[:, 0:Ts], E[:, 0:Ts], ALU.mult, ["kbe", kE], ["kbT"])
                    yield
                    TT(vT_[:, 0:Ts], vT_[:, 0:Ts], Bt[:, 0:Ts], ALU.mult, ["vT_", "Bt"], ["vT_"])
                    TT(qeT[:, 0:Ts], qT_[:, 0:Ts], E[:, 0:Ts], ALU.mult, ["qT_", kE], [kqe])
                    Gc3 = Gc[:, 0:Ts].rearrange("p (n c) -> p n c", c=64)
                    TT(Bt[:, 0:Ts].rearrange("p (n c) -> p n c", c=64), Gc3[:, :, 63:64].to_broadcast([128, nck, 64]), Gc3, ALU.subtract,
                       ["Gc", "Bt"], ["Bt"])
                    yield
                    ACT(Bt[:, 0:Ts], Bt[:, 0:Ts], AF.Exp, ["Bt"], ["Bt"])
                    TT(kdT[:, 0:Ts], kT_[:, 0:Ts], Bt[:, 0:Ts], ALU.mult, ["kT_", "Bt"], ["kdT"])
                    yield
                    for b0 in range(0, nck, NBT):
                        nb = min(NBT, nck - b0)
                        assert nb == NBT or nb <= HB, (nb, NBT)
                        rows = 128 if nb > HB else 64
                        ncl = min(nb, HB)
                        Wc = ncl * 64
                        W = nb * 64
                        b0h = b0 // 2 if cfg.PACK else b0
                        bc = slice(b0 * 64, b0 * 64 + W)
                        TS(pqm[:, 0, 0:W], Gc[0:2, bc], pq[:, 0:1], pq[:, 1:2], ALU.mult, ALU.add, ["Gc", "cst"], ["pqm"])
                        TS(pqm[:, 1, 0:W], Gc[0:2, bc], pq[:, 2:3], pq[:, 3:4], ALU.mult, ALU.add, ["Gc", "cst"], ["pqm"])
                        Pm, Qm = pqm[:, 0, :], pqm[:, 1, :]
                        assert not cfg.PACK
                        pgL, pgN, pgQ, pM = nps(), nps(), nps(), nps()
                        for j in range(nb):
                            ch = slice((b0 + j) * 64, (b0 + j + 1) * 64)
                            o = slice(j * 64, (j + 1) * 64)
                            MM(ps[pgL][0:64, o], kbe[:, ch], kT_[:, ch], True, True, ["kbe", "kT_"], [f"ps{pgL}"])
                            MM(ps[pgN][0:64, o], kT_[:, ch], kbe[:, ch], True, True, ["kbe", "kT_"], [f"ps{pgN}"])
                            MM(ps[pgQ][0:64, o], kT_[:, ch], qT_[:, ch], True, True, ["qT_", "kT_"], [f"ps{pgQ}"])
                            MM(ps[pM][0:64, o], Pm[:, o], Qm[:, o], True, True, ["pqm"], [f"ps{pM}"])
                        M3 = ps[pM][0:64, 0:Wc].rearrange("p (n c) -> p n c", c=64)

                        def mk(name):
                            return cs(name, 64).unsqueeze(1).to_broadcast([64, nb, 64])

                        def d3(i_):
                            return Dm[i_][0:64, 0:Wc].rearrange("p (n c) -> p n c", c=64)

                        STT(d3(0), M3, 1.0, mk("mls"), ALU.mult, ALU.add, [f"ps{pM}", "cst"], ["Dm0"])
                        ACT(Dm[0][0:64, 0:Wc], Dm[0][0:64, 0:Wc], AF.Exp, ["Dm0"], ["Dm0"])
                        STT(Lb[0][0:64, 0:Wc], ps[pgL][0:64, 0:Wc], -1.0, Dm[0][0:64, 0:Wc], ALU.mult, ALU.mult, [f"ps{pgL}", "Dm0"], ["Lb0"])
                        yield
                        STT(d3(1), M3, -1.0, mk("mus"), ALU.mult, ALU.add, [f"ps{pM}", "cst"], ["Dm1"])
                        ACT(Dm[1][0:64, 0:Wc], Dm[1][0:64, 0:Wc], AF.Exp, ["Dm1"], ["Dm1"])
                        STT(Nb[0][0:64, 0:Wc], ps[pgN][0:64, 0:Wc], -1.0, Dm[1][0:64, 0:Wc], ALU.mult, ALU.mult, [f"ps{pgN}", "Dm1"], ["Nb0"])
                        yield
                        STT(d3(0), M3, -1.0, mk("mui"), ALU.mult, ALU.add, [f"ps{pM}", "cst", "Dm0"], ["Dm0"])
                        ACT(Dm[0][0:64, 0:Wc], Dm[0][0:64, 0:Wc], AF.Exp, ["Dm0"], ["Dm0"])
                        TT(qkm[0:64, b0h:b0h + ncl, :], ps[pgQ][0:64, 0:Wc].rearrange("p (n c) -> p n c", c=64), d3(0), ALU.mult, [f"ps{pgQ}", "Dm0"], [kqk])
                        yield
                        TT(Yb[0][0:rows, 0:Wc].rearrange("p (n c) -> p n c", c=64), Nb[0][0:rows, 0:Wc].rearrange("p (n c) -> p n c", c=64),
                           cs("i2")[0:rows, :].unsqueeze(1).to_broadcast([rows, ncl, 64]), ALU.add, ["Nb0", "cst"], ["Yb0"])
                        cur = 0
                        for lev in range(1, 6):
                            a_, b_ = (lev - 1) % 2, lev % 2
                            pa, pb, py = nps(), nps(), nps()
                            for j in range(nb):
                                pr, o, _, _ = place(j)
                                MM(ps[pa][pr, o], Nb[a_][pr, o], Lb[a_][pr, o], True, True, [f"Nb{a_}", f"Lb{a_}"], [f"ps{pa}"])
                                if lev < 5:
                                    MM(ps[pb][pr, o], Lb[a_][pr, o], Nb[a_][pr, o], True, True, [f"Nb{a_}", f"Lb{a_}"], [f"ps{pb}"])
                            CP(Lb[b_][0:rows, 0:Wc], ps[pa][0:rows, 0:Wc], [f"ps{pa}"], [f"Lb{b_}"], eng="act")
                            if lev < 5:
                                CP(Nb[b_][0:rows, 0:Wc], ps[pb][0:rows, 0:Wc], [f"ps{pb}"], [f"Nb{b_}"], eng="dve")
                            yield
                            for j in range(nb):
                                pr, o, _, _ = place(j)
                                MM(ps[py][pr, o], Lb[b_][pr, o], Yb[cur][pr, o], True, True, [f"Lb{b_}", f"Yb{cur}"], [f"ps{py}"])
                            if lev < 5:
                                TT(Yb[1 - cur][0:rows, 0:Wc], ps[py][0:rows, 0:Wc], Yb[cur][0:rows, 0:Wc], ALU.add, [f"ps{py}", f"Yb{cur}"], [f"Yb{1 - cur}"])
                                cur = 1 - cur
                            else:
                                TT(XT[0:rows, 0:Wc], ps[py][0:rows, 0:Wc], Yb[cur][0:rows, 0:Wc], ALU.add, [f"ps{py}", f"Yb{cur}"], ["XT"])
                            yield
                        for (srcT, skey, dst, dkey, dsl) in ((kbT, "kbT", kb_sb, "kb_sb", None), (vT_, "vT_", vb_sb, "vb_sb", None),
                                                             (kdT, "kdT", kd_sb, kkd, b0h)):
                            for j in range(nb):
                                ch = slice((b0 + j) * 64, (b0 + j + 1) * 64)
                                pr, o, _, jj = place(j)
                                TR(psb[pr, jj * 128:(jj + 1) * 128], srcT[:, ch], ident_b[:], [skey, "ident_b"], ["psb"])
                            src3 = psb[0:rows, 0:ncl * 128].rearrange("p (n c) -> p n c", c=128)
                            if dsl is None:
                                CP(dst[0:rows, 0:ncl, :], src3, ["psb"], [dkey], eng="act")
                            else:
                                CP(dst[0:rows, b0h:b0h + ncl, :], src3, ["psb"], [dkey], eng="act")
                            yield
                        for q0_ in range(0, ncl, 4):
                            n4 = min(4, ncl - q0_)
                            pi = nps()
                            for j in range(nb):
                                pr, o, _, jj = place(j)
                                if q0_ <= jj < q0_ + n4:
                                    MM(ps[pi][pr, (jj - q0_) * 128:(jj - q0_ + 1) * 128], XT[pr, o], vb_sb[pr, jj, :], True, True, ["XT", "vb_sb"], [f"ps{pi}"])
                            CP(u_sb[0:rows, b0h + q0_:b0h + q0_ + n4, :], ps[pi][0:rows, 0:n4 * 128].rearrange("p (n c) -> p n c", c=128), [f"ps{pi}"], [ku])
                        pi = nps()
                        for j in range(nb):
                            pr, o, _, jj = place(j)
                            MM(ps[pi][:, j * 64:(j + 1) * 64], kb_sb[pr, jj, :], XT[pr, o], True, True, ["XT", "kb_sb"], [f"ps{pi}"])
                        CP(wT_sb[:, b0 * 64:b0 * 64 + W], ps[pi][:, 0:W], [f"ps{pi}"], [kw], eng="act")
                        yield

                def scan_gen(k):
                    h, si = items[k]
                    par = k % 2
                    cA, cB = segs[si]
                    nck = cB - cA
                    E, oT, qeT, qkm, u_sb, wT_sb, kd_sb = E2[par], oT2[par], qeT2[par], qkm2[par], u2[par], wT2[par], kd2[par]
                    kE, ko, kqe, kqk, ku, kw, kkd = f"E{par}", f"oT{par}", f"qeT{par}", f"qkm{par}", f"u{par}", f"wT{par}", f"kd{par}"
                    if si == 0:
                        MSET(S_f[:], 0.0, ["S_f"])
                        MSET(S_b[:], 0.0, ["S_b"])
                    for n in range(nck):
                        ch = slice(n * 64, (n + 1) * 64)
                        b0 = (n // NBT) * NBT
                        pr, _, _, jj = place(n - b0)
                        idx = (b0 // 2 if cfg.PACK else b0) + jj
                        pv, po, pss = 4, 5, 6
                        vnb = vn[n % 2]
                        vk = f"vn{n % 2}"
                        MM(ps[pv][pr, 0:128], wT_sb[:, ch], S_b[:], True, True, [kw, "S_b"], [f"ps{pv}"])
                        TT(vnb[pr, :], u_sb[pr, idx, :], ps[pv][pr, 0:128], ALU.subtract, [ku, f"ps{pv}"], [vk])
                        MM(ps[pss][:, 0:128], kd_sb[pr, idx, :], vnb[pr, :], True, True, [kkd, vk], [f"ps{pss}"])
                        MM(ps[po][:, 0:64], S_b[:], qeT[:, ch], True, False, ["S_b", kqe], [f"ps{po}"])
                        MM(ps[po][:, 0:64], vnb[pr, :], qkm[pr, idx, :], False, True, [vk, kqk], [f"ps{po}"])
                        STT(S_b[:], S_f[:], E[:, n * 64 + 63:n * 64 + 64], ps[pss][:, 0:128], ALU.mult, ALU.add, ["S_f", kE, f"ps{pss}"], ["S_b"])
                        STT(S_f[:], S_f[:], E[:, n * 64 + 63:n * 64 + 64], ps[pss][:, 0:128], ALU.mult, ALU.add, ["S_f", kE, f"ps{pss}"], ["S_f"])
                        CP(oT[:, ch], ps[po][:, 0:64], [f"ps{po}"], [ko], eng="act")
                        yield

                def post_norm(k):
                    h, si = items[k]
                    par = k % 2
                    cA, cB = segs[si]
                    Ts = (cB - cA) * 64
                    g0 = cA * 64
                    oT, zT_ = oT2[par], zT2[par]
                    ko, kz = f"oT{par}", f"zT{par}"
                    ACT(kbe[:, 0:Ts], oT[:, 0:Ts], AF.Square, [ko], ["kbe"])
                    for bi, t0 in enumerate(range(0, Ts, 512)):
                        w = min(512, Ts - t0)
                        pi = nps()
                        MM(ps[pi][:, 0:w], ones_b[:], kbe[:, t0:t0 + w], True, True, ["ones_b", "kbe"], [f"ps{pi}"])
                        rsb = rs_f[bi % 2]
                        rstd_from_ps(pi, w, rsb, f"rs_f{bi % 2}", 1.0 / 128)
                        tf = t_f[bi % 2]
                        STT(tf[:, 0:w], oT[:, t0:t0 + w], lp("aon"), rsb[:, 0:w], ALU.mult, ALU.mult, [ko, f"rs_f{bi % 2}", "lpt"], [f"t_f{bi % 2}"])
                        TT(oab[:, h, g0 + t0:g0 + t0 + w], tf[:, 0:w], zT_[:, t0:t0 + w], ALU.mult, [f"t_f{bi % 2}", kz], ["oab"])

                for _ in setup_gen(0):
                    pass
                for k in range(len(items)):
                    gs = scan_gen(k)
                    gn = setup_gen(k + 1) if k + 1 < len(items) else iter(())
                    alive_s, alive_n = True, True
                    while alive_s or alive_n:
                        for _ in range(cfg.RATIO):
                            if alive_n:
                                try:
                                    next(gn)
                                except StopIteration:
                                    alive_n = False
                        if alive_s:
                            try:
                                next(gs)
                            except StopIteration:
                                alive_s = False
                    post_norm(k)
                psmod[0] = 5
                P.barrier()

                cv = Carver(arenaH, ARH)
                qT4 = [cv.get(128, [Tp], BF16) for _ in range(4)]
                qk4 = ["qT4_0", "qT4_1", "qT4_2", "qT4_3"]
                iq4 = [cv.get(128, [Tp], BF16) for _ in range(4)]
                ik4 = ["iq4_0", "iq4_1", "iq4_2", "iq4_3"]
                kTd = cv.get(128, [Tp], BF16)
                ikT = cv.get(128, [Tp], BF16)
                v_tm = cv.get(128, [NQT, 128], BF16)
                v_me = cv.get(16, [128], BF16)
                iw_tm = cv.get(128, [NQT, 8], F32)
                sc = cv.get(128, [S], F32)
                nm = cv.get(128, [S], BF16)

                def rope_tile(wv, wkey, col0, dst, dkey, gain, ones_m, ones_key, inv_n, rot, rotkey, cname, sname):
                    for bi, (t0, w) in enumerate(blocks):
                        rt = rtab[bi % 2]
                        rk = f"rtab{bi % 2}"
                        DMA(rt[:, 0, 0:w], tab_d(cname)[:, t0:t0 + w], (), [rk])
                        DMA(rt[:, 1, 0:w], tab_d(sname)[:, t0:t0 + w], (), [rk])
                        pi = nps()
                        proj_block(wv, wkey, col0, 128, t0, w, pi)
                        tb = t_b[bi % 2]
                        tkey = f"t_b{bi % 2}"
                        if gain is not None:
                            ACT(sq_b[:, 0, 0:w], ps[pi][:, 0:w], AF.Square, [f"ps{pi}"], ["sq_b"])
                            p2 = nps()
                            MM(ps[p2][:, 0:w], ones_m[:], sq_b[:, 0, 0:w], True, True, [ones_key, "sq_b"], [f"ps{p2}"])
                            rsb = rs_f[bi % 2]
                            rstd_from_ps(p2, w, rsb, f"rs_f{bi % 2}", inv_n)
                            STT(tb[:, 0:w], ps[pi][:, 0:w], gain, rsb[:, 0:w], ALU.mult, ALU.mult, [f"ps{pi}", f"rs_f{bi % 2}", "lpt"], [tkey])
                        else:
                            CP(tb[:, 0:w], ps[pi][:, 0:w], [f"ps{pi}"], [tkey], eng="act")
                        p3 = nps()
                        MM(ps[p3][:, 0:w], rot[:], tb[:, 0:w], True, True, [rotkey, tkey], [f"ps{p3}"])
                        tf = t_f[bi % 2]
                        TT(tf[:, 0:w], ps[p3][:, 0:w], rt[:, 1, 0:w], ALU.mult, [f"ps{p3}", rk], [f"t_f{bi % 2}"])
                        rsb2 = rs_f[bi % 2]
                        TT(rsb2[:, 0:w], tb[:, 0:w], rt[:, 0, 0:w], ALU.mult, [tkey, rk], [f"rs_f{bi % 2}"])
                        TT(dst[:, t0:t0 + w], rsb2[:, 0:w], tf[:, 0:w], ALU.add, [f"rs_f{bi % 2}", f"t_f{bi % 2}"], [dkey])

                wv, wkey = wload(wk(w_in_d, l, 2056, 512), [128, KC, 512])
                for hh in range(4):
                    rope_tile(wv, wkey, hh * 128, qT4[hh], qk4[hh], lp("qn"), ones_b, "ones_b", 1.0 / 128, r128_b, "r128_b", "cosA", "sinA")
                wv, wkey = wload(wk(w_in_d, l, 2568, 256), [128, KC, 256])
                rope_tile(wv, wkey, 0, kTd, "kTd", lp("kn"), ones_b, "ones_b", 1.0 / 128, r128_b, "r128_b", "cosA", "sinA")
                for kt in range(-1, NQT):
                    c0, rows = (48, 16) if kt < 0 else (64 + 128 * kt, 128)
                    pi = nps()
                    for kc in range(KC):
                        MM(ps[pi][0:rows, 0:128], nT[:, kc, c0:c0 + rows], wv[:, kc, 128:256], kc == 0, kc == KC - 1, ["nT", wkey], [f"ps{pi}"])
                    if kt < 0:
                        CP(v_me[:, :], ps[pi][0:16, 0:128], [f"ps{pi}"], ["v_me"], eng="act")
                    else:
                        CP(v_tm[:, kt, :], ps[pi][:, 0:128], [f"ps{pi}"], ["v_tm"], eng="act")
                wv, wkey = wload(wk(w_in_d, l, 2824, 512), [128, KC, 512])
                for tl in range(4):
                    rope_tile(wv, wkey, tl * 128, iq4[tl], ik4[tl], None, None, None, None, r64_b, "r64_b", "cosI", "sinI")
                wv, wkey = wnext(KC * 136, KC)
                DMA(wv[:, :, 0:64], wi[:, :, 3336:3400], (), [wkey], eng="pool")
                DMA(wv[:, :, 64:128], wi[:, :, 3336:3400], (), [wkey], eng="pool")
                DMA(wv[:, :, 128:136], wi[:, :, 3400:3408], (), [wkey], eng="pool")
                rope_tile(wv, wkey, 0, ikT, "ikT", lp("kin"), blk64_b, "blk64_b", 1.0 / 64, r64_b, "r64_b", "cosI", "sinI")
                for qt in range(NQT):
                    c0 = 64 + 128 * qt
                    pi = nps()
                    for kc in range(KC):
                        MM(ps[pi][:, 0:8], nT[:, kc, c0:c0 + 128], wv[:, kc, 128:136], kc == 0, kc == KC - 1, ["nT", wkey], [f"ps{pi}"])
                    TS(iw_tm[:, qt, :], ps[pi][:, 0:8], (8.0 ** -0.5) * (64.0 ** -0.5), None, ALU.mult, None, [f"ps{pi}"], ["iw_tm"])

                att_scale = 128.0 ** -0.5
                sc2 = cv.get(128, [S], F32)
                scs = [sc, sc2]
                junk = sq_b[:, :, :].rearrange("p a b -> p (a b)")

                def S1(qt):
                    q0 = 64 + 128 * qt
                    Sr = 128 * (qt + 1)
                    scb, sk = scs[qt % 2], f"sc{qt % 2}"
                    for kb0 in range(0, Sr, 512):
                        w = min(512, Sr - kb0)
                        for ih in range(8):
                            tl, hf = divmod(ih, 2)
                            pr = slice(64 * hf, 64 * hf + 64)
                            pi = nps()
                            MM(ps[pi][:, 0:w], iq4[tl][pr, q0:q0 + 128], ikT[pr, 64 + kb0:64 + kb0 + w], True, True,
                               [ik4[tl], "ikT"], [f"ps{pi}"])
                            tf = t_f[ih % 2]
                            ACT(tf[:, 0:w], ps[pi][:, 0:w], AF.Relu, [f"ps{pi}"], [f"t_f{ih % 2}"])
                            if ih == 0:
                                TS(scb[:, kb0:kb0 + w], tf[:, 0:w], iw_tm[:, qt, 0:1], None, ALU.mult, None, [f"t_f{ih % 2}", "iw_tm"], [sk])
                            else:
                                STT(scb[:, kb0:kb0 + w], tf[:, 0:w], iw_tm[:, qt, ih:ih + 1], scb[:, kb0:kb0 + w], ALU.mult, ALU.add,
                                    [f"t_f{ih % 2}", "iw_tm", sk], [sk])

                junkA = rtab_all[:, :, :].rearrange("p a b -> p (a b)").bitcast(BF16)
                nmA = junkA[:, 2048:4096]
                nm_neg = {}

                def S2(qt):
                    Sr = 128 * (qt + 1)
                    scb, sk = scs[qt % 2], f"sc{qt % 2}"
                    NIT = cfg.NIT
                    on_act = (qt % 2 == 1) and Sr <= 2048
                    B, bk, DF, dk = (bisA, "bisA", dfsA, "dfsA") if on_act else (bis, "bis", dfs, "dfs")
                    if Sr > TOPK:
                        P.op("dve", lambda e: e.tensor_reduce(out=B[:, 0:1], in_=scb[:, 0:Sr], axis=mybir.AxisListType.X, op=ALU.min), [sk], [bk])
                        P.op("dve", lambda e: e.tensor_reduce(out=B[:, 1:2], in_=scb[:, 0:Sr], axis=mybir.AxisListType.X, op=ALU.max), [sk], [bk])
                        TT(B[:, 2:3], B[:, 1:2], B[:, 0:1], ALU.subtract, [bk], [bk])
                        if on_act:
                            TS(DF[:, 0:NIT + 1], cs("frow")[:, 0:NIT + 1], B[:, 2:3], 0.5, ALU.mult, ALU.mult, [bk, "cst"], [dk])
                            STT(B[:, 3:4], DF[:, 0:1], 2.0, B[:, 0:1], ALU.mult, ALU.add, [bk, dk], [bk])
                        else:
                            TS(DF[:, 0:NIT + 1], cs("frow")[:, 0:NIT + 1], B[:, 2:3], None, ALU.mult, None, [bk, "cst"], [dk])
                            TT(B[:, 3:4], B[:, 0:1], DF[:, 0:1], ALU.add, [bk, dk], [bk])
                    MSET(scb[0:64, Sr - 64:Sr], -1e30, [sk])
                    if Sr > TOPK:
                        for it in range(NIT):
                            if on_act:
                                ACT(junkA[:, 0:Sr], scb[:, 0:Sr], AF.Sign, [sk, bk], ["junkA", bk], bias=B[:, 3:4], scale=-1.0, accum=B[:, 4:5])
                                ACT(B[:, 5:6], B[:, 4:5], AF.Sign, [bk], [bk], bias=float(Sr - 2 * TOPK + 0.5), scale=-1.0)
                                ACT(B[:, 3:4], B[:, 5:6], AF.Identity, [bk, dk], [bk], bias=B[:, 3:4], scale=DF[:, it:it + 1])
                            else:
                                TS(junk[:, 0:Sr], scb[:, 0:Sr], B[:, 3:4], None, ALU.is_ge, ALU.add, [sk, bk], ["sq_b", bk], accum=B[:, 4:5])
                                TS(B[:, 5:6], B[:, 4:5], TOPK - 0.5, 0.5, ALU.is_ge, ALU.subtract, [bk], [bk])
                                STT(B[:, 3:4], B[:, 5:6], DF[:, it:it + 1], B[:, 3:4], ALU.mult, ALU.add, [bk, dk], [bk])
                        if on_act:
                            ACT(B[:, 6:7], B[:, 3:4], AF.Identity, [bk], [bk], scale=-1.0)
                            ACT(B[:, 0:1], DF[:, NIT:NIT + 1], AF.Identity, [bk, dk], [bk], bias=B[:, 6:7], scale=2.0)
                            if Sr - 64 <= TOPK:
                                MSET(B[0:64, 0:1], 1e29, [bk], eng="pool")
                        else:
                            TT(B[:, 0:1], B[:, 3:4], DF[:, NIT:NIT + 1], ALU.subtract, [bk, dk], [bk])
                            if Sr - 64 <= TOPK:
                                MSET(B[0:64, 0:1], -1e29, [bk])
                    else:
                        on_act = False
                        MSET(B[:, 0:1], -1e29, [bk])
                    if on_act:
                        ACT(junkA[:, 0:Sr], scb[:, 0:Sr], AF.Sign, [sk, bk], ["junkA"], bias=B[:, 0:1])
                        ACT(nmA[:, 0:Sr], junkA[:, 0:Sr], AF.Relu, ["junkA"], ["nmA"], scale=NEGB)
                    elif qt % 2 == 1:
                        TS(nmA[:, 0:Sr], scb[:, 0:Sr], B[:, 0:1], NEGB, ALU.is_lt, ALU.mult, [sk, bk], ["nmA"])
                    else:
                        TS(nm[:, 0:Sr], scb[:, 0:Sr], B[:, 0:1], NEGB, ALU.is_lt, ALU.mult, [sk, bk], ["nm"])
                    nm_neg[qt] = on_act

                def S3(qt):
                    if qt < 0:
                        q0, nq, kts = 48, 16, []
                    else:
                        q0, nq, kts = 64 + 128 * qt, 128, list(range(qt + 1))
                    NQ = 4 * nq
                    keysets = [(-1, 16)] + [(kt, 128) for kt in kts]
                    for ki, (kt, nk) in enumerate(keysets):
                        pi = nps()
                        first, last = ki == 0, ki == len(keysets) - 1
                        kc0 = 48 if kt < 0 else 64 + 128 * kt
                        for hh in range(4):
                            MM(ps[pi][0:nk, hh * nq:(hh + 1) * nq], kTd[:, kc0:kc0 + nk], qT4[hh][:, q0:q0 + nq], True, kt < 0,
                               ["kTd", qk4[hh]], [f"ps{pi}"])
                            if kt >= 0:
                                i4x, i4k = (i4n_b, "i4n_b") if nm_neg.get(qt) else (i4_b, "i4_b")
                                nmx, nmk = (nmA, "nmA") if qt % 2 == 1 else (nm, "nm")
                                MM(ps[pi][0:nk, hh * nq:(hh + 1) * nq], nmx[:, 128 * kt:128 * kt + 128], i4x[:, hh * 128:(hh + 1) * 128], False, True,
                                   [nmk, i4k], [f"ps{pi}"])
                        tb = t_b[ki % 2]
                        tkey = f"t_b{ki % 2}"
                        ACT(tb[0:nk, 0:NQ], ps[pi][0:nk, 0:NQ], AF.Exp, [f"ps{pi}"], [tkey], scale=att_scale)
                        vsrc = v_me[:, :] if kt < 0 else v_tm[:, kt, :]
                        vkey = "v_me" if kt < 0 else "v_tm"
                        MM(ps[5][:, 0:NQ], vsrc, tb[0:nk, 0:NQ], first, last, [vkey, tkey], ["ps5"])
                        MM(ps[6][:, 0:NQ], ones_b[0:nk, :], tb[0:nk, 0:NQ], first, last, ["ones_b", tkey], ["ps6"])
                    rz = rs_f[0]
                    P.op("dve", lambda e: e.reciprocal(out=rz[:, 0:NQ], in_=ps[6][:, 0:NQ]), ["ps6"], ["rs_f0"])
                    TT(oab[:, 4:8, q0:q0 + nq], ps[5][:, 0:NQ].rearrange("p (h q) -> p h q", h=4), rz[:, 0:NQ].rearrange("p (h q) -> p h q", h=4),
                       ALU.mult, ["ps5", "rs_f0"], ["oab"])

                S3(-1)
                pairs = [(a_, a_ + 1 if a_ + 1 < NQT else None) for a_ in range(0, NQT, 2)]

                def s1pair(pp):
                    for t_ in pp:
                        if t_ is not None:
                            S1(t_)

                def s2pair(pp):
                    if pp[1] is not None:
                        S2(pp[1])
                    S2(pp[0])

                s1pair(pairs[0])
                s2pair(pairs[0])
                for pi_ in range(len(pairs)):
                    if pi_ + 1 < len(pairs):
                        s1pair(pairs[pi_ + 1])
                    for t_ in pairs[pi_]:
                        if t_ is not None:
                            S3(t_)
                    if pi_ + 1 < len(pairs):
                        s2pair(pairs[pi_ + 1])
                P.barrier()

                psmod[0] = 7
                cv = Carver(arenaH, ARH, HWD // 2)
                yT = [cv.get(128, [Tp], BF16) for _ in range(KC)]
                ykeys = [f"yT{i}" for i in range(KC)]
                wg_ = w_gate_d[l].rearrange("(kc p) n -> p kc n", p=128)
                for f in range(KC):
                    wv, wkey = wnext(KC * 256, KC)
                    DMA(wv[:, :, 0:128], wg_[:, :, f * 128:(f + 1) * 128], (), [wkey], eng="pool")
                    DMA(wv[:, :, 128:256], wg_[:, :, D + f * 128:D + (f + 1) * 128], (), [wkey], eng="pool")
                    wb, wbkey = wnext(8 * 128, 8)
                    DMA(wb[:, 0:4, :], w_ba_d[l].rearrange("(kc p) n -> p kc n", p=128)[:, :, f * 128:(f + 1) * 128], (), [wbkey], eng="pool")
                    DMA(wb[:, 4:8, :], w_bb_d[l].rearrange("(kc p) n -> p kc n", p=128)[:, :, f * 128:(f + 1) * 128], (), [wbkey], eng="pool")
                    for bi, (t0, w) in enumerate(blocks):
                        pga, pgb, pya, pyb = nps(), nps(), nps(), nps()
                        proj_block(wv, wkey, 0, 128, t0, w, pga)
                        proj_block(wv, wkey, 128, 128, t0, w, pgb)
                        proj_block(wb, wbkey, 0, 128, t0, w, pya, kcs=4, src=oab, srckey="oab", srcoff=0)
                        proj_block(wb[:, 4:8, :], wbkey, 0, 128, t0, w, pyb, kcs=4, src=oab, srckey="oab", srcoff=4)
                        ACT(t_f[0][:, 0:w], ps[pga][:, 0:w], AF.Sigmoid, [f"ps{pga}", "lpt"], ["t_f0"], bias=lp("bgate", f))
                        ACT(t_f[1][:, 0:w], ps[pgb][:, 0:w], AF.Sigmoid, [f"ps{pgb}", "lpt"], ["t_f1"], bias=lp("bgate", KC + f))
                        TT(t_f[0][:, 0:w], t_f[0][:, 0:w], ps[pya][:, 0:w], ALU.mult, ["t_f0", f"ps{pya}"], ["t_f0"])
                        TT(t_f[1][:, 0:w], t_f[1][:, 0:w], ps[pyb][:, 0:w], ALU.mult, ["t_f1", f"ps{pyb}"], ["t_f1"])
                        TT(yT[f][:, t0:t0 + w], t_f[0][:, 0:w], t_f[1][:, 0:w], ALU.add, ["t_f0", "t_f1"], [ykeys[f]])
                P.barrier()
                KH = KC // 2
                hlo = arenaH[:, 0:HWD // 2].rearrange("p (k t) -> p k t", k=KH)
                hhi = arenaN[:, :].rearrange("p (k t) -> p k t", k=KC - KH)
                DMA(arenaH[:, 0:HWD // 2], hsp_d[:, 0:HWD // 2], ["hsp"], ["hlo"])
                DMA(arenaN[:, :], hsp_d[:, HWD // 2:HWD], ["hsp"], ["hhi"])
                for f in range(KC):
                    wv, wkey = wload(wk(w_out_d, l, f * 128, 128), [128, KC, 128])
                    dst, dkey = (hlo[:, f, :], "hlo") if f < KH else (hhi[:, f - KH, :], "hhi")
                    for (t0, w) in blocks:
                        pi = nps()
                        for kc in range(KC):
                            MM(ps[pi][:, 0:w], wv[:, kc, :], yT[kc][:, t0:t0 + w], kc == 0, kc == KC - 1, [wkey, ykeys[kc]], [f"ps{pi}"])
                        TT(dst[:, t0:t0 + w], dst[:, t0:t0 + w], ps[pi][:, 0:w], ALU.add, [dkey, f"ps{pi}"], [dkey])
                P.barrier()
                for f in range(KH, KC):
                    CP(hT[:, f, :], hhi[:, f - KH, :], ["hhi"], ["hT"], eng=("act" if f % 2 else "dve"))
                P.barrier()
                rmsnorm_to_nT("gffn")
                P.barrier()
                cv = Carver(arenaO, ARO)
                f_a = cv.get(128, [Tp + 8], F32)
                f_b = cv.get(128, [Tp], F32)
                b_e = cv.get(128, [Tp], BF16)
                b_f = cv.get(128, [Tp], BF16)
                actb = []
                for i_ in range(GF):
                    if i_ == 2 and 4096 >= Tp:
                        actb.append(rtab_all[:, :, :].rearrange("p a b -> p (a b)").bitcast(BF16)[:, 0:Tp])
                    elif i_ == 3 and KC * 512 >= Tp:
                        actb.append(sq_b[:, :, :].rearrange("p a b -> p (a b)")[:, 0:Tp])
                    else:
                        actb.append(cv.get(128, [Tp], BF16))
                akeys = [f"actb{i}" for i in range(GF)]
                MSET(f_a[:, 0:2], 0.0, ["f_a0"])
                wu_ = w_up_d[l].rearrange("(kc p) n -> p kc n", p=128)
                for g0 in range(0, NF, GF):
                    ng = min(GF, NF - g0)
                    for jj in range(ng):
                        j = g0 + jj
                        wv, wkey = wnext(KC * 256, KC)
                        DMA(wv[:, :, 0:128], wu_[:, :, j * 128:(j + 1) * 128], (), [wkey], eng="pool")
                        DMA(wv[:, :, 128:256], wu_[:, :, cfg.DFF + j * 128:cfg.DFF + (j + 1) * 128], (), [wkey], eng="pool")
                        cw = lambda i, j=j: lp("convf", j * 3 + i)
                        for bi, (t0, w) in enumerate(blocks):
                            pg, pu = nps(), nps()
                            proj_block(wv, wkey, 0, 128, t0, w, pg)
                            proj_block(wv, wkey, 128, 128, t0, w, pu)
                            CP(f_a[:, 2 + t0:2 + t0 + w], ps[pg][:, 0:w], [f"ps{pg}"], [f"f_a{bi}"], eng="act")
                            CP(b_e[:, t0:t0 + w], ps[pu][:, 0:w], [f"ps{pu}"], [f"b_e{bi}"], eng="act")
                            rk = [f"f_a{bi}"] + ([f"f_a{bi - 1}"] if bi else [])
                            fk = f"f_b{bi}"
                            TS(f_b[:, t0:t0 + w], f_a[:, 2 + t0:2 + t0 + w], cw(2), None, ALU.mult, None, rk + ["lpt"], [fk])
                            for i in (1, 0):
                                STT(f_b[:, t0:t0 + w], f_a[:, i + t0:i + t0 + w], cw(i), f_b[:, t0:t0 + w], ALU.mult, ALU.add, rk + [fk, "lpt"], [fk])
                            ACT(b_f[:, t0:t0 + w], f_b[:, t0:t0 + w], AF.Silu, [fk], [f"b_f{bi}"])
                            TT(actb[jj][:, t0:t0 + w], b_f[:, t0:t0 + w], b_e[:, t0:t0 + w], ALU.mult, [f"b_f{bi}", f"b_e{bi}"], [f"{akeys[jj]}_{bi}"])
                    for f in range(KC):
                        wv, wkey = wload(w_down_d[l].rearrange("(kc p) n -> p kc n", p=128)[:, g0:g0 + ng, f * 128:(f + 1) * 128], [128, ng, 128])
                        for bi, (t0, w) in enumerate(blocks):
                            pi = nps()
                            for jj in range(ng):
                                MM(ps[pi][:, 0:w], wv[:, jj, :], actb[jj][:, t0:t0 + w], jj == 0, jj == ng - 1, [wkey, f"{akeys[jj]}_{bi}"], [f"ps{pi}"])
                            TT(hT[:, f, t0:t0 + w], hT[:, f, t0:t0 + w], ps[pi][:, 0:w], ALU.add, [f"hT{f}_{bi}", f"ps{pi}"], [f"hT{f}_{bi}"])
                P.barrier()

            P.barrier()
            xin = Carver(arenaO, ARO).get(128, [D], F32)
            for r in range(NQT):
                c0 = 64 + 128 * r
                for kc in range(KC):
                    pi = nps()
                    TR(ps[pi][:, 0:128], hT[:, kc, c0:c0 + 128], identF[:, :], ["hT", "cst"], [f"ps{pi}"])
                    CP(xin[:, kc * 128:(kc + 1) * 128], ps[pi][:, 0:128], [f"ps{pi}"], ["xin"], eng=("act" if kc % 2 else "dve"))
                final.append(DMA(y_d[sq, 128 * r:128 * (r + 1), :], xin[:, :], ["xin"], ["y"]))
        P.emit(final)
    return nc


_CACHE = {}


def make_in_maps(cfg, ncores, x, meta_tokens, norm_mix, w_in, conv_a, a_log, dt_bias, a_out_norm, q_norm, k_norm,
                 kidx_norm, w_branch_a, w_branch_b, w_gate, b_gate, w_out, norm_ffn, w_up, conv_ffn, w_down):
    f = lambda a: np.ascontiguousarray(np.asarray(a, dtype=np.float32))
    consts = host_consts(cfg)
    lps = np.stack([host_lparams(cfg, l, f(norm_mix), f(norm_ffn), f(conv_a), f(conv_ffn), f(b_gate), f(a_out_norm), f(q_norm),
                                 f(k_norm), f(kidx_norm), f(a_log), f(dt_bias)) for l in range(cfg.DEPTH)])
    x = f(x)
    shared = {"meta": f(meta_tokens), "consts": consts, "lparams": lps, "w_in": f(w_in), "w_branch_a": f(w_branch_a),
              "w_branch_b": f(w_branch_b), "w_gate": f(w_gate), "w_out": f(w_out), "w_up": f(w_up), "w_down": f(w_down)}
    maps = []
    for c in range(ncores):
        m = dict(shared)
        m["x"] = np.ascontiguousarray(x[c * cfg.NSEQ:(c + 1) * cfg.NSEQ])
        maps.append(m)
    return maps


def kernel(**inputs):
    cfg = Cfg()
    ncores = 8
    if "nc" not in _CACHE:
        _CACHE["nc"] = build(cfg)
    nc = _CACHE["nc"]
    maps = make_in_maps(cfg, ncores, **inputs)
    res = run_bass_kernel_spmd(nc, maps, core_ids=list(range(ncores)))
    return np.concatenate([np.asarray(r["y"]) for r in res.results], axis=0).astype(np.float32)
```

```python
import contextlib
import math
import numpy as np
import concourse.bass as bass
import concourse.mybir as mybir
from concourse.bass_utils import run_bass_kernel_spmd

F32 = mybir.dt.float32
BF16 = mybir.dt.bfloat16
ALU = mybir.AluOpType
AF = mybir.ActivationFunctionType

ENGS = ("pe", "act", "dve", "pool", "sp")
DMA_POOL = 16
EPOCH = 12000
EPS = 1e-6
NEGB = -30000.0


class Op:
    __slots__ = ("eng", "idx", "fn", "dma", "deps", "signal", "sem", "val")

    def __init__(self, eng, idx, fn, dma):
        self.eng, self.idx, self.fn, self.dma = eng, idx, fn, dma
        self.deps = ()
        self.signal = False
        self.sem = None
        self.val = 0


class Prog:
    def __init__(self, nc):
        self.nc = nc
        self.ops = {e: [] for e in ENGS}
        self.lastw = {}
        self.readers = {}
        self.fence = {}
        self.dma_since = []

    def barrier(self):
        f = list(self.dma_since)
        for e in ENGS:
            for o in reversed(self.ops[e]):
                if not o.dma:
                    f.append(o)
                    break
        self.dma_since = []
        for e in ENGS:
            self.fence[e] = list(self.fence.get(e, [])) + f

    def op(self, eng, fn, reads=(), writes=(), dma=False):
        o = Op(eng, len(self.ops[eng]), fn, dma)
        deps = {}
        if dma:
            self.dma_since.append(o)
        if self.fence.get(eng):
            for d in self.fence[eng]:
                if d.eng == eng and not d.dma and eng == "pe":
                    continue
                deps[(d.eng, d.idx)] = d
            self.fence[eng] = []

        def add(d, raw):
            if d is None:
                return
            if d.eng == eng and not d.dma:
                if eng == "pe":
                    return
            deps[(d.eng, d.idx)] = d

        for k in reads:
            for wr in self.lastw.get(k, ()):
                add(wr, True)
        for k in writes:
            rd = self.readers.get(k, {})
            prev = self.lastw.get(k, ())
            group = dma and prev and all(p.dma for p in prev) and not rd
            if not group:
                for wr in prev:
                    add(wr, False)
            for r in rd.values():
                if isinstance(r, list):
                    for r_ in r:
                        add(r_, None)
                else:
                    add(r, None)
        o.deps = tuple(deps.values())
        for d in o.deps:
            d.signal = True
        for k in reads:
            rd = self.readers.setdefault(k, {})
            if dma:
                rd.setdefault("dma", []).append(o)
            else:
                rd[eng] = o
        for k in writes:
            prev = self.lastw.get(k, ())
            if dma and prev and all(p.dma for p in prev) and not self.readers.get(k):
                self.lastw[k] = tuple(prev) + (o,)
            else:
                self.lastw[k] = (o,)
            self.readers[k] = {}
        self.ops[eng].append(o)
        return o

    def emit(self, final_ops=()):
        nc = self.nc
        for o in final_ops:
            o.signal = True
        n_sems = {}
        for e in ENGS:
            cnt = 0
            for o in self.ops[e]:
                if o.signal and not o.dma:
                    o.sem = (e, cnt // EPOCH)
                    o.val = cnt % EPOCH + 1
                    cnt += 1
            n_sems[e] = (cnt + EPOCH - 1) // EPOCH
        dma_sems = []
        for e in ENGS:
            dcnt = 0
            dvals = [0] * DMA_POOL
            for o in self.ops[e]:
                if o.signal and o.dma:
                    j = dcnt % DMA_POOL
                    dcnt += 1
                    dvals[j] += 16
                    o.sem = ("dma" + e, j)
                    o.val = dvals[j]
            dma_sems += [("dma" + e, j) for j in range(min(DMA_POOL, dcnt))]
        with contextlib.ExitStack() as st:
            sems = {}
            for e in ENGS:
                for ep in range(n_sems[e]):
                    sems[(e, ep)] = st.enter_context(nc.semaphore(f"s_{e}_{ep}"))
            for k in dma_sems:
                sems[k] = st.enter_context(nc.semaphore(f"s_{k[0]}_{k[1]}"))
            block = st.enter_context(nc.Block())

            def run(e, eng):
                waited = {}
                for o in self.ops[e]:
                    need = {}
                    for d in o.deps:
                        if d.val > need.get(d.sem, 0):
                            need[d.sem] = d.val
                    for sm, v in need.items():
                        if waited.get(sm, 0) >= v:
                            continue
                        eng.wait_ge(sems[sm], v)
                        waited[sm] = v
                    if o.signal and o.dma and o.val > 16 and waited.get(o.sem, 0) < o.val - 16:
                        eng.wait_ge(sems[o.sem], o.val - 16)
                        waited[o.sem] = o.val - 16
                    ins = o.fn(eng)
                    if o.signal:
                        ins.then_inc(sems[o.sem], 16 if o.dma else 1)
                if e == "sp":
                    for d in final_ops:
                        if waited.get(d.sem, 0) >= d.val:
                            continue
                        eng.wait_ge(sems[d.sem], d.val)
                        waited[d.sem] = d.val

            @block.sync
            def _(eng):
                run("sp", eng)

            @block.tensor
            def _(eng):
                run("pe", eng)

            @block.scalar
            def _(eng):
                run("act", eng)

            @block.vector
            def _(eng):
                run("dve", eng)

            @block.gpsimd
            def _(eng):
                run("pool", eng)


class Cfg:
    def __init__(self, D=1024, NCH=32, DFF=2816, NSEQ=4, DEPTH=2, TOPK=256, NIT=16, NSEG=0, NBT=8, GF=4, NWB=3, ARENA_MIN=0, PACK=0, POOLCONV=0, RATIO=2):
        self.D, self.NCH, self.DFF, self.NSEQ, self.DEPTH, self.TOPK, self.NIT = D, NCH, DFF, NSEQ, DEPTH, TOPK, NIT
        self.NSEG, self.NBT, self.GF, self.NWB, self.ARENA_MIN, self.PACK = NSEG, NBT, GF, NWB, ARENA_MIN, PACK
        self.POOLCONV = POOLCONV
        self.RATIO = RATIO
        self.KC = D // 128
        self.S = 64 * NCH
        self.Tp = 64 * (NCH + 1)
        self.NF = DFF // 128
        self.INC = 3408
        o = 0
        self.c = {}
        for name, w in (("ident", 128), ("r128", 128), ("r64", 128), ("blk64", 128), ("i4", 512),
                        ("mls", 64), ("mus", 64), ("mui", 64), ("pq", 4), ("frow", 32), ("i2", 64)):
            self.c[name] = (o, w)
            o += w
        self.NSMALL = o
        for name, w in (("cosA", self.Tp), ("sinA", self.Tp), ("cosI", self.Tp), ("sinI", self.Tp), ("rst", self.Tp)):
            self.c[name] = (o, w)
            o += w
        self.NCONST = o
        o = 0
        self.p = {}
        for name, w in (("gmix", self.KC), ("gffn", self.KC), ("conva", 48), ("convf", 3 * self.NF),
                        ("bgate", 2 * self.KC), ("aon", 1), ("qn", 1), ("kn", 1), ("kin", 1), ("alog", 1), ("dtb", 1)):
            self.p[name] = (o, w)
            o += w
        self.NP = o


def host_consts(cfg):
    C = np.zeros((128, cfg.NCONST), np.float32)

    def put(name, arr):
        o, w = cfg.c[name]
        C[: arr.shape[0], o:o + arr.shape[1]] = arr

    I = np.eye(128, dtype=np.float32)
    put("ident", I)
    R = np.zeros((128, 128), np.float32)
    for i in range(128):
        R[i, (i + 64) % 128] = 1
    put("r128", R)
    R2 = np.zeros((128, 128), np.float32)
    for i in range(128):
        b, j = divmod(i, 64)
        R2[i, b * 64 + (j + 32) % 64] = 1
    put("r64", R2)
    B = np.zeros((128, 128), np.float32)
    B[:64, :64] = 1
    B[64:, 64:] = 1
    put("blk64", B)
    put("i4", np.concatenate([I] * 4, axis=1))
    p = np.arange(64)[:, None]
    j = np.arange(64)[None, :]
    NEG = -10000.0
    put("mls", np.where(p > j, 0.0, NEG).astype(np.float32))
    put("mus", np.where(j > p, 0.0, NEG).astype(np.float32))
    put("mui", np.where(j >= p, 0.0, NEG).astype(np.float32))
    pq = np.zeros((128, 4), np.float32)
    pq[0] = [1, 0, 0, 1]
    pq[1] = [0, 1, -1, 0]
    put("pq", pq)
    put("i2", np.concatenate([np.eye(64, dtype=np.float32)] * 2, axis=0))
    put("frow", np.tile((2.0 ** -(np.arange(32, dtype=np.float64) + 1)).astype(np.float32)[None, :], (128, 1)))
    Tp = cfg.Tp
    pos = (np.arange(Tp) - 48).astype(np.float32)
    pos[:48] = 0

    def tables(dim, reps):
        inv = (1.0 / (10000.0 ** (np.arange(0, dim, 2, dtype=np.float32) / np.float32(dim)))).astype(np.float32)
        ang = pos[:, None] * inv[None, :]
        c, s = np.cos(ang).astype(np.float32), np.sin(ang).astype(np.float32)
        cf = np.concatenate([c, c], axis=1).T
        sf = np.concatenate([-s, s], axis=1).T
        return np.tile(cf, (reps, 1)), np.tile(sf, (reps, 1))

    ca, sa = tables(128, 1)
    ci, si = tables(64, 2)
    put("cosA", ca); put("sinA", sa); put("cosI", ci); put("sinI", si)
    rst = np.ones((128, Tp), np.float32)
    rst[:, ::64] = 0
    put("rst", rst)
    return C


def host_lparams(cfg, l, norm_mix, norm_ffn, conv_a, conv_ffn, b_gate, a_out_norm, q_norm, k_norm, kidx_norm, a_log, dt_bias):
    P = np.zeros((128, cfg.NP), np.float32)

    def put(name, arr):
        o, w = cfg.p[name]
        P[: arr.shape[0], o:o + arr.shape[1]] = arr

    put("gmix", norm_mix[l].reshape(cfg.KC, 128).T)
    put("gffn", norm_ffn[l].reshape(cfg.KC, 128).T)
    put("conva", conv_a[l].reshape(4, 12, 128).transpose(2, 1, 0).reshape(128, 48))
    put("convf", conv_ffn[l].reshape(3, cfg.NF, 128).transpose(2, 1, 0).reshape(128, 3 * cfg.NF))
    put("bgate", b_gate[l].reshape(2 * cfg.KC, 128).T)
    put("aon", a_out_norm[l].reshape(128, 1))
    put("qn", q_norm[l].reshape(128, 1))
    put("kn", k_norm[l].reshape(128, 1))
    put("kin", np.concatenate([kidx_norm[l], kidx_norm[l]]).reshape(128, 1))
    put("alog", a_log[l].reshape(4, 1))
    put("dtb", dt_bias[l].reshape(4, 1))
    return P


def build(cfg):
    nc = bass.Bass("TRN2", target_bir_lowering=False)
    D, KC, NCH, S, Tp, NF, NSEQ, DEPTH, TOPK = cfg.D, cfg.KC, cfg.NCH, cfg.S, cfg.Tp, cfg.NF, cfg.NSEQ, cfg.DEPTH, cfg.TOPK
    NCK = NCH + 1
    NBT, GF, NWB = cfg.NBT, cfg.GF, cfg.NWB
    x_d = nc.dram_tensor("x", [NSEQ, S, D], F32, kind="ExternalInput").ap()
    meta_d = nc.dram_tensor("meta", [16, D], F32, kind="ExternalInput").ap()
    const_d = nc.dram_tensor("consts", [128, cfg.NCONST], F32, kind="ExternalInput").ap()
    lp_d = nc.dram_tensor("lparams", [DEPTH, 128, cfg.NP], F32, kind="ExternalInput").ap()
    w_in_d = nc.dram_tensor("w_in", [DEPTH, D, cfg.INC], F32, kind="ExternalInput").ap()
    w_ba_d = nc.dram_tensor("w_branch_a", [DEPTH, 512, D], F32, kind="ExternalInput").ap()
    w_bb_d = nc.dram_tensor("w_branch_b", [DEPTH, 512, D], F32, kind="ExternalInput").ap()
    w_gate_d = nc.dram_tensor("w_gate", [DEPTH, D, 2 * D], F32, kind="ExternalInput").ap()
    w_out_d = nc.dram_tensor("w_out", [DEPTH, D, D], F32, kind="ExternalInput").ap()
    w_up_d = nc.dram_tensor("w_up", [DEPTH, D, 2 * cfg.DFF], F32, kind="ExternalInput").ap()
    w_down_d = nc.dram_tensor("w_down", [DEPTH, cfg.DFF, D], F32, kind="ExternalInput").ap()
    y_d = nc.dram_tensor("y", [NSEQ, S, D], F32, kind="ExternalOutput").ap()
    gq_d = nc.dram_tensor("gq_scr", [16, 128, Tp], BF16).ap()
    grow_d = nc.dram_tensor("grow_scr", [8, Tp], F32).ap()
    hsp_d = nc.dram_tensor("hsp_scr", [128, KC * Tp], F32).ap()

    def tab_d(name):
        o, w = cfg.c[name]
        return const_d[:, o:o + w]

    segs = []
    if cfg.NSEG == 0:
        segs.append((0, 1))
        a = 1
        while a < NCK:
            segs.append((a, min(NCK, a + cfg.NBT)))
            a += cfg.NBT
    else:
        base, rem = divmod(NCK, cfg.NSEG)
        a = 0
        for i in range(cfg.NSEG):
            n_ = base + (1 if i < rem else 0)
            if n_:
                segs.append((a, a + n_))
            a += n_
    NCKm = max(b - a for a, b in segs)
    Tsm = 64 * NCKm

    P = Prog(nc)
    st = contextlib.ExitStack()
    with st:
        def sb(name, shape, dt):
            return st.enter_context(nc.sbuf_tensor(name, shape, dt))

        cst = sb("cst", [128, cfg.NSMALL], F32)
        lpt = sb("lpt", [128, cfg.NP], F32)
        ident_b = sb("ident_b", [128, 128], BF16)
        ones_b = sb("ones_b", [128, 128], BF16)
        r128_b = sb("r128_b", [128, 128], BF16)
        r64_b = sb("r64_b", [128, 128], BF16)
        blk64_b = sb("blk64_b", [128, 128], BF16)
        i4_b = sb("i4_b", [128, 512], BF16)
        i4n_b = sb("i4n_b", [128, 512], BF16)
        HWD = KC * Tp
        ARH = max(HWD, cfg.ARENA_MIN)
        ARO = max(4 * Tp + 64, cfg.ARENA_MIN)
        arenaH = sb("arenaH", [128, ARH], F32)
        arenaN = sb("arenaN", [128, HWD // 2], F32)
        arenaO = sb("arenaO", [128, ARO], F32)
        hT = arenaH[:, 0:HWD].rearrange("p (k t) -> p k t", k=KC)
        nT = arenaN[:, :].bitcast(BF16).rearrange("p (k t) -> p k t", k=KC)
        oab = arenaO[:, 0:4 * Tp].bitcast(BF16).rearrange("p (k t) -> p k t", k=8)
        wbuf = [sb(f"wbuf{i}", [128, 4096], BF16) for i in range(NWB)]
        wab = sb("wab", [128, KC, 64], BF16)
        sq_b = sb("sq_b", [128, KC, 512], BF16)
        rs_f = [sb(f"rs_f{i}", [128, 512], F32) for i in range(2)]
        t_f = [sb(f"t_f{i}", [128, 512], F32) for i in range(2)]
        t_b = [sb(f"t_b{i}", [128, 512], BF16) for i in range(2)]
        rtab_all = sb("rtab_all", [128, 4, 512], F32)
        rtab = [rtab_all[:, 0:2, :], rtab_all[:, 2:4, :]]
        sml = sb("sml", [128, 16], F32)
        bis = sb("bis", [128, 8], F32)
        dfs = sb("dfs", [128, 32], F32)
        bisA = sb("bisA", [128, 8], F32)
        dfsA = sb("dfsA", [128, 32], F32)
        ps = [st.enter_context(nc.psum_tensor(f"ps{i}", [128, 512], F32)) for i in range(7)]
        psb = st.enter_context(nc.psum_tensor("psb", [128, 1024], BF16))
        NQT = S // 128

        class Carver:
            def __init__(self, arena, nwords, o0=0):
                self.a, self.n, self.o = arena, nwords, o0

            def get(self, rows, free, dt):
                n = 1
                for f_ in free:
                    n *= f_
                words = n if dt == F32 else (n + 1) // 2
                words = (words + 7) // 8 * 8
                assert self.o + words <= self.n, (self.o, words, self.n)
                v = self.a[0:rows, self.o:self.o + words]
                self.o += words
                if dt == BF16:
                    v = v.bitcast(BF16)
                v = v[:, 0:n]
                if len(free) == 2:
                    v = v.rearrange("p (a b) -> p a b", a=free[0])
                return v

        def cs(name, rows=128):
            o, w = cfg.c[name]
            assert o + w <= cfg.NSMALL
            return cst[0:rows, o:o + w]

        def lp(name, j=0, rows=128):
            o, w = cfg.p[name]
            return lpt[0:rows, o + j:o + j + 1]

        def MM(out, lhsT, rhs, start, stop, r, w):
            P.op("pe", lambda e: e.matmul(out, lhsT=lhsT, rhs=rhs, start=start, stop=stop), r, w)

        def TR(out, in_, idn, r, w):
            P.op("pe", lambda e: e.transpose(out, in_, idn), r, w)

        def ACT(out, in_, func, r, w, bias=None, scale=None, accum=None):
            kw = {}
            if bias is not None:
                kw["bias"] = bias
            if scale is not None:
                kw["scale"] = scale
            if accum is not None:
                kw["accum_out"] = accum
            P.op("act", lambda e: e.activation(out=out, in_=in_, func=func, **kw), r, w)

        def TT(out, a, b, op, r, w, eng="dve"):
            P.op(eng, lambda e: e.tensor_tensor(out=out, in0=a, in1=b, op=op), r, w)

        def TS(out, a, s1, s2, op0, op1, r, w, accum=None, eng="dve"):
            kw = {}
            if op1 is not None:
                kw["op1"] = op1
            if accum is not None:
                kw["accum_out"] = accum
            P.op(eng, lambda e: e.tensor_scalar(out=out, in0=a, scalar1=s1, scalar2=s2, op0=op0, **kw), r, w)

        def STT(out, a, s, b, op0, op1, r, w, eng="dve"):
            P.op(eng, lambda e: e.scalar_tensor_tensor(out=out, in0=a, scalar=s, in1=b, op0=op0, op1=op1), r, w)

        def CP(out, in_, r, w, eng="dve"):
            if eng == "act":
                P.op("act", lambda e: e.copy(out=out, in_=in_), r, w)
            else:
                P.op(eng, lambda e: e.tensor_copy(out=out, in_=in_), r, w)

        def MSET(ap, v, w, eng="dve"):
            P.op(eng, lambda e: e.memset(ap, v), (), w)

        def DMA(out, in_, r, w, eng="sp"):
            return P.op(eng, lambda e: e.dma_start(out=out, in_=in_), r, w, dma=True)

        blocks = [(t0, min(512, Tp - t0)) for t0 in range(0, Tp, 512)]
        psrr = [0]

        psmod = [5]

        def nps():
            psrr[0] = (psrr[0] + 1) % psmod[0]
            return psrr[0]

        wrr = [0]

        def wnext(nfree, a=None):
            i = wrr[0] % NWB
            wrr[0] += 1
            v = wbuf[i][:, 0:nfree]
            if a is not None:
                v = v.rearrange("p (a b) -> p a b", a=a)
            return v, f"wbuf{i}"

        def wload(src_ap, shape_view):
            n = 1
            for s_ in shape_view[1:]:
                n *= s_
            v, key = wnext(n, shape_view[1] if len(shape_view) == 3 else None)
            DMA(v, src_ap, (), [key], eng="pool")
            return v, key

        def wk(wd, l, c0, ncols):
            return wd[l].rearrange("(kc p) n -> p kc n", p=128)[:, :, c0:c0 + ncols]

        def rstd_from_ps(psi, w, rsb, rskey, inv_n, rows=128):
            ACT(rsb[0:rows, 0:w], ps[psi][0:rows, 0:w], AF.Ln, [f"ps{psi}", "sml"], [rskey], bias=EPS_AP[0:rows, 0:1], scale=inv_n)
            ACT(rsb[0:rows, 0:w], rsb[0:rows, 0:w], AF.Exp, [rskey], [rskey], scale=-0.5)

        DMA(cst[:], const_d[:, 0:cfg.NSMALL], (), ["cst"])
        EPS_AP = sml[:, 15:16]
        MSET(sml[:], 0.0, ["sml"])
        MSET(sml[:, 15:16], EPS, ["sml"])
        MSET(ones_b[:], 1.0, ["ones_b"])
        CP(ident_b[:], cs("ident"), ["cst"], ["ident_b"])
        CP(r128_b[:], cs("r128"), ["cst"], ["r128_b"])
        CP(r64_b[:], cs("r64"), ["cst"], ["r64_b"])
        CP(blk64_b[:], cs("blk64"), ["cst"], ["blk64_b"])
        CP(i4_b[:], cs("i4"), ["cst"], ["i4_b"])
        TS(i4n_b[:], cs("i4"), -1.0, None, ALU.mult, None, ["cst"], ["i4n_b"])
        MSET(wab[:], 0.0, ["wab"])
        identF = cs("ident")
        MSET(t_b[0][:, :], 0.0, ["t_b0"])
        for ft in range(16):
            DMA(gq_d[ft][:, 0:48], t_b[0][:, 0:48], ["t_b0"], [f"gq{ft}"])

        def rmsnorm_to_nT(gname):
            for bi, (t0, w) in enumerate(blocks):
                for kc in range(KC):
                    ACT(sq_b[:, kc, 0:w], hT[:, kc, t0:t0 + w], AF.Square, ["hT"], ["sq_b"])
                pi = nps()
                for kc in range(KC):
                    MM(ps[pi][:, 0:w], ones_b[:], sq_b[:, kc, 0:w], kc == 0, kc == KC - 1, ["ones_b", "sq_b"], [f"ps{pi}"])
                rsb = rs_f[bi % 2]
                rstd_from_ps(pi, w, rsb, f"rs_f{bi % 2}", 1.0 / D)
                for kc in range(KC):
                    STT(nT[:, kc, t0:t0 + w], hT[:, kc, t0:t0 + w], lp(gname, kc), rsb[:, 0:w], ALU.mult, ALU.mult,
                        ["hT", f"rs_f{bi % 2}", "lpt"], ["nT"])

        def proj_block(wv, wkey, col0, M, t0, w, pi, kcs=None, src=None, srckey="nT", srcoff=0):
            src = nT if src is None else src
            kcs = KC if kcs is None else kcs
            for kc in range(kcs):
                MM(ps[pi][0:M, 0:w], wv[:, kc, col0:col0 + M], src[:, srcoff + kc, t0:t0 + w], kc == 0, kc == kcs - 1,
                   [wkey, srckey], [f"ps{pi}"])

        final = []
        for sq in range(NSEQ):
            P.barrier()
            xin = Carver(arenaO, ARO).get(128, [D], F32)
            MSET(hT[:, :, 0:48], 0.0, ["hT"])
            for r in range(-1, NQT):
                if r < 0:
                    DMA(xin[0:16, :], meta_d, (), ["xin"])
                    rows, c0 = 16, 48
                else:
                    DMA(xin[:, :], x_d[sq, 128 * r:128 * (r + 1), :], (), ["xin"])
                    rows, c0 = 128, 64 + 128 * r
                for kc in range(KC):
                    pi = nps()
                    TR(ps[pi][:, 0:rows], xin[0:rows, kc * 128:(kc + 1) * 128], identF[0:rows, 0:rows], ["xin", "cst"], [f"ps{pi}"])
                    CP(hT[:, kc, c0:c0 + rows], ps[pi][:, 0:rows], [f"ps{pi}"], ["hT"], eng=("act" if kc % 2 else "dve"))

            for l in range(DEPTH):
                P.barrier()
                DMA(lpt[:], lp_d[l], (), ["lpt"])
                ACT(sml[0:4, 0:1], lp("alog", rows=4), AF.Exp, ["lpt"], ["sml"])
                TS(sml[0:4, 0:1], sml[0:4, 0:1], -1.0, None, ALU.mult, None, ["sml"], ["sml"])
                psmod[0] = 7
                DMA(hsp_d[:, :], arenaH[:, 0:HWD], ["hT"], ["hsp"])
                rmsnorm_to_nT("gmix")
                P.barrier()
                MSET(oab[:, :, 0:48], 0.0, ["oab"])
                cv = Carver(arenaH, ARH)
                f_aL = [cv.get(128, [Tp + 8], F32) for _ in range(2)]
                f_bL = [cv.get(128, [Tp], F32) for _ in range(2)]
                b_aL = [cv.get(128, [Tp], BF16) for _ in range(2)]
                b_bL = [cv.get(128, [Tp], BF16) for _ in range(2)]
                for i_ in range(2):
                    MSET(f_aL[i_][:, 0:4], 0.0, [f"f_a{i_}"])
                for grp in range(4):
                    wv, wkey = wload(wk(w_in_d, l, grp * 512, 512), [128, KC, 512])
                    for j in range(4):
                        ft = grp * 4 + j
                        pp = ft % 2
                        f_a, f_b, b_a, b_b = f_aL[pp], f_bL[pp], b_aL[pp], b_bL[pp]
                        ka, kb_, kba, kbb = f"f_a{pp}", f"f_b{pp}", f"b_a{pp}", f"b_b{pp}"
                        if grp < 3:
                            for (t0, w) in blocks:
                                pi = nps()
                                proj_block(wv, wkey, j * 128, 128, t0, w, pi)
                                CP(f_a[:, 3 + t0:3 + t0 + w], ps[pi][:, 0:w], [f"ps{pi}"], [ka], eng="act")
                            cw = lambda i, grp=grp, j=j: lp("conva", (grp * 4 + j) * 4 + i)
                            ceng = "pool" if (pp == 1 and cfg.POOLCONV) else "dve"
                            TS(f_b[:, :], f_a[:, 3:3 + Tp], cw(3), None, ALU.mult, None, [ka, "lpt"], [kb_], eng=ceng)
                            for i in (2, 1, 0):
                                STT(f_b[:, :], f_a[:, i:i + Tp], cw(i), f_b[:, :], ALU.mult, ALU.add, [ka, kb_, "lpt"], [kb_], eng=ceng)
                            if grp == 2:
                                ACT(b_a[:, :], f_b[:, :], AF.Silu, [kb_], [kba])
                            else:
                                ACT(f_b[:, :], f_b[:, :], AF.Silu, [kb_], [kb_])
                                ACT(b_b[:, :], f_b[:, :], AF.Square, [kb_], [kbb])
                                for bi, (t0, w) in enumerate(blocks):
                                    pi = nps()
                                    MM(ps[pi][:, 0:w], ones_b[:], b_b[:, t0:t0 + w], True, True, ["ones_b", kbb], [f"ps{pi}"])
                                    rsb = rs_f[bi % 2]
                                    rstd_from_ps(pi, w, rsb, f"rs_f{bi % 2}", 1.0)
                                    STT(b_a[:, t0:t0 + w], f_b[:, t0:t0 + w], (128.0 ** -0.5) if grp == 0 else 1.0, rsb[:, 0:w],
                                        ALU.mult, ALU.mult, [kb_, f"rs_f{bi % 2}"], [kba])
                        else:
                            for (t0, w) in blocks:
                                pi = nps()
                                proj_block(wv, wkey, j * 128, 128, t0, w, pi)
                                ACT(b_a[:, t0:t0 + w], ps[pi][:, 0:w], AF.Silu, [f"ps{pi}"], [kba])
                        DMA(gq_d[ft][:, 48:Tp], b_a[:, 48:Tp], [kba], [f"gq{ft}"])
                P.barrier()
                cv = Carver(arenaH, ARH)
                grs = cv.get(64, [Tp], F32)
                grs2 = cv.get(64, [Tp], F32)
                rstt = cv.get(4, [Tp], F32)
                DMA(rstt[:, :], tab_d("rst")[0:4, :], (), ["rstt"])
                wi = w_in_d[l].rearrange("(kc p) n -> p kc n", p=128)
                DMA(wab[:, :, 0:4], wi[:, :, 2048:2052], (), ["wab"], eng="pool")
                DMA(wab[:, :, 32:36], wi[:, :, 2052:2056], (), ["wab"], eng="pool")
                for (t0, w) in blocks:
                    pi = nps()
                    proj_block(wab, "wab", 0, 64, t0, w, pi)
                    ACT(grs[0:4, t0:t0 + w], ps[pi][0:4, 0:w], AF.Exp, [f"ps{pi}", "lpt"], ["grs"], bias=lp("dtb", rows=4))
                    ACT(grs[32:36, t0:t0 + w], ps[pi][32:36, 0:w], AF.Sigmoid, [f"ps{pi}"], ["grs"])
                ACT(grs[0:4, :], grs[0:4, :], AF.Ln, ["grs"], ["grs"], bias=1.0)
                TS(grs[0:4, :], grs[0:4, :], sml[0:4, 0:1], None, ALU.mult, None, ["grs", "sml"], ["grs"])
                MSET(grs[0:4, 0:48], 0.0, ["grs"])
                MSET(grs[32:36, 0:48], 0.0, ["grs"])
                P.op("dve", lambda e, grs=grs, grs2=grs2, rstt=rstt: e.tensor_tensor_scan(
                    out=grs2[0:4, :], data0=rstt[0:4, :], data1=grs[0:4, :], initial=0.0, op0=ALU.mult, op1=ALU.add),
                    ["grs", "rstt"], ["grs2"])
                DMA(grow_d[0:4, :], grs2[0:4, :], ["grs2"], ["grow"])
                DMA(grow_d[4:8, :], grs[32:36, :], ["grs"], ["grow"])
                P.barrier()

                psmod[0] = 4
                cv = Carver(arenaH, ARH)
                Gc = cv.get(128, [Tsm], F32)
                Bt = cv.get(128, [Tsm], F32)
                qT_, kT_, vT_, kbe, kbT, kdT = [cv.get(128, [Tsm], BF16) for _ in range(6)]
                BW = NBT * 64
                pqm = cv.get(2, [2, BW], F32)
                BWh = BW // (2 if cfg.PACK else 1)
                Lb = [cv.get(128, [BWh], F32) for _ in range(2)]
                Nb = [cv.get(128, [BWh], F32) for _ in range(2)]
                Dm = [cv.get(128, [BWh], F32) for _ in range(2)]
                Yb = [cv.get(128, [BWh], F32) for _ in range(2)]
                XT = cv.get(128, [BWh], BF16)
                kb_sb = cv.get(128, [NBT // (2 if cfg.PACK else 1), 128], BF16)
                vb_sb = cv.get(128, [NBT // (2 if cfg.PACK else 1), 128], BF16)
                S_f = cv.get(128, [128], F32)
                S_b = cv.get(128, [128], BF16)
                vn = [cv.get(128, [128], BF16) for _ in range(2)]
                E2 = [cv.get(128, [Tsm], F32) for _ in range(2)]
                oT2 = [cv.get(128, [Tsm], F32) for _ in range(2)]
                qeT2 = [cv.get(128, [Tsm], BF16) for _ in range(2)]
                zT2 = [cv.get(128, [Tsm], BF16) for _ in range(2)]
                NH = (NCKm + 1) // 2 if cfg.PACK else NCKm
                qkm2 = [cv.get(128, [NH, 64], BF16) for _ in range(2)]
                u2 = [cv.get(128, [NH, 128], F32) for _ in range(2)]
                wT2 = [cv.get(128, [NCKm * 64], BF16) for _ in range(2)]
                kd2 = [cv.get(128, [NH, 128], BF16) for _ in range(2)]
                pq = cs("pq", 2)
                items = [(h, si) for h in range(4) for si in range(len(segs))]

                HB = NBT // (2 if cfg.PACK else 1)
                BWc = HB * 64

                def place(j):
                    half, jj = divmod(j, HB)
                    return slice(64 * half, 64 * half + 64), slice(jj * 64, (jj + 1) * 64), half, jj

                def setup_gen(k):
                    h, si = items[k]
                    par = k % 2
                    cA, cB = segs[si]
                    nck = cB - cA
                    Ts = nck * 64
                    g0 = cA * 64
                    E, qeT, zT_, qkm, u_sb, wT_sb, kd_sb = E2[par], qeT2[par], zT2[par], qkm2[par], u2[par], wT2[par], kd2[par]
                    kE, kqe, kz, kqk, ku, kw, kkd = f"E{par}", f"qeT{par}", f"zT{par}", f"qkm{par}", f"u{par}", f"wT{par}", f"kd{par}"
                    DMA(qT_[:, 0:Ts], gq_d[h][:, g0:g0 + Ts], [f"gq{h}"], ["qT_"])
                    DMA(kT_[:, 0:Ts], gq_d[4 + h][:, g0:g0 + Ts], [f"gq{4 + h}"], ["kT_"])
                    DMA(vT_[:, 0:Ts], gq_d[8 + h][:, g0:g0 + Ts], [f"gq{8 + h}"], ["vT_"])
                    DMA(zT_[:, 0:Ts], gq_d[12 + h][:, g0:g0 + Ts], [f"gq{12 + h}"], [kz])
                    DMA(Gc[:, 0:Ts], grow_d[h:h + 1, g0:g0 + Ts].partition_broadcast(128), ["grow"], ["Gc"])
                    DMA(Bt[:, 0:Ts], grow_d[4 + h:5 + h, g0:g0 + Ts].partition_broadcast(128), ["grow"], ["Bt"])
                    yield
                    ACT(E[:, 0:Ts], Gc[:, 0:Ts], AF.Exp, ["Gc"], [kE])
                    TT(kbe[:, 0:Ts], kT_[:, 0:Ts], Bt[:, 0:Ts], ALU.mult, ["kT_", "Bt"], ["kbe"])
                    TT(kbT[:, 0:Ts], kbe[:, 0:Ts], E[:, 0:Ts], ALU.mult, ["kbe", kE], ["kbT"])
                    yield
                    TT(vT_[:, 0:Ts], vT_[:, 0:Ts], Bt[:, 0:Ts], ALU.mult, ["vT_", "Bt"], ["vT_"])
                    TT(qeT[:, 0:Ts], qT_[:, 0:Ts], E[:, 0:Ts], ALU.mult, ["qT_", kE], [kqe])
                    Gc3 = Gc[:, 0:Ts].rearrange("p (n c) -> p n c", c=64)
                    TT(Bt[:, 0:Ts].rearrange("p (n c) -> p n c", c=64), Gc3[:, :, 63:64].to_broadcast([128, nck, 64]), Gc3, ALU.subtract,
                       ["Gc", "Bt"], ["Bt"])
                    yield
                    ACT(Bt[:, 0:Ts], Bt[:, 0:Ts], AF.Exp, ["Bt"], ["Bt"])
                    TT(kdT[:, 0:Ts], kT_[:, 0:Ts], Bt[:, 0:Ts], ALU.mult, ["kT_", "Bt"], ["kdT"])
                    yield
                    for b0 in range(0, nck, NBT):
                        nb = min(NBT, nck - b0)
                        assert nb == NBT or nb <= HB, (nb, NBT)
                        rows = 128 if nb > HB else 64
                        ncl = min(nb, HB)
                        Wc = ncl * 64
                        W = nb * 64
                        b0h = b0 // 2 if cfg.PACK else b0
                        bc = slice(b0 * 64, b0 * 64 + W)
                        TS(pqm[:, 0, 0:W], Gc[0:2, bc], pq[:, 0:1], pq[:, 1:2], ALU.mult, ALU.add, ["Gc", "cst"], ["pqm"])
                        TS(pqm[:, 1, 0:W], Gc[0:2, bc], pq[:, 2:3], pq[:, 3:4], ALU.mult, ALU.add, ["Gc", "cst"], ["pqm"])
                        Pm, Qm = pqm[:, 0, :], pqm[:, 1, :]
                        assert not cfg.PACK
                        pgL, pgN, pgQ, pM = nps(), nps(), nps(), nps()
                        for j in range(nb):
                            ch = slice((b0 + j) * 64, (b0 + j + 1) * 64)
                            o = slice(j * 64, (j + 1) * 64)
                            MM(ps[pgL][0:64, o], kbe[:, ch], kT_[:, ch], True, True, ["kbe", "kT_"], [f"ps{pgL}"])
                            MM(ps[pgN][0:64, o], kT_[:, ch], kbe[:, ch], True, True, ["kbe", "kT_"], [f"ps{pgN}"])
                            MM(ps[pgQ][0:64, o], kT_[:, ch], qT_[:, ch], True, True, ["qT_", "kT_"], [f"ps{pgQ}"])
                            MM(ps[pM][0:64, o], Pm[:, o], Qm[:, o], True, True, ["pqm"], [f"ps{pM}"])
                        M3 = ps[pM][0:64, 0:Wc].rearrange("p (n c) -> p n c", c=64)

                        def mk(name):
                            return cs(name, 64).unsqueeze(1).to_broadcast([64, nb, 64])

                        def d3(i_):
                            return Dm[i_][0:64, 0:Wc].rearrange("p (n c) -> p n c", c=64)

                        STT(d3(0), M3, 1.0, mk("mls"), ALU.mult, ALU.add, [f"ps{pM}", "cst"], ["Dm0"])
                        ACT(Dm[0][0:64, 0:Wc], Dm[0][0:64, 0:Wc], AF.Exp, ["Dm0"], ["Dm0"])
                        STT(Lb[0][0:64, 0:Wc], ps[pgL][0:64, 0:Wc], -1.0, Dm[0][0:64, 0:Wc], ALU.mult, ALU.mult, [f"ps{pgL}", "Dm0"], ["Lb0"])
                        yield
                        STT(d3(1), M3, -1.0, mk("mus"), ALU.mult, ALU.add, [f"ps{pM}", "cst"], ["Dm1"])
                        ACT(Dm[1][0:64, 0:Wc], Dm[1][0:64, 0:Wc], AF.Exp, ["Dm1"], ["Dm1"])
                        STT(Nb[0][0:64, 0:Wc], ps[pgN][0:64, 0:Wc], -1.0, Dm[1][0:64, 0:Wc], ALU.mult, ALU.mult, [f"ps{pgN}", "Dm1"], ["Nb0"])
                        yield
                        STT(d3(0), M3, -1.0, mk("mui"), ALU.mult, ALU.add, [f"ps{pM}", "cst", "Dm0"], ["Dm0"])
                        ACT(Dm[0][0:64, 0:Wc], Dm[0][0:64, 0:Wc], AF.Exp, ["Dm0"], ["Dm0"])
                        TT(qkm[0:64, b0h:b0h + ncl, :], ps[pgQ][0:64, 0:Wc].rearrange("p (n c) -> p n c", c=64), d3(0), ALU.mult, [f"ps{pgQ}", "Dm0"], [kqk])
                        yield
                        TT(Yb[0][0:rows, 0:Wc].rearrange("p (n c) -> p n c", c=64), Nb[0][0:rows, 0:Wc].rearrange("p (n c) -> p n c", c=64),
                           cs("i2")[0:rows, :].unsqueeze(1).to_broadcast([rows, ncl, 64]), ALU.add, ["Nb0", "cst"], ["Yb0"])
                        cur = 0
                        for lev in range(1, 6):
                            a_, b_ = (lev - 1) % 2, lev % 2
                            pa, pb, py = nps(), nps(), nps()
                            for j in range(nb):
                                pr, o, _, _ = place(j)
                                MM(ps[pa][pr, o], Nb[a_][pr, o], Lb[a_][pr, o], True, True, [f"Nb{a_}", f"Lb{a_}"], [f"ps{pa}"])
                                if lev < 5:
                                    MM(ps[pb][pr, o], Lb[a_][pr, o], Nb[a_][pr, o], True, True, [f"Nb{a_}", f"Lb{a_}"], [f"ps{pb}"])
                            CP(Lb[b_][0:rows, 0:Wc], ps[pa][0:rows, 0:Wc], [f"ps{pa}"], [f"Lb{b_}"], eng="act")
                            if lev < 5:
                                CP(Nb[b_][0:rows, 0:Wc], ps[pb][0:rows, 0:Wc], [f"ps{pb}"], [f"Nb{b_}"], eng="dve")
                            yield
                            for j in range(nb):
                                pr, o, _, _ = place(j)
                                MM(ps[py][pr, o], Lb[b_][pr, o], Yb[cur][pr, o], True, True, [f"Lb{b_}", f"Yb{cur}"], [f"ps{py}"])
                            if lev < 5:
                                TT(Yb[1 - cur][0:rows, 0:Wc], ps[py][0:rows, 0:Wc], Yb[cur][0:rows, 0:Wc], ALU.add, [f"ps{py}", f"Yb{cur}"], [f"Yb{1 - cur}"])
                                cur = 1 - cur
                            else:
                                TT(XT[0:rows, 0:Wc], ps[py][0:rows, 0:Wc], Yb[cur][0:rows, 0:Wc], ALU.add, [f"ps{py}", f"Yb{cur}"], ["XT"])
                            yield
                        for (srcT, skey, dst, dkey, dsl) in ((kbT, "kbT", kb_sb, "kb_sb", None), (vT_, "vT_", vb_sb, "vb_sb", None),
                                                             (kdT, "kdT", kd_sb, kkd, b0h)):
                            for j in range(nb):
                                ch = slice((b0 + j) * 64, (b0 + j + 1) * 64)
                                pr, o, _, jj = place(j)
                                TR(psb[pr, jj * 128:(jj + 1) * 128], srcT[:, ch], ident_b[:], [skey, "ident_b"], ["psb"])
                            src3 = psb[0:rows, 0:ncl * 128].rearrange("p (n c) -> p n c", c=128)
                            if dsl is None:
                                CP(dst[0:rows, 0:ncl, :], src3, ["psb"], [dkey], eng="act")
                            else:
                                CP(dst[0:rows, b0h:b0h + ncl, :], src3, ["psb"], [dkey], eng="act")
                            yield
                        for q0_ in range(0, ncl, 4):
                            n4 = min(4, ncl - q0_)
                            pi = nps()
                            for j in range(nb):
                                pr, o, _, jj = place(j)
                                if q0_ <= jj < q0_ + n4:
                                    MM(ps[pi][pr, (jj - q0_) * 128:(jj - q0_ + 1) * 128], XT[pr, o], vb_sb[pr, jj, :], True, True, ["XT", "vb_sb"], [f"ps{pi}"])
                            CP(u_sb[0:rows, b0h + q0_:b0h + q0_ + n4, :], ps[pi][0:rows, 0:n4 * 128].rearrange("p (n c) -> p n c", c=128), [f"ps{pi}"], [ku])
                        pi = nps()
                        for j in range(nb):
                            pr, o, _, jj = place(j)
                            MM(ps[pi][:, j * 64:(j + 1) * 64], kb_sb[pr, jj, :], XT[pr, o], True, True, ["XT", "kb_sb"], [f"ps{pi}"])
                        CP(wT_sb[:, b0 * 64:b0 * 64 + W], ps[pi][:, 0:W], [f"ps{pi}"], [kw], eng="act")
                        yield

                def scan_gen(k):
                    h, si = items[k]
                    par = k % 2
                    cA, cB = segs[si]
                    nck = cB - cA
                    E, oT, qeT, qkm, u_sb, wT_sb, kd_sb = E2[par], oT2[par], qeT2[par], qkm2[par], u2[par], wT2[par], kd2[par]
                    kE, ko, kqe, kqk, ku, kw, kkd = f"E{par}", f"oT{par}", f"qeT{par}", f"qkm{par}", f"u{par}", f"wT{par}", f"kd{par}"
                    if si == 0:
                        MSET(S_f[:], 0.0, ["S_f"])
                        MSET(S_b[:], 0.0, ["S_b"])
                    for n in range(nck):
                        ch = slice(n * 64, (n + 1) * 64)
                        b0 = (n // NBT) * NBT
                        pr, _, _, jj = place(n - b0)
                        idx = (b0 // 2 if cfg.PACK else b0) + jj
                        pv, po, pss = 4, 5, 6
                        vnb = vn[n % 2]
                        vk = f"vn{n % 2}"
                        MM(ps[pv][pr, 0:128], wT_sb[:, ch], S_b[:], True, True, [kw, "S_b"], [f"ps{pv}"])
                        TT(vnb[pr, :], u_sb[pr, idx, :], ps[pv][pr, 0:128], ALU.subtract, [ku, f"ps{pv}"], [vk])
                        MM(ps[pss][:, 0:128], kd_sb[pr, idx, :], vnb[pr, :], True, True, [kkd, vk], [f"ps{pss}"])
                        MM(ps[po][:, 0:64], S_b[:], qeT[:, ch], True, False, ["S_b", kqe], [f"ps{po}"])
                        MM(ps[po][:, 0:64], vnb[pr, :], qkm[pr, idx, :], False, True, [vk, kqk], [f"ps{po}"])
                        STT(S_b[:], S_f[:], E[:, n * 64 + 63:n * 64 + 64], ps[pss][:, 0:128], ALU.mult, ALU.add, ["S_f", kE, f"ps{pss}"], ["S_b"])
                        STT(S_f[:], S_f[:], E[:, n * 64 + 63:n * 64 + 64], ps[pss][:, 0:128], ALU.mult, ALU.add, ["S_f", kE, f"ps{pss}"], ["S_f"])
                        CP(oT[:, ch], ps[po][:, 0:64], [f"ps{po}"], [ko], eng="act")
                        yield

                def post_norm(k):
                    h, si = items[k]
                    par = k % 2
                    cA, cB = segs[si]
                    Ts = (cB - cA) * 64
                    g0 = cA * 64
                    oT, zT_ = oT2[par], zT2[par]
                    ko, kz = f"oT{par}", f"zT{par}"
                    ACT(kbe[:, 0:Ts], oT[:, 0:Ts], AF.Square, [ko], ["kbe"])
                    for bi, t0 in enumerate(range(0, Ts, 512)):
                        w = min(512, Ts - t0)
                        pi = nps()
                        MM(ps[pi][:, 0:w], ones_b[:], kbe[:, t0:t0 + w], True, True, ["ones_b", "kbe"], [f"ps{pi}"])
                        rsb = rs_f[bi % 2]
                        rstd_from_ps(pi, w, rsb, f"rs_f{bi % 2}", 1.0 / 128)
                        tf = t_f[bi % 2]
                        STT(tf[:, 0:w], oT[:, t0:t0 + w], lp("aon"), rsb[:, 0:w], ALU.mult, ALU.mult, [ko, f"rs_f{bi % 2}", "lpt"], [f"t_f{bi % 2}"])
                        TT(oab[:, h, g0 + t0:g0 + t0 + w], tf[:, 0:w], zT_[:, t0:t0 + w], ALU.mult, [f"t_f{bi % 2}", kz], ["oab"])

                for _ in setup_gen(0):
                    pass
                for k in range(len(items)):
                    gs = scan_gen(k)
                    gn = setup_gen(k + 1) if k + 1 < len(items) else iter(())
                    alive_s, alive_n = True, True
                    while alive_s or alive_n:
                        for _ in range(cfg.RATIO):
                            if alive_n:
                                try:
                                    next(gn)
                                except StopIteration:
                                    alive_n = False
                        if alive_s:
                            try:
                                next(gs)
                            except StopIteration:
                                alive_s = False
                    post_norm(k)
                psmod[0] = 5
                P.barrier()

                cv = Carver(arenaH, ARH)
                qT4 = [cv.get(128, [Tp], BF16) for _ in range(4)]
                qk4 = ["qT4_0", "qT4_1", "qT4_2", "qT4_3"]
                iq4 = [cv.get(128, [Tp], BF16) for _ in range(4)]
                ik4 = ["iq4_0", "iq4_1", "iq4_2", "iq4_3"]
                kTd = cv.get(128, [Tp], BF16)
                ikT = cv.get(128, [Tp], BF16)
                v_tm = cv.get(128, [NQT, 128], BF16)
                v_me = cv.get(16, [128], BF16)
                iw_tm = cv.get(128, [NQT, 8], F32)
                sc = cv.get(128, [S], F32)
                nm = cv.get(128, [S], BF16)

                def rope_tile(wv, wkey, col0, dst, dkey, gain, ones_m, ones_key, inv_n, rot, rotkey, cname, sname):
                    for bi, (t0, w) in enumerate(blocks):
                        rt = rtab[bi % 2]
                        rk = f"rtab{bi % 2}"
                        DMA(rt[:, 0, 0:w], tab_d(cname)[:, t0:t0 + w], (), [rk])
                        DMA(rt[:, 1, 0:w], tab_d(sname)[:, t0:t0 + w], (), [rk])
                        pi = nps()
                        proj_block(wv, wkey, col0, 128, t0, w, pi)
                        tb = t_b[bi % 2]
                        tkey = f"t_b{bi % 2}"
                        if gain is not None:
                            ACT(sq_b[:, 0, 0:w], ps[pi][:, 0:w], AF.Square, [f"ps{pi}"], ["sq_b"])
                            p2 = nps()
                            MM(ps[p2][:, 0:w], ones_m[:], sq_b[:, 0, 0:w], True, True, [ones_key, "sq_b"], [f"ps{p2}"])
                            rsb = rs_f[bi % 2]
                            rstd_from_ps(p2, w, rsb, f"rs_f{bi % 2}", inv_n)
                            STT(tb[:, 0:w], ps[pi][:, 0:w], gain, rsb[:, 0:w], ALU.mult, ALU.mult, [f"ps{pi}", f"rs_f{bi % 2}", "lpt"], [tkey])
                        else:
                            CP(tb[:, 0:w], ps[pi][:, 0:w], [f"ps{pi}"], [tkey], eng="act")
                        p3 = nps()
                        MM(ps[p3][:, 0:w], rot[:], tb[:, 0:w], True, True, [rotkey, tkey], [f"ps{p3}"])
                        tf = t_f[bi % 2]
                        TT(tf[:, 0:w], ps[p3][:, 0:w], rt[:, 1, 0:w], ALU.mult, [f"ps{p3}", rk], [f"t_f{bi % 2}"])
                        rsb2 = rs_f[bi % 2]
                        TT(rsb2[:, 0:w], tb[:, 0:w], rt[:, 0, 0:w], ALU.mult, [tkey, rk], [f"rs_f{bi % 2}"])
                        TT(dst[:, t0:t0 + w], rsb2[:, 0:w], tf[:, 0:w], ALU.add, [f"rs_f{bi % 2}", f"t_f{bi % 2}"], [dkey])

                wv, wkey = wload(wk(w_in_d, l, 2056, 512), [128, KC, 512])
                for hh in range(4):
                    rope_tile(wv, wkey, hh * 128, qT4[hh], qk4[hh], lp("qn"), ones_b, "ones_b", 1.0 / 128, r128_b, "r128_b", "cosA", "sinA")
                wv, wkey = wload(wk(w_in_d, l, 2568, 256), [128, KC, 256])
                rope_tile(wv, wkey, 0, kTd, "kTd", lp("kn"), ones_b, "ones_b", 1.0 / 128, r128_b, "r128_b", "cosA", "sinA")
                for kt in range(-1, NQT):
                    c0, rows = (48, 16) if kt < 0 else (64 + 128 * kt, 128)
                    pi = nps()
                    for kc in range(KC):
                        MM(ps[pi][0:rows, 0:128], nT[:, kc, c0:c0 + rows], wv[:, kc, 128:256], kc == 0, kc == KC - 1, ["nT", wkey], [f"ps{pi}"])
                    if kt < 0:
                        CP(v_me[:, :], ps[pi][0:16, 0:128], [f"ps{pi}"], ["v_me"], eng="act")
                    else:
                        CP(v_tm[:, kt, :], ps[pi][:, 0:128], [f"ps{pi}"], ["v_tm"], eng="act")
                wv, wkey = wload(wk(w_in_d, l, 2824, 512), [128, KC, 512])
                for tl in range(4):
                    rope_tile(wv, wkey, tl * 128, iq4[tl], ik4[tl], None, None, None, None, r64_b, "r64_b", "cosI", "sinI")
                wv, wkey = wnext(KC * 136, KC)
                DMA(wv[:, :, 0:64], wi[:, :, 3336:3400], (), [wkey], eng="pool")
                DMA(wv[:, :, 64:128], wi[:, :, 3336:3400], (), [wkey], eng="pool")
                DMA(wv[:, :, 128:136], wi[:, :, 3400:3408], (), [wkey], eng="pool")
                rope_tile(wv, wkey, 0, ikT, "ikT", lp("kin"), blk64_b, "blk64_b", 1.0 / 64, r64_b, "r64_b", "cosI", "sinI")
                for qt in range(NQT):
                    c0 = 64 + 128 * qt
                    pi = nps()
                    for kc in range(KC):
                        MM(ps[pi][:, 0:8], nT[:, kc, c0:c0 + 128], wv[:, kc, 128:136], kc == 0, kc == KC - 1, ["nT", wkey], [f"ps{pi}"])
                    TS(iw_tm[:, qt, :], ps[pi][:, 0:8], (8.0 ** -0.5) * (64.0 ** -0.5), None, ALU.mult, None, [f"ps{pi}"], ["iw_tm"])

                att_scale = 128.0 ** -0.5
                sc2 = cv.get(128, [S], F32)
                scs = [sc, sc2]
                junk = sq_b[:, :, :].rearrange("p a b -> p (a b)")

                def S1(qt):
                    q0 = 64 + 128 * qt
                    Sr = 128 * (qt + 1)
                    scb, sk = scs[qt % 2], f"sc{qt % 2}"
                    for kb0 in range(0, Sr, 512):
                        w = min(512, Sr - kb0)
                        for ih in range(8):
                            tl, hf = divmod(ih, 2)
                            pr = slice(64 * hf, 64 * hf + 64)
                            pi = nps()
                            MM(ps[pi][:, 0:w], iq4[tl][pr, q0:q0 + 128], ikT[pr, 64 + kb0:64 + kb0 + w], True, True,
                               [ik4[tl], "ikT"], [f"ps{pi}"])
                            tf = t_f[ih % 2]
                            ACT(tf[:, 0:w], ps[pi][:, 0:w], AF.Relu, [f"ps{pi}"], [f"t_f{ih % 2}"])
                            if ih == 0:
                                TS(scb[:, kb0:kb0 + w], tf[:, 0:w], iw_tm[:, qt, 0:1], None, ALU.mult, None, [f"t_f{ih % 2}", "iw_tm"], [sk])
                            else:
                                STT(scb[:, kb0:kb0 + w], tf[:, 0:w], iw_tm[:, qt, ih:ih + 1], scb[:, kb0:kb0 + w], ALU.mult, ALU.add,
                                    [f"t_f{ih % 2}", "iw_tm", sk], [sk])

                junkA = rtab_all[:, :, :].rearrange("p a b -> p (a b)").bitcast(BF16)
                nmA = junkA[:, 2048:4096]
                nm_neg = {}

                def S2(qt):
                    Sr = 128 * (qt + 1)
                    scb, sk = scs[qt % 2], f"sc{qt % 2}"
                    NIT = cfg.NIT
                    on_act = (qt % 2 == 1) and Sr <= 2048
                    B, bk, DF, dk = (bisA, "bisA", dfsA, "dfsA") if on_act else (bis, "bis", dfs, "dfs")
                    if Sr > TOPK:
                        P.op("dve", lambda e: e.tensor_reduce(out=B[:, 0:1], in_=scb[:, 0:Sr], axis=mybir.AxisListType.X, op=ALU.min), [sk], [bk])
                        P.op("dve", lambda e: e.tensor_reduce(out=B[:, 1:2], in_=scb[:, 0:Sr], axis=mybir.AxisListType.X, op=ALU.max), [sk], [bk])
                        TT(B[:, 2:3], B[:, 1:2], B[:, 0:1], ALU.subtract, [bk], [bk])
                        if on_act:
                            TS(DF[:, 0:NIT + 1], cs("frow")[:, 0:NIT + 1], B[:, 2:3], 0.5, ALU.mult, ALU.mult, [bk, "cst"], [dk])
                            STT(B[:, 3:4], DF[:, 0:1], 2.0, B[:, 0:1], ALU.mult, ALU.add, [bk, dk], [bk])
                        else:
                            TS(DF[:, 0:NIT + 1], cs("frow")[:, 0:NIT + 1], B[:, 2:3], None, ALU.mult, None, [bk, "cst"], [dk])
                            TT(B[:, 3:4], B[:, 0:1], DF[:, 0:1], ALU.add, [bk, dk], [bk])
                    MSET(scb[0:64, Sr - 64:Sr], -1e30, [sk])
                    if Sr > TOPK:
                        for it in range(NIT):
                            if on_act:
                                ACT(junkA[:, 0:Sr], scb[:, 0:Sr], AF.Sign, [sk, bk], ["junkA", bk], bias=B[:, 3:4], scale=-1.0, accum=B[:, 4:5])
                                ACT(B[:, 5:6], B[:, 4:5], AF.Sign, [bk], [bk], bias=float(Sr - 2 * TOPK + 0.5), scale=-1.0)
                                ACT(B[:, 3:4], B[:, 5:6], AF.Identity, [bk, dk], [bk], bias=B[:, 3:4], scale=DF[:, it:it + 1])
                            else:
                                TS(junk[:, 0:Sr], scb[:, 0:Sr], B[:, 3:4], None, ALU.is_ge, ALU.add, [sk, bk], ["sq_b", bk], accum=B[:, 4:5])
                                TS(B[:, 5:6], B[:, 4:5], TOPK - 0.5, 0.5, ALU.is_ge, ALU.subtract, [bk], [bk])
                                STT(B[:, 3:4], B[:, 5:6], DF[:, it:it + 1], B[:, 3:4], ALU.mult, ALU.add, [bk, dk], [bk])
                        if on_act:
                            ACT(B[:, 6:7], B[:, 3:4], AF.Identity, [bk], [bk], scale=-1.0)
                            ACT(B[:, 0:1], DF[:, NIT:NIT + 1], AF.Identity, [bk, dk], [bk], bias=B[:, 6:7], scale=2.0)
                            if Sr - 64 <= TOPK:
                                MSET(B[0:64, 0:1], 1e29, [bk], eng="pool")
                        else:
                            TT(B[:, 0:1], B[:, 3:4], DF[:, NIT:NIT + 1], ALU.subtract, [bk, dk], [bk])
                            if Sr - 64 <= TOPK:
                                MSET(B[0:64, 0:1], -1e29, [bk])
                    else:
                        on_act = False
                        MSET(B[:, 0:1], -1e29, [bk])
                    if on_act:
                        ACT(junkA[:, 0:Sr], scb[:, 0:Sr], AF.Sign, [sk, bk], ["junkA"], bias=B[:, 0:1])
                        ACT(nmA[:, 0:Sr], junkA[:, 0:Sr], AF.Relu, ["junkA"], ["nmA"], scale=NEGB)
                    elif qt % 2 == 1:
                        TS(nmA[:, 0:Sr], scb[:, 0:Sr], B[:, 0:1], NEGB, ALU.is_lt, ALU.mult, [sk, bk], ["nmA"])
                    else:
                        TS(nm[:, 0:Sr], scb[:, 0:Sr], B[:, 0:1], NEGB, ALU.is_lt, ALU.mult, [sk, bk], ["nm"])
                    nm_neg[qt] = on_act

                def S3(qt):
                    if qt < 0:
                        q0, nq, kts = 48, 16, []
                    else:
                        q0, nq, kts = 64 + 128 * qt, 128, list(range(qt + 1))
                    NQ = 4 * nq
                    keysets = [(-1, 16)] + [(kt, 128) for kt in kts]
                    for ki, (kt, nk) in enumerate(keysets):
                        pi = nps()
                        first, last = ki == 0, ki == len(keysets) - 1
                        kc0 = 48 if kt < 0 else 64 + 128 * kt
                        for hh in range(4):
                            MM(ps[pi][0:nk, hh * nq:(hh + 1) * nq], kTd[:, kc0:kc0 + nk], qT4[hh][:, q0:q0 + nq], True, kt < 0,
                               ["kTd", qk4[hh]], [f"ps{pi}"])
                            if kt >= 0:
                                i4x, i4k = (i4n_b, "i4n_b") if nm_neg.get(qt) else (i4_b, "i4_b")
                                nmx, nmk = (nmA, "nmA") if qt % 2 == 1 else (nm, "nm")
                                MM(ps[pi][0:nk, hh * nq:(hh + 1) * nq], nmx[:, 128 * kt:128 * kt + 128], i4x[:, hh * 128:(hh + 1) * 128], False, True,
                                   [nmk, i4k], [f"ps{pi}"])
                        tb = t_b[ki % 2]
                        tkey = f"t_b{ki % 2}"
                        ACT(tb[0:nk, 0:NQ], ps[pi][0:nk, 0:NQ], AF.Exp, [f"ps{pi}"], [tkey], scale=att_scale)
                        vsrc = v_me[:, :] if kt < 0 else v_tm[:, kt, :]
                        vkey = "v_me" if kt < 0 else "v_tm"
                        MM(ps[5][:, 0:NQ], vsrc, tb[0:nk, 0:NQ], first, last, [vkey, tkey], ["ps5"])
                        MM(ps[6][:, 0:NQ], ones_b[0:nk, :], tb[0:nk, 0:NQ], first, last, ["ones_b", tkey], ["ps6"])
                    rz = rs_f[0]
                    P.op("dve", lambda e: e.reciprocal(out=rz[:, 0:NQ], in_=ps[6][:, 0:NQ]), ["ps6"], ["rs_f0"])
                    TT(oab[:, 4:8, q0:q0 + nq], ps[5][:, 0:NQ].rearrange("p (h q) -> p h q", h=4), rz[:, 0:NQ].rearrange("p (h q) -> p h q", h=4),
                       ALU.mult, ["ps5", "rs_f0"], ["oab"])

                S3(-1)
                pairs = [(a_, a_ + 1 if a_ + 1 < NQT else None) for a_ in range(0, NQT, 2)]

                def s1pair(pp):
                    for t_ in pp:
                        if t_ is not None:
                            S1(t_)

                def s2pair(pp):
                    if pp[1] is not None:
                        S2(pp[1])
                    S2(pp[0])

                s1pair(pairs[0])
                s2pair(pairs[0])
                for pi_ in range(len(pairs)):
                    if pi_ + 1 < len(pairs):
                        s1pair(pairs[pi_ + 1])
                    for t_ in pairs[pi_]:
                        if t_ is not None:
                            S3(t_)
                    if pi_ + 1 < len(pairs):
                        s2pair(pairs[pi_ + 1])
                P.barrier()

                psmod[0] = 7
                cv = Carver(arenaH, ARH, HWD // 2)
                yT = [cv.get(128, [Tp], BF16) for _ in range(KC)]
                ykeys = [f"yT{i}" for i in range(KC)]
                wg_ = w_gate_d[l].rearrange("(kc p) n -> p kc n", p=128)
                for f in range(KC):
                    wv, wkey = wnext(KC * 256, KC)
                    DMA(wv[:, :, 0:128], wg_[:, :, f * 128:(f + 1) * 128], (), [wkey], eng="pool")
                    DMA(wv[:, :, 128:256], wg_[:, :, D + f * 128:D + (f + 1) * 128], (), [wkey], eng="pool")
                    wb, wbkey = wnext(8 * 128, 8)
                    DMA(wb[:, 0:4, :], w_ba_d[l].rearrange("(kc p) n -> p kc n", p=128)[:, :, f * 128:(f + 1) * 128], (), [wbkey], eng="pool")
                    DMA(wb[:, 4:8, :], w_bb_d[l].rearrange("(kc p) n -> p kc n", p=128)[:, :, f * 128:(f + 1) * 128], (), [wbkey], eng="pool")
                    for bi, (t0, w) in enumerate(blocks):
                        pga, pgb, pya, pyb = nps(), nps(), nps(), nps()
                        proj_block(wv, wkey, 0, 128, t0, w, pga)
                        proj_block(wv, wkey, 128, 128, t0, w, pgb)
                        proj_block(wb, wbkey, 0, 128, t0, w, pya, kcs=4, src=oab, srckey="oab", srcoff=0)
                        proj_block(wb[:, 4:8, :], wbkey, 0, 128, t0, w, pyb, kcs=4, src=oab, srckey="oab", srcoff=4)
                        ACT(t_f[0][:, 0:w], ps[pga][:, 0:w], AF.Sigmoid, [f"ps{pga}", "lpt"], ["t_f0"], bias=lp("bgate", f))
                        ACT(t_f[1][:, 0:w], ps[pgb][:, 0:w], AF.Sigmoid, [f"ps{pgb}", "lpt"], ["t_f1"], bias=lp("bgate", KC + f))
                        TT(t_f[0][:, 0:w], t_f[0][:, 0:w], ps[pya][:, 0:w], ALU.mult, ["t_f0", f"ps{pya}"], ["t_f0"])
                        TT(t_f[1][:, 0:w], t_f[1][:, 0:w], ps[pyb][:, 0:w], ALU.mult, ["t_f1", f"ps{pyb}"], ["t_f1"])
                        TT(yT[f][:, t0:t0 + w], t_f[0][:, 0:w], t_f[1][:, 0:w], ALU.add, ["t_f0", "t_f1"], [ykeys[f]])
                P.barrier()
                KH = KC // 2
                hlo = arenaH[:, 0:HWD // 2].rearrange("p (k t) -> p k t", k=KH)
                hhi = arenaN[:, :].rearrange("p (k t) -> p k t", k=KC - KH)
                DMA(arenaH[:, 0:HWD // 2], hsp_d[:, 0:HWD // 2], ["hsp"], ["hlo"])
                DMA(arenaN[:, :], hsp_d[:, HWD // 2:HWD], ["hsp"], ["hhi"])
                for f in range(KC):
                    wv, wkey = wload(wk(w_out_d, l, f * 128, 128), [128, KC, 128])
                    dst, dkey = (hlo[:, f, :], "hlo") if f < KH else (hhi[:, f - KH, :], "hhi")
                    for (t0, w) in blocks:
                        pi = nps()
                        for kc in range(KC):
                            MM(ps[pi][:, 0:w], wv[:, kc, :], yT[kc][:, t0:t0 + w], kc == 0, kc == KC - 1, [wkey, ykeys[kc]], [f"ps{pi}"])
                        TT(dst[:, t0:t0 + w], dst[:, t0:t0 + w], ps[pi][:, 0:w], ALU.add, [dkey, f"ps{pi}"], [dkey])
                P.barrier()
                for f in range(KH, KC):
                    CP(hT[:, f, :], hhi[:, f - KH, :], ["hhi"], ["hT"], eng=("act" if f % 2 else "dve"))
                P.barrier()
                rmsnorm_to_nT("gffn")
                P.barrier()
                cv = Carver(arenaO, ARO)
                f_a = cv.get(128, [Tp + 8], F32)
                f_b = cv.get(128, [Tp], F32)
                b_e = cv.get(128, [Tp], BF16)
                b_f = cv.get(128, [Tp], BF16)
                actb = []
                for i_ in range(GF):
                    if i_ == 2 and 4096 >= Tp:
                        actb.append(rtab_all[:, :, :].rearrange("p a b -> p (a b)").bitcast(BF16)[:, 0:Tp])
                    elif i_ == 3 and KC * 512 >= Tp:
                        actb.append(sq_b[:, :, :].rearrange("p a b -> p (a b)")[:, 0:Tp])
                    else:
                        actb.append(cv.get(128, [Tp], BF16))
                akeys = [f"actb{i}" for i in range(GF)]
                MSET(f_a[:, 0:2], 0.0, ["f_a0"])
                wu_ = w_up_d[l].rearrange("(kc p) n -> p kc n", p=128)
                for g0 in range(0, NF, GF):
                    ng = min(GF, NF - g0)
                    for jj in range(ng):
                        j = g0 + jj
                        wv, wkey = wnext(KC * 256, KC)
                        DMA(wv[:, :, 0:128], wu_[:, :, j * 128:(j + 1) * 128], (), [wkey], eng="pool")
                        DMA(wv[:, :, 128:256], wu_[:, :, cfg.DFF + j * 128:cfg.DFF + (j + 1) * 128], (), [wkey], eng="pool")
                        cw = lambda i, j=j: lp("convf", j * 3 + i)
                        for bi, (t0, w) in enumerate(blocks):
                            pg, pu = nps(), nps()
                            proj_block(wv, wkey, 0, 128, t0, w, pg)
                            proj_block(wv, wkey, 128, 128, t0, w, pu)
                            CP(f_a[:, 2 + t0:2 + t0 + w], ps[pg][:, 0:w], [f"ps{pg}"], [f"f_a{bi}"], eng="act")
                            CP(b_e[:, t0:t0 + w], ps[pu][:, 0:w], [f"ps{pu}"], [f"b_e{bi}"], eng="act")
                            rk = [f"f_a{bi}"] + ([f"f_a{bi - 1}"] if bi else [])
                            fk = f"f_b{bi}"
                            TS(f_b[:, t0:t0 + w], f_a[:, 2 + t0:2 + t0 + w], cw(2), None, ALU.mult, None, rk + ["lpt"], [fk])
                            for i in (1, 0):
                                STT(f_b[:, t0:t0 + w], f_a[:, i + t0:i + t0 + w], cw(i), f_b[:, t0:t0 + w], ALU.mult, ALU.add, rk + [fk, "lpt"], [fk])
                            ACT(b_f[:, t0:t0 + w], f_b[:, t0:t0 + w], AF.Silu, [fk], [f"b_f{bi}"])
                            TT(actb[jj][:, t0:t0 + w], b_f[:, t0:t0 + w], b_e[:, t0:t0 + w], ALU.mult, [f"b_f{bi}", f"b_e{bi}"], [f"{akeys[jj]}_{bi}"])
                    for f in range(KC):
                        wv, wkey = wload(w_down_d[l].rearrange("(kc p) n -> p kc n", p=128)[:, g0:g0 + ng, f * 128:(f + 1) * 128], [128, ng, 128])
                        for bi, (t0, w) in enumerate(blocks):
                            pi = nps()
                            for jj in range(ng):
                                MM(ps[pi][:, 0:w], wv[:, jj, :], actb[jj][:, t0:t0 + w], jj == 0, jj == ng - 1, [wkey, f"{akeys[jj]}_{bi}"], [f"ps{pi}"])
                            TT(hT[:, f, t0:t0 + w], hT[:, f, t0:t0 + w], ps[pi][:, 0:w], ALU.add, [f"hT{f}_{bi}", f"ps{pi}"], [f"hT{f}_{bi}"])
                P.barrier()

            P.barrier()
            xin = Carver(arenaO, ARO).get(128, [D], F32)
            for r in range(NQT):
                c0 = 64 + 128 * r
                for kc in range(KC):
                    pi = nps()
                    TR(ps[pi][:, 0:128], hT[:, kc, c0:c0 + 128], identF[:, :], ["hT", "cst"], [f"ps{pi}"])
                    CP(xin[:, kc * 128:(kc + 1) * 128], ps[pi][:, 0:128], [f"ps{pi}"], ["xin"], eng=("act" if kc % 2 else "dve"))
                final.append(DMA(y_d[sq, 128 * r:128 * (r + 1), :], xin[:, :], ["xin"], ["y"]))
        P.emit(final)
    return nc


_CACHE = {}


def make_in_maps(cfg, ncores, x, meta_tokens, norm_mix, w_in, conv_a, a_log, dt_bias, a_out_norm, q_norm, k_norm,
                 kidx_norm, w_branch_a, w_branch_b, w_gate, b_gate, w_out, norm_ffn, w_up, conv_ffn, w_down):
    f = lambda a: np.ascontiguousarray(np.asarray(a, dtype=np.float32))
    consts = host_consts(cfg)
    lps = np.stack([host_lparams(cfg, l, f(norm_mix), f(norm_ffn), f(conv_a), f(conv_ffn), f(b_gate), f(a_out_norm), f(q_norm),
                                 f(k_norm), f(kidx_norm), f(a_log), f(dt_bias)) for l in range(cfg.DEPTH)])
    x = f(x)
    shared = {"meta": f(meta_tokens), "consts": consts, "lparams": lps, "w_in": f(w_in), "w_branch_a": f(w_branch_a),
              "w_branch_b": f(w_branch_b), "w_gate": f(w_gate), "w_out": f(w_out), "w_up": f(w_up), "w_down": f(w_down)}
    maps = []
    for c in range(ncores):
        m = dict(shared)
        m["x"] = np.ascontiguousarray(x[c * cfg.NSEQ:(c + 1) * cfg.NSEQ])
        maps.append(m)
    return maps


def kernel(**inputs):
    cfg = Cfg()
    ncores = 8
    if "nc" not in _CACHE:
        _CACHE["nc"] = build(cfg)
    nc = _CACHE["nc"]
    maps = make_in_maps(cfg, ncores, **inputs)
    res = run_bass_kernel_spmd(nc, maps, core_ids=list(range(ncores)))
    return np.concatenate([np.asarray(r["y"]) for r in res.results], axis=0).astype(np.float32)
```

```python
import contextlib
import math
import numpy as np
import concourse.bass as bass
import concourse.mybir as mybir
from concourse.bass_utils import run_bass_kernel_spmd

F32 = mybir.dt.float32
BF16 = mybir.dt.bfloat16
ALU = mybir.AluOpType
AF = mybir.ActivationFunctionType

ENGS = ("pe", "act", "dve", "pool", "sp")
DMA_POOL = 16
EPOCH = 12000
EPS = 1e-6
NEGB = -30000.0


class Op:
    __slots__ = ("eng", "idx", "fn", "dma", "deps", "signal", "sem", "val")

    def __init__(self, eng, idx, fn, dma):
        self.eng, self.idx, self.fn, self.dma = eng, idx, fn, dma
        self.deps = ()
        self.signal = False
        self.sem = None
        self.val = 0


class Prog:
    def __init__(self, nc):
        self.nc = nc
        self.ops = {e: [] for e in ENGS}
        self.lastw = {}
        self.readers = {}
        self.fence = {}
        self.dma_since = []

    def barrier(self):
        f = list(self.dma_since)
        for e in ENGS:
            for o in reversed(self.ops[e]):
                if not o.dma:
                    f.append(o)
                    break
        self.dma_since = []
        for e in ENGS:
            self.fence[e] = list(self.fence.get(e, [])) + f

    def op(self, eng, fn, reads=(), writes=(), dma=False):
        o = Op(eng, len(self.ops[eng]), fn, dma)
        deps = {}
        if dma:
            self.dma_since.append(o)
        if self.fence.get(eng):
            for d in self.fence[eng]:
                if d.eng == eng and not d.dma and eng == "pe":
                    continue
                deps[(d.eng, d.idx)] = d
            self.fence[eng] = []

        def add(d, raw):
            if d is None:
                return
            if d.eng == eng and not d.dma:
                if eng == "pe":
                    return
            deps[(d.eng, d.idx)] = d

        for k in reads:
            for wr in self.lastw.get(k, ()):
                add(wr, True)
        for k in writes:
            rd = self.readers.get(k, {})
            prev = self.lastw.get(k, ())
            group = dma and prev and all(p.dma for p in prev) and not rd
            if not group:
                for wr in prev:
                    add(wr, False)
            for r in rd.values():
                if isinstance(r, list):
                    for r_ in r:
                        add(r_, None)
                else:
                    add(r, None)
        o.deps = tuple(deps.values())
        for d in o.deps:
            d.signal = True
        for k in reads:
            rd = self.readers.setdefault(k, {})
            if dma:
                rd.setdefault("dma", []).append(o)
            else:
                rd[eng] = o
        for k in writes:
            prev = self.lastw.get(k, ())
            if dma and prev and all(p.dma for p in prev) and not self.readers.get(k):
                self.lastw[k] = tuple(prev) + (o,)
            else:
                self.lastw[k] = (o,)
            self.readers[k] = {}
        self.ops[eng].append(o)
        return o

    def emit(self, final_ops=()):
        nc = self.nc
        for o in final_ops:
            o.signal = True
        n_sems = {}
        for e in ENGS:
            cnt = 0
            for o in self.ops[e]:
                if o.signal and not o.dma:
                    o.sem = (e, cnt // EPOCH)
                    o.val = cnt % EPOCH + 1
                    cnt += 1
            n_sems[e] = (cnt + EPOCH - 1) // EPOCH
        dma_sems = []
        for e in ENGS:
            dcnt = 0
            dvals = [0] * DMA_POOL
            for o in self.ops[e]:
                if o.signal and o.dma:
                    j = dcnt % DMA_POOL
                    dcnt += 1
                    dvals[j] += 16
                    o.sem = ("dma" + e, j)
                    o.val = dvals[j]
            dma_sems += [("dma" + e, j) for j in range(min(DMA_POOL, dcnt))]
        with contextlib.ExitStack() as st:
            sems = {}
            for e in ENGS:
                for ep in range(n_sems[e]):
                    sems[(e, ep)] = st.enter_context(nc.semaphore(f"s_{e}_{ep}"))
            for k in dma_sems:
                sems[k] = st.enter_context(nc.semaphore(f"s_{k[0]}_{k[1]}"))
            block = st.enter_context(nc.Block())

            def run(e, eng):
                waited = {}
                for o in self.ops[e]:
                    need = {}
                    for d in o.deps:
                        if d.val > need.get(d.sem, 0):
                            need[d.sem] = d.val
                    for sm, v in need.items():
                        if waited.get(sm, 0) >= v:
                            continue
                        eng.wait_ge(sems[sm], v)
                        waited[sm] = v
                    if o.signal and o.dma and o.val > 16 and waited.get(o.sem, 0) < o.val - 16:
                        eng.wait_ge(sems[o.sem], o.val - 16)
                        waited[o.sem] = o.val - 16
                    ins = o.fn(eng)
                    if o.signal:
                        ins.then_inc(sems[o.sem], 16 if o.dma else 1)
                if e == "sp":
                    for d in final_ops:
                        if waited.get(d.sem, 0) >= d.val:
                            continue
                        eng.wait_ge(sems[d.sem], d.val)
                        waited[d.sem] = d.val

            @block.sync
            def _(eng):
                run("sp", eng)

            @block.tensor
            def _(eng):
                run("pe", eng)

            @block.scalar
            def _(eng):
                run("act", eng)

            @block.vector
            def _(eng):
                run("dve", eng)

            @block.gpsimd
            def _(eng):
                run("pool", eng)


class Cfg:
    def __init__(self, D=1024, NCH=32, DFF=2816, NSEQ=4, DEPTH=2, TOPK=256, NIT=16, NSEG=0, NBT=8, GF=4, NWB=3, ARENA_MIN=0, PACK=0, POOLCONV=0, RATIO=2):
        self.D, self.NCH, self.DFF, self.NSEQ, self.DEPTH, self.TOPK, self.NIT = D, NCH, DFF, NSEQ, DEPTH, TOPK, NIT
        self.NSEG, self.NBT, self.GF, self.NWB, self.ARENA_MIN, self.PACK = NSEG, NBT, GF, NWB, ARENA_MIN, PACK
        self.POOLCONV = POOLCONV
        self.RATIO = RATIO
        self.KC = D // 128
        self.S = 64 * NCH
        self.Tp = 64 * (NCH + 1)
        self.NF = DFF // 128
        self.INC = 3408
        o = 0
        self.c = {}
        for name, w in (("ident", 128), ("r128", 128), ("r64", 128), ("blk64", 128), ("i4", 512),
                        ("mls", 64), ("mus", 64), ("mui", 64), ("pq", 4), ("frow", 32), ("i2", 64)):
            self.c[name] = (o, w)
            o += w
        self.NSMALL = o
        for name, w in (("cosA", self.Tp), ("sinA", self.Tp), ("cosI", self.Tp), ("sinI", self.Tp), ("rst", self.Tp)):
            self.c[name] = (o, w)
            o += w
        self.NCONST = o
        o = 0
        self.p = {}
        for name, w in (("gmix", self.KC), ("gffn", self.KC), ("conva", 48), ("convf", 3 * self.NF),
                        ("bgate", 2 * self.KC), ("aon", 1), ("qn", 1), ("kn", 1), ("kin", 1), ("alog", 1), ("dtb", 1)):
            self.p[name] = (o, w)
            o += w
        self.NP = o


def host_consts(cfg):
    C = np.zeros((128, cfg.NCONST), np.float32)

    def put(name, arr):
        o, w = cfg.c[name]
        C[: arr.shape[0], o:o + arr.shape[1]] = arr

    I = np.eye(128, dtype=np.float32)
    put("ident", I)
    R = np.zeros((128, 128), np.float32)
    for i in range(128):
        R[i, (i + 64) % 128] = 1
    put("r128", R)
    R2 = np.zeros((128, 128), np.float32)
    for i in range(128):
        b, j = divmod(i, 64)
        R2[i, b * 64 + (j + 32) % 64] = 1
    put("r64", R2)
    B = np.zeros((128, 128), np.float32)
    B[:64, :64] = 1
    B[64:, 64:] = 1
    put("blk64", B)
    put("i4", np.concatenate([I] * 4, axis=1))
    p = np.arange(64)[:, None]
    j = np.arange(64)[None, :]
    NEG = -10000.0
    put("mls", np.where(p > j, 0.0, NEG).astype(np.float32))
    put("mus", np.where(j > p, 0.0, NEG).astype(np.float32))
    put("mui", np.where(j >= p, 0.0, NEG).astype(np.float32))
    pq = np.zeros((128, 4), np.float32)
    pq[0] = [1, 0, 0, 1]
    pq[1] = [0, 1, -1, 0]
    put("pq", pq)
    put("i2", np.concatenate([np.eye(64, dtype=np.float32)] * 2, axis=0))
    put("frow", np.tile((2.0 ** -(np.arange(32, dtype=np.float64) + 1)).astype(np.float32)[None, :], (128, 1)))
    Tp = cfg.Tp
    pos = (np.arange(Tp) - 48).astype(np.float32)
    pos[:48] = 0

    def tables(dim, reps):
        inv = (1.0 / (10000.0 ** (np.arange(0, dim, 2, dtype=np.float32) / np.float32(dim)))).astype(np.float32)
        ang = pos[:, None] * inv[None, :]
        c, s = np.cos(ang).astype(np.float32), np.sin(ang).astype(np.float32)
        cf = np.concatenate([c, c], axis=1).T
        sf = np.concatenate([-s, s], axis=1).T
        return np.tile(cf, (reps, 1)), np.tile(sf, (reps, 1))

    ca, sa = tables(128, 1)
    ci, si = tables(64, 2)
    put("cosA", ca); put("sinA", sa); put("cosI", ci); put("sinI", si)
    rst = np.ones((128, Tp), np.float32)
    rst[:, ::64] = 0
    put("rst", rst)
    return C


def host_lparams(cfg, l, norm_mix, norm_ffn, conv_a, conv_ffn, b_gate, a_out_norm, q_norm, k_norm, kidx_norm, a_log, dt_bias):
    P = np.zeros((128, cfg.NP), np.float32)

    def put(name, arr):
        o, w = cfg.p[name]
        P[: arr.shape[0], o:o + arr.shape[1]] = arr

    put("gmix", norm_mix[l].reshape(cfg.KC, 128).T)
    put("gffn", norm_ffn[l].reshape(cfg.KC, 128).T)
    put("conva", conv_a[l].reshape(4, 12, 128).transpose(2, 1, 0).reshape(128, 48))
    put("convf", conv_ffn[l].reshape(3, cfg.NF, 128).transpose(2, 1, 0).reshape(128, 3 * cfg.NF))
    put("bgate", b_gate[l].reshape(2 * cfg.KC, 128).T)
    put("aon", a_out_norm[l].reshape(128, 1))
    put("qn", q_norm[l].reshape(128, 1))
    put("kn", k_norm[l].reshape(128, 1))
    put("kin", np.concatenate([kidx_norm[l], kidx_norm[l]]).reshape(128, 1))
    put("alog", a_log[l].reshape(4, 1))
    put("dtb", dt_bias[l].reshape(4, 1))
    return P


def build(cfg):
    nc = bass.Bass("TRN2", target_bir_lowering=False)
    D, KC, NCH, S, Tp, NF, NSEQ, DEPTH, TOPK = cfg.D, cfg.KC, cfg.NCH, cfg.S, cfg.Tp, cfg.NF, cfg.NSEQ, cfg.DEPTH, cfg.TOPK
    NCK = NCH + 1
    NBT, GF, NWB = cfg.NBT, cfg.GF, cfg.NWB
    x_d = nc.dram_tensor("x", [NSEQ, S, D], F32, kind="ExternalInput").ap()
    meta_d = nc.dram_tensor("meta", [16, D], F32, kind="ExternalInput").ap()
    const_d = nc.dram_tensor("consts", [128, cfg.NCONST], F32, kind="ExternalInput").ap()
    lp_d = nc.dram_tensor("lparams", [DEPTH, 128, cfg.NP], F32, kind="ExternalInput").ap()
    w_in_d = nc.dram_tensor("w_in", [DEPTH, D, cfg.INC], F32, kind="ExternalInput").ap()
    w_ba_d = nc.dram_tensor("w_branch_a", [DEPTH, 512, D], F32, kind="ExternalInput").ap()
    w_bb_d = nc.dram_tensor("w_branch_b", [DEPTH, 512, D], F32, kind="ExternalInput").ap()
    w_gate_d = nc.dram_tensor("w_gate", [DEPTH, D, 2 * D], F32, kind="ExternalInput").ap()
    w_out_d = nc.dram_tensor("w_out", [DEPTH, D, D], F32, kind="ExternalInput").ap()
    w_up_d = nc.dram_tensor("w_up", [DEPTH, D, 2 * cfg.DFF], F32, kind="ExternalInput").ap()
    w_down_d = nc.dram_tensor("w_down", [DEPTH, cfg.DFF, D], F32, kind="ExternalInput").ap()
    y_d = nc.dram_tensor("y", [NSEQ, S, D], F32, kind="ExternalOutput").ap()
    gq_d = nc.dram_tensor("gq_scr", [16, 128, Tp], BF16).ap()
    grow_d = nc.dram_tensor("grow_scr", [8, Tp], F32).ap()
    hsp_d = nc.dram_tensor("hsp_scr", [128, KC * Tp], F32).ap()

    def tab_d(name):
        o, w = cfg.c[name]
        return const_d[:, o:o + w]

    segs = []
    if cfg.NSEG == 0:
        segs.append((0, 1))
        a = 1
        while a < NCK:
            segs.append((a, min(NCK, a + cfg.NBT)))
            a += cfg.NBT
    else:
        base, rem = divmod(NCK, cfg.NSEG)
        a = 0
        for i in range(cfg.NSEG):
            n_ = base + (1 if i < rem else 0)
            if n_:
                segs.append((a, a + n_))
            a += n_
    NCKm = max(b - a for a, b in segs)
    Tsm = 64 * NCKm

    P = Prog(nc)
    st = contextlib.ExitStack()
    with st:
        def sb(name, shape, dt):
            return st.enter_context(nc.sbuf_tensor(name, shape, dt))

        cst = sb("cst", [128, cfg.NSMALL], F32)
        lpt = sb("lpt", [128, cfg.NP], F32)
        ident_b = sb("ident_b", [128, 128], BF16)
        ones_b = sb("ones_b", [128, 128], BF16)
        r128_b = sb("r128_b", [128, 128], BF16)
        r64_b = sb("r64_b", [128, 128], BF16)
        blk64_b = sb("blk64_b", [128, 128], BF16)
        i4_b = sb("i4_b", [128, 512], BF16)
        i4n_b = sb("i4n_b", [128, 512], BF16)
        HWD = KC * Tp
        ARH = max(HWD, cfg.ARENA_MIN)
        ARO = max(4 * Tp + 64, cfg.ARENA_MIN)
        arenaH = sb("arenaH", [128, ARH], F32)
        arenaN = sb("arenaN", [128, HWD // 2], F32)
        arenaO = sb("arenaO", [128, ARO], F32)
        hT = arenaH[:, 0:HWD].rearrange("p (k t) -> p k t", k=KC)
        nT = arenaN[:, :].bitcast(BF16).rearrange("p (k t) -> p k t", k=KC)
        oab = arenaO[:, 0:4 * Tp].bitcast(BF16).rearrange("p (k t) -> p k t", k=8)
        wbuf = [sb(f"wbuf{i}", [128, 4096], BF16) for i in range(NWB)]
        wab = sb("wab", [128, KC, 64], BF16)
        sq_b = sb("sq_b", [128, KC, 512], BF16)
        rs_f = [sb(f"rs_f{i}", [128, 512], F32) for i in range(2)]
        t_f = [sb(f"t_f{i}", [128, 512], F32) for i in range(2)]
        t_b = [sb(f"t_b{i}", [128, 512], BF16) for i in range(2)]
        rtab_all = sb("rtab_all", [128, 4, 512], F32)
        rtab = [rtab_all[:, 0:2, :], rtab_all[:, 2:4, :]]
        sml = sb("sml", [128, 16], F32)
        bis = sb("bis", [128, 8], F32)
        dfs = sb("dfs", [128, 32], F32)
        bisA = sb("bisA", [128, 8], F32)
        dfsA = sb("dfsA", [128, 32], F32)
        ps = [st.enter_context(nc.psum_tensor(f"ps{i}", [128, 512], F32)) for i in range(7)]
        psb = st.enter_context(nc.psum_tensor("psb", [128, 1024], BF16))
        NQT = S // 128

        class Carver:
            def __init__(self, arena, nwords, o0=0):
                self.a, self.n, self.o = arena, nwords, o0

            def get(self, rows, free, dt):
                n = 1
                for f_ in free:
                    n *= f_
                words = n if dt == F32 else (n + 1) // 2
                words = (words + 7) // 8 * 8
                assert self.o + words <= self.n, (self.o, words, self.n)
                v = self.a[0:rows, self.o:self.o + words]
                self.o += words
                if dt == BF16:
                    v = v.bitcast(BF16)
                v = v[:, 0:n]
                if len(free) == 2:
                    v = v.rearrange("p (a b) -> p a b", a=free[0])
                return v

        def cs(name, rows=128):
            o, w = cfg.c[name]
            assert o + w <= cfg.NSMALL
            return cst[0:rows, o:o + w]

        def lp(name, j=0, rows=128):
            o, w = cfg.p[name]
            return lpt[0:rows, o + j:o + j + 1]

        def MM(out, lhsT, rhs, start, stop, r, w):
            P.op("pe", lambda e: e.matmul(out, lhsT=lhsT, rhs=rhs, start=start, stop=stop), r, w)

        def TR(out, in_, idn, r, w):
            P.op("pe", lambda e: e.transpose(out, in_, idn), r, w)

        def ACT(out, in_, func, r, w, bias=None, scale=None, accum=None):
            kw = {}
            if bias is not None:
                kw["bias"] = bias
            if scale is not None:
                kw["scale"] = scale
            if accum is not None:
                kw["accum_out"] = accum
            P.op("act", lambda e: e.activation(out=out, in_=in_, func=func, **kw), r, w)

        def TT(out, a, b, op, r, w, eng="dve"):
            P.op(eng, lambda e: e.tensor_tensor(out=out, in0=a, in1=b, op=op), r, w)

        def TS(out, a, s1, s2, op0, op1, r, w, accum=None, eng="dve"):
            kw = {}
            if op1 is not None:
                kw["op1"] = op1
            if accum is not None:
                kw["accum_out"] = accum
            P.op(eng, lambda e: e.tensor_scalar(out=out, in0=a, scalar1=s1, scalar2=s2, op0=op0, **kw), r, w)

        def STT(out, a, s, b, op0, op1, r, w, eng="dve"):
            P.op(eng, lambda e: e.scalar_tensor_tensor(out=out, in0=a, scalar=s, in1=b, op0=op0, op1=op1), r, w)

        def CP(out, in_, r, w, eng="dve"):
            if eng == "act":
                P.op("act", lambda e: e.copy(out=out, in_=in_), r, w)
            else:
                P.op(eng, lambda e: e.tensor_copy(out=out, in_=in_), r, w)

        def MSET(ap, v, w, eng="dve"):
            P.op(eng, lambda e: e.memset(ap, v), (), w)

        def DMA(out, in_, r, w, eng="sp"):
            return P.op(eng, lambda e: e.dma_start(out=out, in_=in_), r, w, dma=True)

        blocks = [(t0, min(512, Tp - t0)) for t0 in range(0, Tp, 512)]
        psrr = [0]

        psmod = [5]

        def nps():
            psrr[0] = (psrr[0] + 1) % psmod[0]
            return psrr[0]

        wrr = [0]

        def wnext(nfree, a=None):
            i = wrr[0] % NWB
            wrr[0] += 1
            v = wbuf[i][:, 0:nfree]
            if a is not None:
                v = v.rearrange("p (a b) -> p a b", a=a)
            return v, f"wbuf{i}"

        def wload(src_ap, shape_view):
            n = 1
            for s_ in shape_view[1:]:
                n *= s_
            v, key = wnext(n, shape_view[1] if len(shape_view) == 3 else None)
            DMA(v, src_ap, (), [key], eng="pool")
            return v, key

        def wk(wd, l, c0, ncols):
            return wd[l].rearrange("(kc p) n -> p kc n", p=128)[:, :, c0:c0 + ncols]

        def rstd_from_ps(psi, w, rsb, rskey, inv_n, rows=128):
            ACT(rsb[0:rows, 0:w], ps[psi][0:rows, 0:w], AF.Ln, [f"ps{psi}", "sml"], [rskey], bias=EPS_AP[0:rows, 0:1], scale=inv_n)
            ACT(rsb[0:rows, 0:w], rsb[0:rows, 0:w], AF.Exp, [rskey], [rskey], scale=-0.5)

        DMA(cst[:], const_d[:, 0:cfg.NSMALL], (), ["cst"])
        EPS_AP = sml[:, 15:16]
        MSET(sml[:], 0.0, ["sml"])
        MSET(sml[:, 15:16], EPS, ["sml"])
        MSET(ones_b[:], 1.0, ["ones_b"])
        CP(ident_b[:], cs("ident"), ["cst"], ["ident_b"])
        CP(r128_b[:], cs("r128"), ["cst"], ["r128_b"])
        CP(r64_b[:], cs("r64"), ["cst"], ["r64_b"])
        CP(blk64_b[:], cs("blk64"), ["cst"], ["blk64_b"])
        CP(i4_b[:], cs("i4"), ["cst"], ["i4_b"])
        TS(i4n_b[:], cs("i4"), -1.0, None, ALU.mult, None, ["cst"], ["i4n_b"])
        MSET(wab[:], 0.0, ["wab"])
        identF = cs("ident")
        MSET(t_b[0][:, :], 0.0, ["t_b0"])
        for ft in range(16):
            DMA(gq_d[ft][:, 0:48], t_b[0][:, 0:48], ["t_b0"], [f"gq{ft}"])

        def rmsnorm_to_nT(gname):
            for bi, (t0, w) in enumerate(blocks):
                for kc in range(KC):
                    ACT(sq_b[:, kc, 0:w], hT[:, kc, t0:t0 + w], AF.Square, ["hT"], ["sq_b"])
                pi = nps()
                for kc in range(KC):
                    MM(ps[pi][:, 0:w], ones_b[:], sq_b[:, kc, 0:w], kc == 0, kc == KC - 1, ["ones_b", "sq_b"], [f"ps{pi}"])
                rsb = rs_f[bi % 2]
                rstd_from_ps(pi, w, rsb, f"rs_f{bi % 2}", 1.0 / D)
                for kc in range(KC):
                    STT(nT[:, kc, t0:t0 + w], hT[:, kc, t0:t0 + w], lp(gname, kc), rsb[:, 0:w], ALU.mult, ALU.mult,
                        ["hT", f"rs_f{bi % 2}", "lpt"], ["nT"])

        def proj_block(wv, wkey, col0, M, t0, w, pi, kcs=None, src=None, srckey="nT", srcoff=0):
            src = nT if src is None else src
            kcs = KC if kcs is None else kcs
            for kc in range(kcs):
                MM(ps[pi][0:M, 0:w], wv[:, kc, col0:col0 + M], src[:, srcoff + kc, t0:t0 + w], kc == 0, kc == kcs - 1,
                   [wkey, srckey], [f"ps{pi}"])

        final = []
        for sq in range(NSEQ):
            P.barrier()
            cvx_ = Carver(arenaO, ARO)
            xins_ = [cvx_.get(128, [D], F32) for _ in range(2)]
            MSET(hT[:, :, 0:48], 0.0, ["hT"])
            for r in range(-1, NQT):
                xin, xk = xins_[r % 2], f"xin{r % 2}"
                if r < 0:
                    DMA(xin[0:16, :], meta_d, (), [xk])
                    rows, c0 = 16, 48
                else:
                    DMA(xin[:, :], x_d[sq, 128 * r:128 * (r + 1), :], (), [xk])
                    rows, c0 = 128, 64 + 128 * r
                for kc in range(KC):
                    pi = nps()
                    TR(ps[pi][:, 0:rows], xin[0:rows, kc * 128:(kc + 1) * 128], identF[0:rows, 0:rows], [xk, "cst"], [f"ps{pi}"])
                    CP(hT[:, kc, c0:c0 + rows], ps[pi][:, 0:rows], [f"ps{pi}"], ["hT"], eng=("act" if kc % 2 else "dve"))

            for l in range(DEPTH):
                P.barrier()
                DMA(lpt[:], lp_d[l], (), ["lpt"])
                ACT(sml[0:4, 0:1], lp("alog", rows=4), AF.Exp, ["lpt"], ["sml"])
                TS(sml[0:4, 0:1], sml[0:4, 0:1], -1.0, None, ALU.mult, None, ["sml"], ["sml"])
                psmod[0] = 7
                DMA(hsp_d[:, :], arenaH[:, 0:HWD], ["hT"], ["hsp"])
                rmsnorm_to_nT("gmix")
                P.barrier()
                MSET(oab[:, :, 0:48], 0.0, ["oab"])
                cv = Carver(arenaH, ARH)
                f_aL = [cv.get(128, [Tp + 8], F32) for _ in range(2)]
                f_bL = [cv.get(128, [Tp], F32) for _ in range(2)]
                b_aL = [cv.get(128, [Tp], BF16) for _ in range(2)]
                b_bL = [cv.get(128, [Tp], BF16) for _ in range(2)]
                for i_ in range(2):
                    MSET(f_aL[i_][:, 0:4], 0.0, [f"f_a{i_}"])
                for grp in range(4):
                    wv, wkey = wload(wk(w_in_d, l, grp * 512, 512), [128, KC, 512])
                    for j in range(4):
                        ft = grp * 4 + j
                        pp = ft % 2
                        f_a, f_b, b_a, b_b = f_aL[pp], f_bL[pp], b_aL[pp], b_bL[pp]
                        ka, kb_, kba, kbb = f"f_a{pp}", f"f_b{pp}", f"b_a{pp}", f"b_b{pp}"
                        if grp < 3:
                            for (t0, w) in blocks:
                                pi = nps()
                                proj_block(wv, wkey, j * 128, 128, t0, w, pi)
                                CP(f_a[:, 3 + t0:3 + t0 + w], ps[pi][:, 0:w], [f"ps{pi}"], [ka], eng="act")
                            cw = lambda i, grp=grp, j=j: lp("conva", (grp * 4 + j) * 4 + i)
                            ceng = "pool" if (pp == 1 and cfg.POOLCONV) else "dve"
                            TS(f_b[:, :], f_a[:, 3:3 + Tp], cw(3), None, ALU.mult, None, [ka, "lpt"], [kb_], eng=ceng)
                            for i in (2, 1, 0):
                                STT(f_b[:, :], f_a[:, i:i + Tp], cw(i), f_b[:, :], ALU.mult, ALU.add, [ka, kb_, "lpt"], [kb_], eng=ceng)
                            if grp == 2:
                                ACT(b_a[:, :], f_b[:, :], AF.Silu, [kb_], [kba])
                            else:
                                ACT(f_b[:, :], f_b[:, :], AF.Silu, [kb_], [kb_])
                                ACT(b_b[:, :], f_b[:, :], AF.Square, [kb_], [kbb])
                                for bi, (t0, w) in enumerate(blocks):
                                    pi = nps()
                                    MM(ps[pi][:, 0:w], ones_b[:], b_b[:, t0:t0 + w], True, True, ["ones_b", kbb], [f"ps{pi}"])
                                    rsb = rs_f[bi % 2]
                                    rstd_from_ps(pi, w, rsb, f"rs_f{bi % 2}", 1.0)
                                    STT(b_a[:, t0:t0 + w], f_b[:, t0:t0 + w], (128.0 ** -0.5) if grp == 0 else 1.0, rsb[:, 0:w],
                                        ALU.mult, ALU.mult, [kb_, f"rs_f{bi % 2}"], [kba])
                        else:
                            for (t0, w) in blocks:
                                pi = nps()
                                proj_block(wv, wkey, j * 128, 128, t0, w, pi)
                                ACT(b_a[:, t0:t0 + w], ps[pi][:, 0:w], AF.Silu, [f"ps{pi}"], [kba])
                        DMA(gq_d[ft][:, 48:Tp], b_a[:, 48:Tp], [kba], [f"gq{ft}"])
                P.barrier()
                cv = Carver(arenaH, ARH)
                grs = cv.get(64, [Tp], F32)
                grs2 = cv.get(64, [Tp], F32)
                rstt = cv.get(4, [Tp], F32)
                DMA(rstt[:, :], tab_d("rst")[0:4, :], (), ["rstt"])
                wi = w_in_d[l].rearrange("(kc p) n -> p kc n", p=128)
                DMA(wab[:, :, 0:4], wi[:, :, 2048:2052], (), ["wab"], eng="pool")
                DMA(wab[:, :, 32:36], wi[:, :, 2052:2056], (), ["wab"], eng="pool")
                for (t0, w) in blocks:
                    pi = nps()
                    proj_block(wab, "wab", 0, 64, t0, w, pi)
                    ACT(grs[0:4, t0:t0 + w], ps[pi][0:4, 0:w], AF.Exp, [f"ps{pi}", "lpt"], ["grs"], bias=lp("dtb", rows=4))
                    ACT(grs[32:36, t0:t0 + w], ps[pi][32:36, 0:w], AF.Sigmoid, [f"ps{pi}"], ["grs"])
                ACT(grs[0:4, :], grs[0:4, :], AF.Ln, ["grs"], ["grs"], bias=1.0)
                TS(grs[0:4, :], grs[0:4, :], sml[0:4, 0:1], None, ALU.mult, None, ["grs", "sml"], ["grs"])
                MSET(grs[0:4, 0:48], 0.0, ["grs"])
                MSET(grs[32:36, 0:48], 0.0, ["grs"])
                P.op("dve", lambda e, grs=grs, grs2=grs2, rstt=rstt: e.tensor_tensor_scan(
                    out=grs2[0:4, :], data0=rstt[0:4, :], data1=grs[0:4, :], initial=0.0, op0=ALU.mult, op1=ALU.add),
                    ["grs", "rstt"], ["grs2"])
                DMA(grow_d[0:4, :], grs2[0:4, :], ["grs2"], ["grow"])
                DMA(grow_d[4:8, :], grs[32:36, :], ["grs"], ["grow"])
                P.barrier()

                psmod[0] = 4
                cv = Carver(arenaH, ARH)
                Gc = cv.get(128, [Tsm], F32)
                Bt = cv.get(128, [Tsm], F32)
                qT_, kT_, vT_, kbe, kbT, kdT = [cv.get(128, [Tsm], BF16) for _ in range(6)]
                BW = NBT * 64
                pqm = cv.get(2, [2, BW], F32)
                BWh = BW // (2 if cfg.PACK else 1)
                Lb = [cv.get(128, [BWh], F32) for _ in range(2)]
                Nb = [cv.get(128, [BWh], F32) for _ in range(2)]
                Dm = [cv.get(128, [BWh], F32) for _ in range(2)]
                Yb = [cv.get(128, [BWh], F32) for _ in range(2)]
                XT = cv.get(128, [BWh], BF16)
                kb_sb = cv.get(128, [NBT // (2 if cfg.PACK else 1), 128], BF16)
                vb_sb = cv.get(128, [NBT // (2 if cfg.PACK else 1), 128], BF16)
                S_f = cv.get(128, [128], F32)
                S_b = cv.get(128, [128], BF16)
                vn = [cv.get(128, [128], BF16) for _ in range(2)]
                E2 = [cv.get(128, [Tsm], F32) for _ in range(2)]
                oT2 = [cv.get(128, [Tsm], F32) for _ in range(2)]
                qeT2 = [cv.get(128, [Tsm], BF16) for _ in range(2)]
                zT2 = [cv.get(128, [Tsm], BF16) for _ in range(2)]
                NH = (NCKm + 1) // 2 if cfg.PACK else NCKm
                qkm2 = [cv.get(128, [NH, 64], BF16) for _ in range(2)]
                u2 = [cv.get(128, [NH, 128], F32) for _ in range(2)]
                wT2 = [cv.get(128, [NCKm * 64], BF16) for _ in range(2)]
                kd2 = [cv.get(128, [NH, 128], BF16) for _ in range(2)]
                pq = cs("pq", 2)
                items = [(h, si) for h in range(4) for si in range(len(segs))]

                HB = NBT // (2 if cfg.PACK else 1)
                BWc = HB * 64

                def place(j):
                    half, jj = divmod(j, HB)
                    return slice(64 * half, 64 * half + 64), slice(jj * 64, (jj + 1) * 64), half, jj

                def setup_gen(k):
                    h, si = items[k]
                    par = k % 2
                    cA, cB = segs[si]
                    nck = cB - cA
                    Ts = nck * 64
                    g0 = cA * 64
                    E, qeT, zT_, qkm, u_sb, wT_sb, kd_sb = E2[par], qeT2[par], zT2[par], qkm2[par], u2[par], wT2[par], kd2[par]
                    kE, kqe, kz, kqk, ku, kw, kkd = f"E{par}", f"qeT{par}", f"zT{par}", f"qkm{par}", f"u{par}", f"wT{par}", f"kd{par}"
                    DMA(qT_[:, 0:Ts], gq_d[h][:, g0:g0 + Ts], [f"gq{h}"], ["qT_"])
                    DMA(kT_[:, 0:Ts], gq_d[4 + h][:, g0:g0 + Ts], [f"gq{4 + h}"], ["kT_"])
                    DMA(vT_[:, 0:Ts], gq_d[8 + h][:, g0:g0 + Ts], [f"gq{8 + h}"], ["vT_"])
                    DMA(zT_[:, 0:Ts], gq_d[12 + h][:, g0:g0 + Ts], [f"gq{12 + h}"], [kz])
                    DMA(Gc[:, 0:Ts], grow_d[h:h + 1, g0:g0 + Ts].partition_broadcast(128), ["grow"], ["Gc"])
                    DMA(Bt[:, 0:Ts], grow_d[4 + h:5 + h, g0:g0 + Ts].partition_broadcast(128), ["grow"], ["Bt"])
                    yield
                    ACT(E[:, 0:Ts], Gc[:, 0:Ts], AF.Exp, ["Gc"], [kE])
                    TT(kbe[:, 0:Ts], kT_[:, 0:Ts], Bt[:, 0:Ts], ALU.mult, ["kT_", "Bt"], ["kbe"])
                    TT(kbT[:, 0:Ts], kbe[:, 0:Ts], E[:, 0:Ts], ALU.mult, ["kbe", kE], ["kbT"])
                    yield
                    TT(vT_[:, 0:Ts], vT_[:, 0:Ts], Bt[:, 0:Ts], ALU.mult, ["vT_", "Bt"], ["vT_"])
                    TT(qeT[:, 0:Ts], qT_[:, 0:Ts], E[:, 0:Ts], ALU.mult, ["qT_", kE], [kqe])
                    Gc3 = Gc[:, 0:Ts].rearrange("p (n c) -> p n c", c=64)
                    TT(Bt[:, 0:Ts].rearrange("p (n c) -> p n c", c=64), Gc3[:, :, 63:64].to_broadcast([128, nck, 64]), Gc3, ALU.subtract,
                       ["Gc", "Bt"], ["Bt"])
                    yield
                    ACT(Bt[:, 0:Ts], Bt[:, 0:Ts], AF.Exp, ["Bt"], ["Bt"])
                    TT(kdT[:, 0:Ts], kT_[:, 0:Ts], Bt[:, 0:Ts], ALU.mult, ["kT_", "Bt"], ["kdT"])
                    yield
                    for b0 in range(0, nck, NBT):
                        nb = min(NBT, nck - b0)
                        assert nb == NBT or nb <= HB, (nb, NBT)
                        rows = 128 if nb > HB else 64
                        ncl = min(nb, HB)
                        Wc = ncl * 64
                        W = nb * 64
                        b0h = b0 // 2 if cfg.PACK else b0
                        bc = slice(b0 * 64, b0 * 64 + W)
                        TS(pqm[:, 0, 0:W], Gc[0:2, bc], pq[:, 0:1], pq[:, 1:2], ALU.mult, ALU.add, ["Gc", "cst"], ["pqm"])
                        TS(pqm[:, 1, 0:W], Gc[0:2, bc], pq[:, 2:3], pq[:, 3:4], ALU.mult, ALU.add, ["Gc", "cst"], ["pqm"])
                        Pm, Qm = pqm[:, 0, :], pqm[:, 1, :]
                        assert not cfg.PACK
                        pgL, pgN, pgQ, pM = nps(), nps(), nps(), nps()
                        for j in range(nb):
                            ch = slice((b0 + j) * 64, (b0 + j + 1) * 64)
                            o = slice(j * 64, (j + 1) * 64)
                            MM(ps[pgL][0:64, o], kbe[:, ch], kT_[:, ch], True, True, ["kbe", "kT_"], [f"ps{pgL}"])
                            MM(ps[pgN][0:64, o], kT_[:, ch], kbe[:, ch], True, True, ["kbe", "kT_"], [f"ps{pgN}"])
                            MM(ps[pgQ][0:64, o], kT_[:, ch], qT_[:, ch], True, True, ["qT_", "kT_"], [f"ps{pgQ}"])
                            MM(ps[pM][0:64, o], Pm[:, o], Qm[:, o], True, True, ["pqm"], [f"ps{pM}"])
                        M3 = ps[pM][0:64, 0:Wc].rearrange("p (n c) -> p n c", c=64)

                        def mk(name):
                            return cs(name, 64).unsqueeze(1).to_broadcast([64, nb, 64])

                        def d3(i_):
                            return Dm[i_][0:64, 0:Wc].rearrange("p (n c) -> p n c", c=64)

                        STT(d3(0), M3, 1.0, mk("mls"), ALU.mult, ALU.add, [f"ps{pM}", "cst"], ["Dm0"])
                        ACT(Dm[0][0:64, 0:Wc], Dm[0][0:64, 0:Wc], AF.Exp, ["Dm0"], ["Dm0"])
                        STT(Lb[0][0:64, 0:Wc], ps[pgL][0:64, 0:Wc], -1.0, Dm[0][0:64, 0:Wc], ALU.mult, ALU.mult, [f"ps{pgL}", "Dm0"], ["Lb0"])
                        yield
                        STT(d3(1), M3, -1.0, mk("mus"), ALU.mult, ALU.add, [f"ps{pM}", "cst"], ["Dm1"])
                        ACT(Dm[1][0:64, 0:Wc], Dm[1][0:64, 0:Wc], AF.Exp, ["Dm1"], ["Dm1"])
                        STT(Nb[0][0:64, 0:Wc], ps[pgN][0:64, 0:Wc], -1.0, Dm[1][0:64, 0:Wc], ALU.mult, ALU.mult, [f"ps{pgN}", "Dm1"], ["Nb0"])
                        yield
                        STT(d3(0), M3, -1.0, mk("mui"), ALU.mult, ALU.add, [f"ps{pM}", "cst", "Dm0"], ["Dm0"])
                        ACT(Dm[0][0:64, 0:Wc], Dm[0][0:64, 0:Wc], AF.Exp, ["Dm0"], ["Dm0"])
                        TT(qkm[0:64, b0h:b0h + ncl, :], ps[pgQ][0:64, 0:Wc].rearrange("p (n c) -> p n c", c=64), d3(0), ALU.mult, [f"ps{pgQ}", "Dm0"], [kqk])
                        yield
                        TT(Yb[0][0:rows, 0:Wc].rearrange("p (n c) -> p n c", c=64), Nb[0][0:rows, 0:Wc].rearrange("p (n c) -> p n c", c=64),
                           cs("i2")[0:rows, :].unsqueeze(1).to_broadcast([rows, ncl, 64]), ALU.add, ["Nb0", "cst"], ["Yb0"])
                        cur = 0
                        for lev in range(1, 6):
                            a_, b_ = (lev - 1) % 2, lev % 2
                            pa, pb, py = nps(), nps(), nps()
                            for j in range(nb):
                                pr, o, _, _ = place(j)
                                MM(ps[pa][pr, o], Nb[a_][pr, o], Lb[a_][pr, o], True, True, [f"Nb{a_}", f"Lb{a_}"], [f"ps{pa}"])
                                if lev < 5:
                                    MM(ps[pb][pr, o], Lb[a_][pr, o], Nb[a_][pr, o], True, True, [f"Nb{a_}", f"Lb{a_}"], [f"ps{pb}"])
                            CP(Lb[b_][0:rows, 0:Wc], ps[pa][0:rows, 0:Wc], [f"ps{pa}"], [f"Lb{b_}"], eng="act")
                            if lev < 5:
                                CP(Nb[b_][0:rows, 0:Wc], ps[pb][0:rows, 0:Wc], [f"ps{pb}"], [f"Nb{b_}"], eng="dve")
                            yield
                            for j in range(nb):
                                pr, o, _, _ = place(j)
                                MM(ps[py][pr, o], Lb[b_][pr, o], Yb[cur][pr, o], True, True, [f"Lb{b_}", f"Yb{cur}"], [f"ps{py}"])
                            if lev < 5:
                                TT(Yb[1 - cur][0:rows, 0:Wc], ps[py][0:rows, 0:Wc], Yb[cur][0:rows, 0:Wc], ALU.add, [f"ps{py}", f"Yb{cur}"], [f"Yb{1 - cur}"])
                                cur = 1 - cur
                            else:
                                TT(XT[0:rows, 0:Wc], ps[py][0:rows, 0:Wc], Yb[cur][0:rows, 0:Wc], ALU.add, [f"ps{py}", f"Yb{cur}"], ["XT"])
                            yield
                        for (srcT, skey, dst, dkey, dsl) in ((kbT, "kbT", kb_sb, "kb_sb", None), (vT_, "vT_", vb_sb, "vb_sb", None),
                                                             (kdT, "kdT", kd_sb, kkd, b0h)):
                            for j in range(nb):
                                ch = slice((b0 + j) * 64, (b0 + j + 1) * 64)
                                pr, o, _, jj = place(j)
                                TR(psb[pr, jj * 128:(jj + 1) * 128], srcT[:, ch], ident_b[:], [skey, "ident_b"], ["psb"])
                            src3 = psb[0:rows, 0:ncl * 128].rearrange("p (n c) -> p n c", c=128)
                            if dsl is None:
                                CP(dst[0:rows, 0:ncl, :], src3, ["psb"], [dkey], eng="act")
                            else:
                                CP(dst[0:rows, b0h:b0h + ncl, :], src3, ["psb"], [dkey], eng="act")
                            yield
                        for q0_ in range(0, ncl, 4):
                            n4 = min(4, ncl - q0_)
                            pi = nps()
                            for j in range(nb):
                                pr, o, _, jj = place(j)
                                if q0_ <= jj < q0_ + n4:
                                    MM(ps[pi][pr, (jj - q0_) * 128:(jj - q0_ + 1) * 128], XT[pr, o], vb_sb[pr, jj, :], True, True, ["XT", "vb_sb"], [f"ps{pi}"])
                            CP(u_sb[0:rows, b0h + q0_:b0h + q0_ + n4, :], ps[pi][0:rows, 0:n4 * 128].rearrange("p (n c) -> p n c", c=128), [f"ps{pi}"], [ku])
                        pi = nps()
                        for j in range(nb):
                            pr, o, _, jj = place(j)
                            MM(ps[pi][:, j * 64:(j + 1) * 64], kb_sb[pr, jj, :], XT[pr, o], True, True, ["XT", "kb_sb"], [f"ps{pi}"])
                        CP(wT_sb[:, b0 * 64:b0 * 64 + W], ps[pi][:, 0:W], [f"ps{pi}"], [kw], eng="act")
                        yield

                def scan_gen(k):
                    h, si = items[k]
                    par = k % 2
                    cA, cB = segs[si]
                    nck = cB - cA
                    E, oT, qeT, qkm, u_sb, wT_sb, kd_sb = E2[par], oT2[par], qeT2[par], qkm2[par], u2[par], wT2[par], kd2[par]
                    kE, ko, kqe, kqk, ku, kw, kkd = f"E{par}", f"oT{par}", f"qeT{par}", f"qkm{par}", f"u{par}", f"wT{par}", f"kd{par}"
                    if si == 0:
                        MSET(S_f[:], 0.0, ["S_f"])
                        MSET(S_b[:], 0.0, ["S_b"])
                    for n in range(nck):
                        ch = slice(n * 64, (n + 1) * 64)
                        b0 = (n // NBT) * NBT
                        pr, _, _, jj = place(n - b0)
                        idx = (b0 // 2 if cfg.PACK else b0) + jj
                        pv, po, pss = 4, 5, 6
                        vnb = vn[n % 2]
                        vk = f"vn{n % 2}"
                        MM(ps[pv][pr, 0:128], wT_sb[:, ch], S_b[:], True, True, [kw, "S_b"], [f"ps{pv}"])
                        TT(vnb[pr, :], u_sb[pr, idx, :], ps[pv][pr, 0:128], ALU.subtract, [ku, f"ps{pv}"], [vk])
                        MM(ps[pss][:, 0:128], kd_sb[pr, idx, :], vnb[pr, :], True, True, [kkd, vk], [f"ps{pss}"])
                        MM(ps[po][:, 0:64], S_b[:], qeT[:, ch], True, False, ["S_b", kqe], [f"ps{po}"])
                        MM(ps[po][:, 0:64], vnb[pr, :], qkm[pr, idx, :], False, True, [vk, kqk], [f"ps{po}"])
                        STT(S_b[:], S_f[:], E[:, n * 64 + 63:n * 64 + 64], ps[pss][:, 0:128], ALU.mult, ALU.add, ["S_f", kE, f"ps{pss}"], ["S_b"])
                        STT(S_f[:], S_f[:], E[:, n * 64 + 63:n * 64 + 64], ps[pss][:, 0:128], ALU.mult, ALU.add, ["S_f", kE, f"ps{pss}"], ["S_f"])
                        CP(oT[:, ch], ps[po][:, 0:64], [f"ps{po}"], [ko], eng="act")
                        yield

                def post_norm(k):
                    h, si = items[k]
                    par = k % 2
                    cA, cB = segs[si]
                    Ts = (cB - cA) * 64
                    g0 = cA * 64
                    oT, zT_ = oT2[par], zT2[par]
                    ko, kz = f"oT{par}", f"zT{par}"
                    ACT(kbe[:, 0:Ts], oT[:, 0:Ts], AF.Square, [ko], ["kbe"])
                    for bi, t0 in enumerate(range(0, Ts, 512)):
                        w = min(512, Ts - t0)
                        pi = nps()
                        MM(ps[pi][:, 0:w], ones_b[:], kbe[:, t0:t0 + w], True, True, ["ones_b", "kbe"], [f"ps{pi}"])
                        rsb = rs_f[bi % 2]
                        rstd_from_ps(pi, w, rsb, f"rs_f{bi % 2}", 1.0 / 128)
                        tf = t_f[bi % 2]
                        STT(tf[:, 0:w], oT[:, t0:t0 + w], lp("aon"), rsb[:, 0:w], ALU.mult, ALU.mult, [ko, f"rs_f{bi % 2}", "lpt"], [f"t_f{bi % 2}"])
                        TT(oab[:, h, g0 + t0:g0 + t0 + w], tf[:, 0:w], zT_[:, t0:t0 + w], ALU.mult, [f"t_f{bi % 2}", kz], ["oab"])

                for _ in setup_gen(0):
                    pass
                for k in range(len(items)):
                    gs = scan_gen(k)
                    gn = setup_gen(k + 1) if k + 1 < len(items) else iter(())
                    alive_s, alive_n = True, True
                    while alive_s or alive_n:
                        for _ in range(cfg.RATIO):
                            if alive_n:
                                try:
                                    next(gn)
                                except StopIteration:
                                    alive_n = False
                        if alive_s:
                            try:
                                next(gs)
                            except StopIteration:
                                alive_s = False
                    post_norm(k)
                psmod[0] = 5
                P.barrier()

                cv = Carver(arenaH, ARH)
                qT4 = [cv.get(128, [Tp], BF16) for _ in range(4)]
                qk4 = ["qT4_0", "qT4_1", "qT4_2", "qT4_3"]
                iq4 = [cv.get(128, [Tp], BF16) for _ in range(4)]
                ik4 = ["iq4_0", "iq4_1", "iq4_2", "iq4_3"]
                kTd = cv.get(128, [Tp], BF16)
                ikT = cv.get(128, [Tp], BF16)
                v_tm = cv.get(128, [NQT, 128], BF16)
                v_me = cv.get(16, [128], BF16)
                iw_tm = cv.get(128, [NQT, 8], F32)
                sc = cv.get(128, [S], F32)
                nm = cv.get(128, [S], BF16)

                def rope_tile(wv, wkey, col0, dst, dkey, gain, ones_m, ones_key, inv_n, rot, rotkey, cname, sname):
                    for bi, (t0, w) in enumerate(blocks):
                        rt = rtab[bi % 2]
                        rk = f"rtab{bi % 2}"
                        DMA(rt[:, 0, 0:w], tab_d(cname)[:, t0:t0 + w], (), [rk])
                        DMA(rt[:, 1, 0:w], tab_d(sname)[:, t0:t0 + w], (), [rk])
                        pi = nps()
                        proj_block(wv, wkey, col0, 128, t0, w, pi)
                        tb = t_b[bi % 2]
                        tkey = f"t_b{bi % 2}"
                        if gain is not None:
                            ACT(sq_b[:, 0, 0:w], ps[pi][:, 0:w], AF.Square, [f"ps{pi}"], ["sq_b"])
                            p2 = nps()
                            MM(ps[p2][:, 0:w], ones_m[:], sq_b[:, 0, 0:w], True, True, [ones_key, "sq_b"], [f"ps{p2}"])
                            rsb = rs_f[bi % 2]
                            rstd_from_ps(p2, w, rsb, f"rs_f{bi % 2}", inv_n)
                            STT(tb[:, 0:w], ps[pi][:, 0:w], gain, rsb[:, 0:w], ALU.mult, ALU.mult, [f"ps{pi}", f"rs_f{bi % 2}", "lpt"], [tkey])
                        else:
                            CP(tb[:, 0:w], ps[pi][:, 0:w], [f"ps{pi}"], [tkey], eng="act")
                        p3 = nps()
                        MM(ps[p3][:, 0:w], rot[:], tb[:, 0:w], True, True, [rotkey, tkey], [f"ps{p3}"])
                        tf = t_f[bi % 2]
                        TT(tf[:, 0:w], ps[p3][:, 0:w], rt[:, 1, 0:w], ALU.mult, [f"ps{p3}", rk], [f"t_f{bi % 2}"])
                        rsb2 = rs_f[bi % 2]
                        TT(rsb2[:, 0:w], tb[:, 0:w], rt[:, 0, 0:w], ALU.mult, [tkey, rk], [f"rs_f{bi % 2}"])
                        TT(dst[:, t0:t0 + w], rsb2[:, 0:w], tf[:, 0:w], ALU.add, [f"rs_f{bi % 2}", f"t_f{bi % 2}"], [dkey])

                wv, wkey = wload(wk(w_in_d, l, 2056, 512), [128, KC, 512])
                for hh in range(4):
                    rope_tile(wv, wkey, hh * 128, qT4[hh], qk4[hh], lp("qn"), ones_b, "ones_b", 1.0 / 128, r128_b, "r128_b", "cosA", "sinA")
                wv, wkey = wload(wk(w_in_d, l, 2568, 256), [128, KC, 256])
                rope_tile(wv, wkey, 0, kTd, "kTd", lp("kn"), ones_b, "ones_b", 1.0 / 128, r128_b, "r128_b", "cosA", "sinA")
                for kt in range(-1, NQT):
                    c0, rows = (48, 16) if kt < 0 else (64 + 128 * kt, 128)
                    pi = nps()
                    for kc in range(KC):
                        MM(ps[pi][0:rows, 0:128], nT[:, kc, c0:c0 + rows], wv[:, kc, 128:256], kc == 0, kc == KC - 1, ["nT", wkey], [f"ps{pi}"])
                    if kt < 0:
                        CP(v_me[:, :], ps[pi][0:16, 0:128], [f"ps{pi}"], ["v_me"], eng="act")
                    else:
                        CP(v_tm[:, kt, :], ps[pi][:, 0:128], [f"ps{pi}"], ["v_tm"], eng="act")
                wv, wkey = wload(wk(w_in_d, l, 2824, 512), [128, KC, 512])
                for tl in range(4):
                    rope_tile(wv, wkey, tl * 128, iq4[tl], ik4[tl], None, None, None, None, r64_b, "r64_b", "cosI", "sinI")
                wv, wkey = wnext(KC * 136, KC)
                DMA(wv[:, :, 0:64], wi[:, :, 3336:3400], (), [wkey], eng="pool")
                DMA(wv[:, :, 64:128], wi[:, :, 3336:3400], (), [wkey], eng="pool")
                DMA(wv[:, :, 128:136], wi[:, :, 3400:3408], (), [wkey], eng="pool")
                rope_tile(wv, wkey, 0, ikT, "ikT", lp("kin"), blk64_b, "blk64_b", 1.0 / 64, r64_b, "r64_b", "cosI", "sinI")
                for qt in range(NQT):
                    c0 = 64 + 128 * qt
                    pi = nps()
                    for kc in range(KC):
                        MM(ps[pi][:, 0:8], nT[:, kc, c0:c0 + 128], wv[:, kc, 128:136], kc == 0, kc == KC - 1, ["nT", wkey], [f"ps{pi}"])
                    TS(iw_tm[:, qt, :], ps[pi][:, 0:8], (8.0 ** -0.5) * (64.0 ** -0.5), None, ALU.mult, None, [f"ps{pi}"], ["iw_tm"])

                att_scale = 128.0 ** -0.5
                sc2 = cv.get(128, [S], F32)
                scs = [sc, sc2]
                junk = sq_b[:, :, :].rearrange("p a b -> p (a b)")

                def S1(qt):
                    q0 = 64 + 128 * qt
                    Sr = 128 * (qt + 1)
                    scb, sk = scs[qt % 2], f"sc{qt % 2}"
                    for kb0 in range(0, Sr, 512):
                        w = min(512, Sr - kb0)
                        for ih in range(8):
                            tl, hf = divmod(ih, 2)
                            pr = slice(64 * hf, 64 * hf + 64)
                            pi = nps()
                            MM(ps[pi][:, 0:w], iq4[tl][pr, q0:q0 + 128], ikT[pr, 64 + kb0:64 + kb0 + w], True, True,
                               [ik4[tl], "ikT"], [f"ps{pi}"])
                            tf = t_f[ih % 2]
                            ACT(tf[:, 0:w], ps[pi][:, 0:w], AF.Relu, [f"ps{pi}"], [f"t_f{ih % 2}"])
                            if ih == 0:
                                TS(scb[:, kb0:kb0 + w], tf[:, 0:w], iw_tm[:, qt, 0:1], None, ALU.mult, None, [f"t_f{ih % 2}", "iw_tm"], [sk])
                            else:
                                STT(scb[:, kb0:kb0 + w], tf[:, 0:w], iw_tm[:, qt, ih:ih + 1], scb[:, kb0:kb0 + w], ALU.mult, ALU.add,
                                    [f"t_f{ih % 2}", "iw_tm", sk], [sk])

                junkA = rtab_all[:, :, :].rearrange("p a b -> p (a b)").bitcast(BF16)
                nmA = junkA[:, 2048:4096]
                nm_neg = {}

                def S2(qt):
                    Sr = 128 * (qt + 1)
                    scb, sk = scs[qt % 2], f"sc{qt % 2}"
                    NIT = cfg.NIT
                    on_act = (qt % 2 == 1) and Sr <= 2048
                    B, bk, DF, dk = (bisA, "bisA", dfsA, "dfsA") if on_act else (bis, "bis", dfs, "dfs")
                    if Sr > TOPK:
                        P.op("dve", lambda e: e.tensor_reduce(out=B[:, 0:1], in_=scb[:, 0:Sr], axis=mybir.AxisListType.X, op=ALU.min), [sk], [bk])
                        P.op("dve", lambda e: e.tensor_reduce(out=B[:, 1:2], in_=scb[:, 0:Sr], axis=mybir.AxisListType.X, op=ALU.max), [sk], [bk])
                        TT(B[:, 2:3], B[:, 1:2], B[:, 0:1], ALU.subtract, [bk], [bk])
                        if on_act:
                            TS(DF[:, 0:NIT + 1], cs("frow")[:, 0:NIT + 1], B[:, 2:3], 0.5, ALU.mult, ALU.mult, [bk, "cst"], [dk])
                            STT(B[:, 3:4], DF[:, 0:1], 2.0, B[:, 0:1], ALU.mult, ALU.add, [bk, dk], [bk])
                        else:
                            TS(DF[:, 0:NIT + 1], cs("frow")[:, 0:NIT + 1], B[:, 2:3], None, ALU.mult, None, [bk, "cst"], [dk])
                            TT(B[:, 3:4], B[:, 0:1], DF[:, 0:1], ALU.add, [bk, dk], [bk])
                    MSET(scb[0:64, Sr - 64:Sr], -1e30, [sk])
                    if Sr > TOPK:
                        for it in range(NIT):
                            if on_act:
                                ACT(junkA[:, 0:Sr], scb[:, 0:Sr], AF.Sign, [sk, bk], ["junkA", bk], bias=B[:, 3:4], scale=-1.0, accum=B[:, 4:5])
                                ACT(B[:, 5:6], B[:, 4:5], AF.Sign, [bk], [bk], bias=float(Sr - 2 * TOPK + 0.5), scale=-1.0)
                                ACT(B[:, 3:4], B[:, 5:6], AF.Identity, [bk, dk], [bk], bias=B[:, 3:4], scale=DF[:, it:it + 1])
                            else:
                                TS(junk[:, 0:Sr], scb[:, 0:Sr], B[:, 3:4], None, ALU.is_ge, ALU.add, [sk, bk], ["sq_b", bk], accum=B[:, 4:5])
                                TS(B[:, 5:6], B[:, 4:5], TOPK - 0.5, 0.5, ALU.is_ge, ALU.subtract, [bk], [bk])
                                STT(B[:, 3:4], B[:, 5:6], DF[:, it:it + 1], B[:, 3:4], ALU.mult, ALU.add, [bk, dk], [bk])
                        if on_act:
                            ACT(B[:, 6:7], B[:, 3:4], AF.Identity, [bk], [bk], scale=-1.0)
                            ACT(B[:, 0:1], DF[:, NIT:NIT + 1], AF.Identity, [bk, dk], [bk], bias=B[:, 6:7], scale=2.0)
                            if Sr - 64 <= TOPK:
                                MSET(B[0:64, 0:1], 1e29, [bk], eng="pool")
                        else:
                            TT(B[:, 0:1], B[:, 3:4], DF[:, NIT:NIT + 1], ALU.subtract, [bk, dk], [bk])
                            if Sr - 64 <= TOPK:
                                MSET(B[0:64, 0:1], -1e29, [bk])
                    else:
                        on_act = False
                        MSET(B[:, 0:1], -1e29, [bk])
                    if on_act:
                        ACT(junkA[:, 0:Sr], scb[:, 0:Sr], AF.Sign, [sk, bk], ["junkA"], bias=B[:, 0:1])
                        ACT(nmA[:, 0:Sr], junkA[:, 0:Sr], AF.Relu, ["junkA"], ["nmA"], scale=NEGB)
                    elif qt % 2 == 1:
                        TS(nmA[:, 0:Sr], scb[:, 0:Sr], B[:, 0:1], NEGB, ALU.is_lt, ALU.mult, [sk, bk], ["nmA"])
                    else:
                        TS(nm[:, 0:Sr], scb[:, 0:Sr], B[:, 0:1], NEGB, ALU.is_lt, ALU.mult, [sk, bk], ["nm"])
                    nm_neg[qt] = on_act

                def S3(qt):
                    if qt < 0:
                        q0, nq, kts = 48, 16, []
                    else:
                        q0, nq, kts = 64 + 128 * qt, 128, list(range(qt + 1))
                    NQ = 4 * nq
                    keysets = [(-1, 16)] + [(kt, 128) for kt in kts]
                    for ki, (kt, nk) in enumerate(keysets):
                        pi = nps()
                        first, last = ki == 0, ki == len(keysets) - 1
                        kc0 = 48 if kt < 0 else 64 + 128 * kt
                        for hh in range(4):
                            MM(ps[pi][0:nk, hh * nq:(hh + 1) * nq], kTd[:, kc0:kc0 + nk], qT4[hh][:, q0:q0 + nq], True, kt < 0,
                               ["kTd", qk4[hh]], [f"ps{pi}"])
                            if kt >= 0:
                                i4x, i4k = (i4n_b, "i4n_b") if nm_neg.get(qt) else (i4_b, "i4_b")
                                nmx, nmk = (nmA, "nmA") if qt % 2 == 1 else (nm, "nm")
                                MM(ps[pi][0:nk, hh * nq:(hh + 1) * nq], nmx[:, 128 * kt:128 * kt + 128], i4x[:, hh * 128:(hh + 1) * 128], False, True,
                                   [nmk, i4k], [f"ps{pi}"])
                        tb = t_b[ki % 2]
                        tkey = f"t_b{ki % 2}"
                        ACT(tb[0:nk, 0:NQ], ps[pi][0:nk, 0:NQ], AF.Exp, [f"ps{pi}"], [tkey], scale=att_scale)
                        vsrc = v_me[:, :] if kt < 0 else v_tm[:, kt, :]
                        vkey = "v_me" if kt < 0 else "v_tm"
                        MM(ps[5][:, 0:NQ], vsrc, tb[0:nk, 0:NQ], first, last, [vkey, tkey], ["ps5"])
                        MM(ps[6][:, 0:NQ], ones_b[0:nk, :], tb[0:nk, 0:NQ], first, last, ["ones_b", tkey], ["ps6"])
                    rz = rs_f[0]
                    P.op("dve", lambda e: e.reciprocal(out=rz[:, 0:NQ], in_=ps[6][:, 0:NQ]), ["ps6"], ["rs_f0"])
                    TT(oab[:, 4:8, q0:q0 + nq], ps[5][:, 0:NQ].rearrange("p (h q) -> p h q", h=4), rz[:, 0:NQ].rearrange("p (h q) -> p h q", h=4),
                       ALU.mult, ["ps5", "rs_f0"], ["oab"])

                S3(-1)
                pairs = [(a_, a_ + 1 if a_ + 1 < NQT else None) for a_ in range(0, NQT, 2)]

                def s1pair(pp):
                    for t_ in pp:
                        if t_ is not None:
                            S1(t_)

                def s2pair(pp):
                    if pp[1] is not None:
                        S2(pp[1])
                    S2(pp[0])

                s1pair(pairs[0])
                s2pair(pairs[0])
                for pi_ in range(len(pairs)):
                    if pi_ + 1 < len(pairs):
                        s1pair(pairs[pi_ + 1])
                    for t_ in pairs[pi_]:
                        if t_ is not None:
                            S3(t_)
                    if pi_ + 1 < len(pairs):
                        s2pair(pairs[pi_ + 1])
                P.barrier()

                psmod[0] = 7
                cv = Carver(arenaH, ARH, HWD // 2)
                yT = [cv.get(128, [Tp], BF16) for _ in range(KC)]
                ykeys = [f"yT{i}" for i in range(KC)]
                wg_ = w_gate_d[l].rearrange("(kc p) n -> p kc n", p=128)
                for f in range(KC):
                    wv, wkey = wnext(KC * 256, KC)
                    DMA(wv[:, :, 0:128], wg_[:, :, f * 128:(f + 1) * 128], (), [wkey], eng="pool")
                    DMA(wv[:, :, 128:256], wg_[:, :, D + f * 128:D + (f + 1) * 128], (), [wkey], eng="pool")
                    wb, wbkey = wnext(8 * 128, 8)
                    DMA(wb[:, 0:4, :], w_ba_d[l].rearrange("(kc p) n -> p kc n", p=128)[:, :, f * 128:(f + 1) * 128], (), [wbkey], eng="pool")
                    DMA(wb[:, 4:8, :], w_bb_d[l].rearrange("(kc p) n -> p kc n", p=128)[:, :, f * 128:(f + 1) * 128], (), [wbkey], eng="pool")
                    for bi, (t0, w) in enumerate(blocks):
                        pga, pgb, pya, pyb = nps(), nps(), nps(), nps()
                        proj_block(wv, wkey, 0, 128, t0, w, pga)
                        proj_block(wv, wkey, 128, 128, t0, w, pgb)
                        proj_block(wb, wbkey, 0, 128, t0, w, pya, kcs=4, src=oab, srckey="oab", srcoff=0)
                        proj_block(wb[:, 4:8, :], wbkey, 0, 128, t0, w, pyb, kcs=4, src=oab, srckey="oab", srcoff=4)
                        ACT(t_f[0][:, 0:w], ps[pga][:, 0:w], AF.Sigmoid, [f"ps{pga}", "lpt"], ["t_f0"], bias=lp("bgate", f))
                        ACT(t_f[1][:, 0:w], ps[pgb][:, 0:w], AF.Sigmoid, [f"ps{pgb}", "lpt"], ["t_f1"], bias=lp("bgate", KC + f))
                        TT(t_f[0][:, 0:w], t_f[0][:, 0:w], ps[pya][:, 0:w], ALU.mult, ["t_f0", f"ps{pya}"], ["t_f0"])
                        TT(t_f[1][:, 0:w], t_f[1][:, 0:w], ps[pyb][:, 0:w], ALU.mult, ["t_f1", f"ps{pyb}"], ["t_f1"])
                        TT(yT[f][:, t0:t0 + w], t_f[0][:, 0:w], t_f[1][:, 0:w], ALU.add, ["t_f0", "t_f1"], [ykeys[f]])
                P.barrier()
                KH = KC // 2
                hlo = arenaH[:, 0:HWD // 2].rearrange("p (k t) -> p k t", k=KH)
                hhi = arenaN[:, :].rearrange("p (k t) -> p k t", k=KC - KH)
                DMA(arenaH[:, 0:HWD // 2], hsp_d[:, 0:HWD // 2], ["hsp"], ["hlo"])
                DMA(arenaN[:, :], hsp_d[:, HWD // 2:HWD], ["hsp"], ["hhi"])
                for f in range(KC):
                    wv, wkey = wload(wk(w_out_d, l, f * 128, 128), [128, KC, 128])
                    dst, dkey = (hlo[:, f, :], "hlo") if f < KH else (hhi[:, f - KH, :], "hhi")
                    for (t0, w) in blocks:
                        pi = nps()
                        for kc in range(KC):
                            MM(ps[pi][:, 0:w], wv[:, kc, :], yT[kc][:, t0:t0 + w], kc == 0, kc == KC - 1, [wkey, ykeys[kc]], [f"ps{pi}"])
                        TT(dst[:, t0:t0 + w], dst[:, t0:t0 + w], ps[pi][:, 0:w], ALU.add, [dkey, f"ps{pi}"], [dkey])
                P.barrier()
                for f in range(KH, KC):
                    CP(hT[:, f, :], hhi[:, f - KH, :], ["hhi"], ["hT"], eng=("act" if f % 2 else "dve"))
                P.barrier()
                rmsnorm_to_nT("gffn")
                P.barrier()
                cv = Carver(arenaO, ARO)
                f_a = cv.get(128, [Tp + 8], F32)
                f_b = cv.get(128, [Tp], F32)
                b_e = cv.get(128, [Tp], BF16)
                b_f = cv.get(128, [Tp], BF16)
                actb = []
                for i_ in range(GF):
                    if i_ == 2 and 4096 >= Tp:
                        actb.append(rtab_all[:, :, :].rearrange("p a b -> p (a b)").bitcast(BF16)[:, 0:Tp])
                    elif i_ == 3 and KC * 512 >= Tp:
                        actb.append(sq_b[:, :, :].rearrange("p a b -> p (a b)")[:, 0:Tp])
                    else:
                        actb.append(cv.get(128, [Tp], BF16))
                akeys = [f"actb{i}" for i in range(GF)]
                MSET(f_a[:, 0:2], 0.0, ["f_a0"])
                wu_ = w_up_d[l].rearrange("(kc p) n -> p kc n", p=128)
                for g0 in range(0, NF, GF):
                    ng = min(GF, NF - g0)
                    for jj in range(ng):
                        j = g0 + jj
                        wv, wkey = wnext(KC * 256, KC)
                        DMA(wv[:, :, 0:128], wu_[:, :, j * 128:(j + 1) * 128], (), [wkey], eng="pool")
                        DMA(wv[:, :, 128:256], wu_[:, :, cfg.DFF + j * 128:cfg.DFF + (j + 1) * 128], (), [wkey], eng="pool")
                        cw = lambda i, j=j: lp("convf", j * 3 + i)
                        for bi, (t0, w) in enumerate(blocks):
                            pg, pu = nps(), nps()
                            proj_block(wv, wkey, 0, 128, t0, w, pg)
                            proj_block(wv, wkey, 128, 128, t0, w, pu)
                            CP(f_a[:, 2 + t0:2 + t0 + w], ps[pg][:, 0:w], [f"ps{pg}"], [f"f_a{bi}"], eng="act")
                            CP(b_e[:, t0:t0 + w], ps[pu][:, 0:w], [f"ps{pu}"], [f"b_e{bi}"], eng="act")
                            rk = [f"f_a{bi}"] + ([f"f_a{bi - 1}"] if bi else [])
                            fk = f"f_b{bi}"
                            TS(f_b[:, t0:t0 + w], f_a[:, 2 + t0:2 + t0 + w], cw(2), None, ALU.mult, None, rk + ["lpt"], [fk])
                            for i in (1, 0):
                                STT(f_b[:, t0:t0 + w], f_a[:, i + t0:i + t0 + w], cw(i), f_b[:, t0:t0 + w], ALU.mult, ALU.add, rk + [fk, "lpt"], [fk])
                            ACT(b_f[:, t0:t0 + w], f_b[:, t0:t0 + w], AF.Silu, [fk], [f"b_f{bi}"])
                            TT(actb[jj][:, t0:t0 + w], b_f[:, t0:t0 + w], b_e[:, t0:t0 + w], ALU.mult, [f"b_f{bi}", f"b_e{bi}"], [f"{akeys[jj]}_{bi}"])
                    for f in range(KC):
                        wv, wkey = wload(w_down_d[l].rearrange("(kc p) n -> p kc n", p=128)[:, g0:g0 + ng, f * 128:(f + 1) * 128], [128, ng, 128])
                        for bi, (t0, w) in enumerate(blocks):
                            pi = nps()
                            for jj in range(ng):
                                MM(ps[pi][:, 0:w], wv[:, jj, :], actb[jj][:, t0:t0 + w], jj == 0, jj == ng - 1, [wkey, f"{akeys[jj]}_{bi}"], [f"ps{pi}"])
                            TT(hT[:, f, t0:t0 + w], hT[:, f, t0:t0 + w], ps[pi][:, 0:w], ALU.add, [f"hT{f}_{bi}", f"ps{pi}"], [f"hT{f}_{bi}"])
                P.barrier()

            P.barrier()
            cvx_ = Carver(arenaO, ARO)
            xins_ = [cvx_.get(128, [D], F32) for _ in range(2)]
            for r in range(NQT):
                c0 = 64 + 128 * r
                xin, xk = xins_[r % 2], f"xin{r % 2}"
                for kc in range(KC):
                    pi = nps()
                    TR(ps[pi][:, 0:128], hT[:, kc, c0:c0 + 128], identF[:, :], ["hT", "cst"], [f"ps{pi}"])
                    CP(xin[:, kc * 128:(kc + 1) * 128], ps[pi][:, 0:128], [f"ps{pi}"], [xk], eng=("act" if kc % 2 else "dve"))
                final.append(DMA(y_d[sq, 128 * r:128 * (r + 1), :], xin[:, :], [xk], [f"y{r}"]))
        P.emit(final)
    return nc


_CACHE = {}


def make_in_maps(cfg, ncores, x, meta_tokens, norm_mix, w_in, conv_a, a_log, dt_bias, a_out_norm, q_norm, k_norm,
                 kidx_norm, w_branch_a, w_branch_b, w_gate, b_gate, w_out, norm_ffn, w_up, conv_ffn, w_down):
    f = lambda a: np.ascontiguousarray(np.asarray(a, dtype=np.float32))
    consts = host_consts(cfg)
    lps = np.stack([host_lparams(cfg, l, f(norm_mix), f(norm_ffn), f(conv_a), f(conv_ffn), f(b_gate), f(a_out_norm), f(q_norm),
                                 f(k_norm), f(kidx_norm), f(a_log), f(dt_bias)) for l in range(cfg.DEPTH)])
    x = f(x)
    shared = {"meta": f(meta_tokens), "consts": consts, "lparams": lps, "w_in": f(w_in), "w_branch_a": f(w_branch_a),
              "w_branch_b": f(w_branch_b), "w_gate": f(w_gate), "w_out": f(w_out), "w_up": f(w_up), "w_down": f(w_down)}
    maps = []
    for c in range(ncores):
        m = dict(shared)
        m["x"] = np.ascontiguousarray(x[c * cfg.NSEQ:(c + 1) * cfg.NSEQ])
        maps.append(m)
    return maps


def kernel(**inputs):
    cfg = Cfg()
    ncores = 8
    if "nc" not in _CACHE:
        _CACHE["nc"] = build(cfg)
    nc = _CACHE["nc"]
    maps = make_in_maps(cfg, ncores, **inputs)
    res = run_bass_kernel_spmd(nc, maps, core_ids=list(range(ncores)))
    return np.concatenate([np.asarray(r["y"]) for r in res.results], axis=0).astype(np.float32)
```
